# Optimizing a Trainium2 kernel written in Bass

```python
import math
import jax
import jax.numpy as jnp
from jax import lax
import numpy as np

D_MODEL = 1024
BATCH = 16
SEQ = 256
DEPTH = 4
DEC_BATCH = 8
DEC_SEQ = 2048
PAST_LEN = 512

GRID_W = 64
N_MIXERS = 3
N_RET = (DEPTH + 2) // 3
N_GLA = (DEPTH + 1) // 3
N_S5 = DEPTH // 3
RET_HEADS = 4
RET_DK = D_MODEL // RET_HEADS
RET_DV = 2 * D_MODEL // RET_HEADS
RET_CHUNK = 128
ROPE_BASE = 10000.0
GLA_HEADS = 4
GLA_KEY = D_MODEL // 2
GLA_DK = GLA_KEY // GLA_HEADS
GLA_DV = D_MODEL // GLA_HEADS
GLA_RANK = 16
GLA_TAU = 16.0
GLA_CHUNK = 16
S5_GROUP = 16
S5_GROUPS = D_MODEL // S5_GROUP
S5_STATE = 64
N_EXPERTS = 32
TOP_K = 4
D_FF = D_MODEL
SWIGLU_ALPHA = 1.702
SWIGLU_LIMIT = 7.0
MOE_BLOCK = 128
DN_ALPHA = (2 * DEPTH) ** 0.25
DN_BETA = (8 * DEPTH) ** -0.25
LN_EPS = 1e-5
GN_EPS = 1e-5

kernel_name = 'hybrid_ret_gla_s5_moe_diffusion_step'


def layer_norm(x, g, b):
    xf = x.astype(jnp.float32)
    mu = jnp.mean(xf, axis=-1, keepdims=True)
    var = jnp.mean(jnp.square(xf - mu), axis=-1, keepdims=True)
    return ((xf - mu) * lax.rsqrt(var + LN_EPS) * g.astype(jnp.float32) + b.astype(jnp.float32)).astype(x.dtype)


def head_norm(o):
    of = o.astype(jnp.float32)
    mu = jnp.mean(of, axis=-1, keepdims=True)
    var = jnp.mean(jnp.square(of - mu), axis=-1, keepdims=True)
    return ((of - mu) * lax.rsqrt(var + GN_EPS)).astype(o.dtype)


def grid_rotary(x):
    L = x.shape[1]
    rows = L // GRID_W
    row = jnp.repeat(jnp.arange(rows, dtype=jnp.float32), GRID_W)
    col = jnp.tile(jnp.arange(GRID_W, dtype=jnp.float32), rows)
    n_freq = x.shape[-1] // 4
    inv = ROPE_BASE ** (-jnp.arange(n_freq, dtype=jnp.float32) / n_freq)
    ang = jnp.concatenate([row[:, None] * inv, col[:, None] * inv], axis=-1)
    cos = jnp.cos(ang)[None, :, None, :].astype(x.dtype)
    sin = jnp.sin(ang)[None, :, None, :].astype(x.dtype)
    half = x.shape[-1] // 2
    x1, x2 = x[..., :half], x[..., half:]
    return jnp.concatenate([x1 * cos - x2 * sin, x1 * sin + x2 * cos], axis=-1)


def _to_chunks(t, chunk):
    bsz, L = t.shape[:2]
    return t.reshape(bsz, L // chunk, chunk, *t.shape[2:]).transpose(1, 0, 3, 2, 4)


def _from_chunks(t):
    n, bsz, H, chunk, d = t.shape
    return t.transpose(1, 0, 3, 2, 4).reshape(bsz, n * chunk, H, d)


def retention_scan(q, k, v, log_gamma, s0):
    dt = q.dtype
    idx = jnp.arange(RET_CHUNK, dtype=jnp.float32)
    rel = idx[:, None] - idx[None, :]
    lg = log_gamma[:, None, None]
    dmat = jnp.where(rel >= 0, jnp.exp(lg * jnp.maximum(rel, 0.0)), 0.0).astype(dt)
    xi = jnp.exp(log_gamma[:, None] * (idx + 1.0))[..., None].astype(dt)
    zeta = jnp.exp(log_gamma[:, None] * (RET_CHUNK - 1.0 - idx))[..., None].astype(dt)
    g_chunk = jnp.exp(log_gamma * RET_CHUNK)[:, None, None].astype(dt)

    def step(s, inp):
        qc, kc, vc = inp
        scores = jnp.einsum('bhnd,bhmd->bhnm', qc, kc) * dmat
        o = jnp.einsum('bhnm,bhme->bhne', scores, vc) + jnp.einsum('bhnd,bhde->bhne', qc, s) * xi
        s = s * g_chunk + jnp.einsum('bhmd,bhme->bhde', kc * zeta, vc)
        return s, o

    s, o = lax.scan(step, s0.astype(dt), (_to_chunks(q, RET_CHUNK), _to_chunks(k, RET_CHUNK), _to_chunks(v, RET_CHUNK)))
    return _from_chunks(o), s


def retention_mixer(h, w_in, decay_logit, w_out, s0, grid):
    bsz, L, _ = h.shape
    q, k, v, g = jnp.split(h @ w_in, [D_MODEL, 2 * D_MODEL, 4 * D_MODEL], axis=-1)
    q = q.reshape(bsz, L, RET_HEADS, RET_DK) * (RET_DK ** -0.5)
    k = k.reshape(bsz, L, RET_HEADS, RET_DK)
    v = v.reshape(bsz, L, RET_HEADS, RET_DV)
    if grid:
        q, k = grid_rotary(q), grid_rotary(k)
    log_gamma = jax.nn.log_sigmoid(decay_logit.astype(jnp.float32))
    o_f, s_f = retention_scan(q, k, v, log_gamma[0], s0[:, 0])
    o_b, s_b = retention_scan(q[:, ::-1], k[:, ::-1], v[:, ::-1], log_gamma[1], s0[:, 1])
    o = head_norm(o_f + o_b[:, ::-1]).reshape(bsz, L, 2 * D_MODEL)
    return (jax.nn.silu(g) * o) @ w_out, jnp.stack([s_f, s_b], axis=1)


def gla_scan(q, k, v, log_a, s0):
    mask = jnp.tril(jnp.ones((GLA_CHUNK, GLA_CHUNK), dtype=bool))[:, :, None]

    def step(s, inp):
        qc, kc, vc, gc = inp
        b = jnp.cumsum(gc, axis=2)
        diff = b[:, :, :, None, :] - b[:, :, None, :, :]
        decay = jnp.where(mask, jnp.exp(jnp.minimum(diff, 0.0)), 0.0).astype(qc.dtype)
        scores = jnp.einsum('bhnd,bhmd,bhnmd->bhnm', qc, kc, decay)
        b_last = b[:, :, -1:, :]
        o = (jnp.einsum('bhnm,bhme->bhne', scores, vc)
             + jnp.einsum('bhnd,bhde->bhne', qc * jnp.exp(b).astype(qc.dtype), s))
        s = (jnp.exp(b_last[:, :, 0, :, None]).astype(s.dtype) * s
             + jnp.einsum('bhmd,bhme->bhde', kc * jnp.exp(b_last - b).astype(kc.dtype), vc))
        return s, o

    s, o = lax.scan(step, s0.astype(q.dtype),
                    (_to_chunks(q, GLA_CHUNK), _to_chunks(k, GLA_CHUNK), _to_chunks(v, GLA_CHUNK), _to_chunks(log_a, GLA_CHUNK)))
    return _from_chunks(o), s


def gla_mixer(h, w_in, w_a1, w_a2, b_a, w_out, s0):
    bsz, L, _ = h.shape
    q, k, v, r = jnp.split(h @ w_in, [GLA_KEY, 2 * GLA_KEY, 2 * GLA_KEY + D_MODEL], axis=-1)
    q = q.reshape(bsz, L, GLA_HEADS, GLA_DK) * (GLA_DK ** -0.5)
    k = k.reshape(bsz, L, GLA_HEADS, GLA_DK)
    v = v.reshape(bsz, L, GLA_HEADS, GLA_DV)

    def log_gate(d):
        z = (h @ w_a1[d]) @ w_a2[d] + b_a[d]
        return (jax.nn.log_sigmoid(z.astype(jnp.float32)) / GLA_TAU).reshape(bsz, L, GLA_HEADS, GLA_DK)

    la_f, la_b = log_gate(0), log_gate(1)
    o_f, s_f = gla_scan(q, k, v, la_f, s0[:, 0])
    o_b, s_b = gla_scan(q[:, ::-1], k[:, ::-1], v[:, ::-1], la_b[:, ::-1], s0[:, 1])
    o = head_norm(o_f + o_b[:, ::-1]).reshape(bsz, L, D_MODEL)
    return (jax.nn.silu(r) * o) @ w_out, jnp.stack([s_f, s_b], axis=1)


def _complex_affine_combine(e1, e2):
    a1r, a1i, b1r, b1i = e1
    a2r, a2i, b2r, b2i = e2
    return (a2r * a1r - a2i * a1i,
            a2r * a1i + a2i * a1r,
            a2r * b1r - a2i * b1i + b2r,
            a2r * b1i + a2i * b1r + b2i)


def s5_scan(u, a_re, a_im, log_step, b_re, b_im, c_re, c_im, x0_re, x0_im):
    f32 = jnp.float32
    dt = u.dtype
    step = jnp.exp(log_step.astype(f32))[:, None]
    ar, ai = a_re.astype(f32), a_im.astype(f32)
    mag = jnp.exp(ar * step)
    ab_re, ab_im = mag * jnp.cos(ai * step), mag * jnp.sin(ai * step)
    den = ar * ar + ai * ai
    f_re = ((ab_re - 1.0) * ar + ab_im * ai) / den
    f_im = (ab_im * ar - (ab_re - 1.0) * ai) / den
    br, bi = b_re.astype(f32), b_im.astype(f32)
    bb_re = (f_re[..., None] * br - f_im[..., None] * bi).astype(dt)
    bb_im = (f_re[..., None] * bi + f_im[..., None] * br).astype(dt)
    ab_re, ab_im = ab_re.astype(dt), ab_im.astype(dt)
    bu_re = jnp.einsum('blgc,gpc->blgp', u, bb_re)
    bu_im = jnp.einsum('blgc,gpc->blgp', u, bb_im)
    x0r, x0i = x0_re.astype(dt), x0_im.astype(dt)
    bu_re = bu_re.at[:, 0].add(ab_re * x0r - ab_im * x0i)
    bu_im = bu_im.at[:, 0].add(ab_re * x0i + ab_im * x0r)
    L = u.shape[1]
    a_seq_re = jnp.broadcast_to(ab_re, (1, L) + ab_re.shape)
    a_seq_im = jnp.broadcast_to(ab_im, (1, L) + ab_im.shape)
    _, _, x_re, x_im = lax.associative_scan(_complex_affine_combine, (a_seq_re, a_seq_im, bu_re, bu_im), axis=1)
    y = jnp.einsum('blgp,gcp->blgc', x_re, c_re) - jnp.einsum('blgp,gcp->blgc', x_im, c_im)
    return y, x_re[:, -1], x_im[:, -1]


def s5_mixer(h, a_re, a_im, log_step, b_re, b_im, c_re, c_im, d_skip, w_glu, s0_re, s0_im):
    bsz, L, _ = h.shape
    u = h.reshape(bsz, L, S5_GROUPS, S5_GROUP)
    y_f, xf_re, xf_im = s5_scan(u, a_re[0], a_im[0], log_step[0], b_re[0], b_im[0], c_re[0], c_im[0], s0_re[:, 0], s0_im[:, 0])
    ub = u[:, ::-1]
    y_b, xb_re, xb_im = s5_scan(ub, a_re[1], a_im[1], log_step[1], b_re[1], b_im[1], c_re[1], c_im[1], s0_re[:, 1], s0_im[:, 1])
    y = (y_f + y_b[:, ::-1]).reshape(bsz, L, D_MODEL) + d_skip * h
    gv = jax.nn.gelu(y) @ w_glu
    out = gv[..., :D_MODEL] * jax.nn.sigmoid(gv[..., D_MODEL:])
    return out, jnp.stack([xf_re, xb_re], axis=1), jnp.stack([xf_im, xb_im], axis=1)


def moe(h, w_router, b_router, w_gu, b_gu, w_down, b_down):
    bsz, L, D = h.shape
    x = h.reshape(-1, D)
    n_tok = x.shape[0]
    logits = (x @ w_router + b_router).astype(jnp.float32)
    top_val, top_idx = lax.top_k(logits, TOP_K)
    gates = jax.nn.softmax(top_val, axis=-1)
    n_assign = n_tok * TOP_K
    flat_e = top_idx.reshape(-1)
    flat_tok = jnp.arange(n_assign, dtype=jnp.int32) // TOP_K
    order = jnp.argsort(flat_e)
    sorted_e = flat_e[order]
    counts = jnp.bincount(flat_e, length=N_EXPERTS)
    padded = (counts + MOE_BLOCK - 1) // MOE_BLOCK * MOE_BLOCK
    start = jnp.cumsum(counts) - counts
    pend = jnp.cumsum(padded)
    pstart = pend - padded
    dest = pstart[sorted_e] + jnp.arange(n_assign) - start[sorted_e]
    n_blocks = -(-n_assign // MOE_BLOCK) + N_EXPERTS
    n_rows = n_blocks * MOE_BLOCK
    row_tok = jnp.full((n_rows,), n_tok, jnp.int32).at[dest].set(flat_tok[order])
    row_gate = jnp.zeros((n_rows,), jnp.float32).at[dest].set(gates.reshape(-1)[order])
    block_e = jnp.minimum(jnp.searchsorted(pend, jnp.arange(n_blocks) * MOE_BLOCK, side='right'), N_EXPERTS - 1)
    x_pad = jnp.concatenate([x, jnp.zeros((1, D), x.dtype)], axis=0)
    xb = x_pad[row_tok].reshape(n_blocks, MOE_BLOCK, D)

    def expert_block(args):
        xblk, e = args
        gu = xblk @ w_gu[e] + b_gu[e]
        gate = jnp.minimum(gu[:, :D_FF], SWIGLU_LIMIT)
        lin = jnp.clip(gu[:, D_FF:], -SWIGLU_LIMIT, SWIGLU_LIMIT)
        act = gate * jax.nn.sigmoid(SWIGLU_ALPHA * gate) * (lin + 1.0)
        return act @ w_down[e] + b_down[e]

    yb = lax.map(expert_block, (xb, block_e)).reshape(n_rows, D)
    y = jax.ops.segment_sum(yb * row_gate[:, None].astype(yb.dtype), row_tok, num_segments=n_tok + 1)[:n_tok]
    return y.reshape(bsz, L, D)


def trunk(x, cond, ret_s0, gla_s0, s5_s0_re, s5_s0_im, p, grid):
    ret_i = gla_i = s5_i = 0
    new_ret, new_gla, new_s5_re, new_s5_im = [], [], [], []
    for layer in range(DEPTH):
        mod = jax.nn.silu(cond) @ p['w_mod'][layer] + p['b_mod'][layer]
        sh1, sc1, g1, sh2, sc2, g2 = jnp.split(mod[:, None, :], 6, axis=-1)
        h = x * (1.0 + sc1) + sh1
        kind = layer % N_MIXERS
        if kind == 0:
            y, st = retention_mixer(h, p['ret_w_in'][ret_i], p['ret_decay'][ret_i], p['ret_w_out'][ret_i], ret_s0[:, ret_i], grid)
            new_ret.append(st)
            ret_i += 1
        elif kind == 1:
            y, st = gla_mixer(h, p['gla_w_in'][gla_i], p['gla_w_a1'][gla_i], p['gla_w_a2'][gla_i], p['gla_b_a'][gla_i],
                              p['gla_w_out'][gla_i], gla_s0[:, gla_i])
            new_gla.append(st)
            gla_i += 1
        else:
            y, st_re, st_im = s5_mixer(h, p['s5_a_re'][s5_i], p['s5_a_im'][s5_i], p['s5_log_step'][s5_i],
                                       p['s5_b_re'][s5_i], p['s5_b_im'][s5_i], p['s5_c_re'][s5_i], p['s5_c_im'][s5_i],
                                       p['s5_d'][s5_i], p['s5_w_glu'][s5_i], s5_s0_re[:, s5_i], s5_s0_im[:, s5_i])
            new_s5_re.append(st_re)
            new_s5_im.append(st_im)
            s5_i += 1
        x = layer_norm(DN_ALPHA * x + g1 * y, p['ln_g'][layer, 0], p['ln_b'][layer, 0])
        h = x * (1.0 + sc2) + sh2
        y = moe(h, p['moe_w_router'][layer], p['moe_b_router'][layer], p['moe_w_gu'][layer], p['moe_b_gu'][layer],
                p['moe_w_down'][layer], p['moe_b_down'][layer])
        x = layer_norm(DN_ALPHA * x + g2 * y, p['ln_g'][layer, 1], p['ln_b'][layer, 1])
    return x, jnp.stack(new_ret, axis=1), jnp.stack(new_gla, axis=1), jnp.stack(new_s5_re, axis=1), jnp.stack(new_s5_im, axis=1)


def setup_inputs(seed: int = 0) -> dict:
    key = jax.random.key(seed)
    ks = iter(jax.random.split(key, 48))
    f32 = jnp.float32

    def nrm(shape, scale):
        return jax.random.normal(next(ks), shape, f32) * scale

    D = D_MODEL
    G, P = S5_GROUPS, S5_STATE
    ret_logit0 = jnp.log(2.0 ** (5.0 + jnp.arange(RET_HEADS, dtype=f32)) - 1.0)
    s5_log_step = jax.random.uniform(next(ks), (N_S5, 2, G), f32, minval=math.log(1e-3), maxval=math.log(1e-1))
    glu = nrm((N_S5, D, 2 * D), D ** -0.5)
    glu = glu.at[:, :, :D].multiply(DN_BETA)
    return {
        'x_prompt': nrm((BATCH, SEQ, D), 1.0),
        'x_sample': nrm((DEC_BATCH, DEC_SEQ, D), 1.0),
        'state_ret': nrm((DEC_BATCH, N_RET, 2, RET_HEADS, RET_DK, RET_DV), 1.0),
        'state_gla': nrm((DEC_BATCH, N_GLA, 2, GLA_HEADS, GLA_DK, GLA_DV), 1.0),
        'state_s5_re': nrm((DEC_BATCH, N_S5, 2, G, P), 0.1),
        'state_s5_im': nrm((DEC_BATCH, N_S5, 2, G, P), 0.1),
        'c': nrm((DEC_BATCH, D), 1.0),
        'c_ctx': nrm((D,), 1.0),
        'w_mod': nrm((DEPTH, D, 6 * D), D ** -0.5),
        'b_mod': nrm((DEPTH, 6 * D), 0.01),
        'ln_g': 1.0 + nrm((DEPTH, 2, D), 0.01),
        'ln_b': nrm((DEPTH, 2, D), 0.01),
        'ret_w_in': nrm((N_RET, D, 6 * D), D ** -0.5),
        'ret_decay': ret_logit0 + nrm((N_RET, 2, RET_HEADS), 0.01),
        'ret_w_out': nrm((N_RET, 2 * D, D), (2 * D) ** -0.5 * DN_BETA),
        'gla_w_in': nrm((N_GLA, D, 2 * GLA_KEY + 2 * D), D ** -0.5),
        'gla_w_a1': nrm((N_GLA, 2, D, GLA_RANK), D ** -0.5),
        'gla_w_a2': nrm((N_GLA, 2, GLA_RANK, GLA_KEY), GLA_RANK ** -0.5),
        'gla_b_a': nrm((N_GLA, 2, GLA_KEY), 0.01),
        'gla_w_out': nrm((N_GLA, D, D), D ** -0.5 * DN_BETA),
        's5_a_re': -0.5 + nrm((N_S5, 2, G, P), 0.01),
        's5_a_im': jnp.pi * jnp.arange(P, dtype=f32) + nrm((N_S5, 2, G, P), 0.01),
        's5_log_step': s5_log_step,
        's5_b_re': nrm((N_S5, 2, G, P, S5_GROUP), (2 * S5_GROUP) ** -0.5),
        's5_b_im': nrm((N_S5, 2, G, P, S5_GROUP), (2 * S5_GROUP) ** -0.5),
        's5_c_re': nrm((N_S5, 2, G, S5_GROUP, P), (2 * P) ** -0.5),
        's5_c_im': nrm((N_S5, 2, G, S5_GROUP, P), (2 * P) ** -0.5),
        's5_d': nrm((N_S5, D), 1.0),
        's5_w_glu': glu,
        'moe_w_router': nrm((DEPTH, D, N_EXPERTS), D ** -0.5),
        'moe_b_router': nrm((DEPTH, N_EXPERTS), 0.01),
        'moe_w_gu': nrm((DEPTH, N_EXPERTS, D, 2 * D_FF), D ** -0.5),
        'moe_b_gu': nrm((DEPTH, N_EXPERTS, 2 * D_FF), 0.01),
        'moe_w_down': nrm((DEPTH, N_EXPERTS, D_FF, D), D_FF ** -0.5 * DN_BETA),
        'moe_b_down': nrm((DEPTH, N_EXPERTS, D), 0.01),
    }


def reference(x_prompt, x_sample, state_ret, state_gla, state_s5_re, state_s5_im, c, c_ctx,
              w_mod, b_mod, ln_g, ln_b, ret_w_in, ret_decay, ret_w_out,
              gla_w_in, gla_w_a1, gla_w_a2, gla_b_a, gla_w_out,
              s5_a_re, s5_a_im, s5_log_step, s5_b_re, s5_b_im, s5_c_re, s5_c_im, s5_d, s5_w_glu,
              moe_w_router, moe_b_router, moe_w_gu, moe_b_gu, moe_w_down, moe_b_down):
    p = dict(w_mod=w_mod, b_mod=b_mod, ln_g=ln_g, ln_b=ln_b,
             ret_w_in=ret_w_in, ret_decay=ret_decay, ret_w_out=ret_w_out,
             gla_w_in=gla_w_in, gla_w_a1=gla_w_a1, gla_w_a2=gla_w_a2, gla_b_a=gla_b_a, gla_w_out=gla_w_out,
             s5_a_re=s5_a_re, s5_a_im=s5_a_im, s5_log_step=s5_log_step, s5_b_re=s5_b_re, s5_b_im=s5_b_im,
             s5_c_re=s5_c_re, s5_c_im=s5_c_im, s5_d=s5_d, s5_w_glu=s5_w_glu,
             moe_w_router=moe_w_router, moe_b_router=moe_b_router, moe_w_gu=moe_w_gu, moe_b_gu=moe_b_gu,
             moe_w_down=moe_w_down, moe_b_down=moe_b_down)
    nb = x_prompt.shape[0]
    dt = x_prompt.dtype
    z_ret = jnp.zeros((nb, N_RET, 2, RET_HEADS, RET_DK, RET_DV), dt)
    z_gla = jnp.zeros((nb, N_GLA, 2, GLA_HEADS, GLA_DK, GLA_DV), dt)
    z_s5 = jnp.zeros((nb, N_S5, 2, S5_GROUPS, S5_STATE), dt)
    y_prompt, new_ret, new_gla, new_s5_re, new_s5_im = trunk(x_prompt, c_ctx[None, :], z_ret, z_gla, z_s5, z_s5, p, False)
    y_sample, _, _, _, _ = trunk(x_sample, c, state_ret, state_gla, state_s5_re, state_s5_im, p, True)
    return (y_prompt, y_sample, new_ret, new_gla, new_s5_re, new_s5_im)
```

```python
import contextlib
import os
import types
import numpy as np
import concourse.bass as bass
import concourse.mybir as mybir
from concourse.bass_utils import run_bass_kernel_spmd

F32 = mybir.dt.float32
BF16 = mybir.dt.bfloat16
I32 = mybir.dt.int32
ALU = mybir.AluOpType
AF = mybir.ActivationFunctionType
AX = mybir.AxisListType

EPOCH = 30000
PI = float(np.pi)


class Prog:
    def __init__(self, nc):
        self.nc = nc
        self.ops = []
        self.dch_order = []

    @staticmethod
    def _freeze(fn):
        if fn.__closure__ is None:
            return fn
        cells = []
        for c in fn.__closure__:
            try:
                cells.append(types.CellType(c.cell_contents))
            except ValueError:
                cells.append(c)
        return types.FunctionType(fn.__code__, fn.__globals__, fn.__name__, fn.__defaults__, tuple(cells))

    def op(self, eng, fn, reads=(), writes=(), dch=None):
        if dch is not None and dch not in self.dch_order:
            self.dch_order.append(dch)
        self.ops.append((eng, self._freeze(fn), tuple(reads), tuple(writes), dch))

    def pe(self, fn, reads=(), writes=()):
        self.op("pe", fn, reads, writes)

    def dve(self, fn, reads=(), writes=()):
        self.op("dve", fn, reads, writes)

    def act(self, fn, reads=(), writes=()):
        self.op("act", fn, reads, writes)

    def pool(self, fn, reads=(), writes=()):
        self.op("pool", fn, reads, writes)

    def dma(self, eng, fn, dch, reads=(), writes=()):
        self.op(eng, fn, reads, writes, dch)

    def barrier(self):
        self.ops.append(("BAR", None, (), (), None))

    def emit(self, final_keys=()):
        nc = self.nc
        ops = self.ops
        ENGS = ("pe", "dve", "act", "pool", "sp")
        last_w = {}
        readers = {}
        n_eng = {e: 0 for e in ENGS}
        dch_cnt = {}
        tokens = []
        waits = []
        needed = {e: set() for e in ENGS}
        seen = {e: {} for e in ENGS}
        pending_bar = {e: None for e in ENGS}

        def src_of(tok):
            return tok[1] if tok[0] == "e" else ("d", tok[1])

        for (eng, fn, reads, writes, dch) in ops:
            if eng == "BAR":
                snap = [("e", e, n_eng[e]) for e in ENGS if n_eng[e] > 0]
                snap += [("d", d, c) for d, c in dch_cnt.items()]
                for e in ENGS:
                    pending_bar[e] = snap
                tokens.append(None)
                waits.append([])
                continue
            if dch is None:
                n_eng[eng] += 1
                tok = ("e", eng, n_eng[eng])
            else:
                dch_cnt[dch] = dch_cnt.get(dch, 0) + 1
                tok = ("d", dch, dch_cnt[dch])
            deps = []
            if pending_bar[eng] is not None:
                for t in pending_bar[eng]:
                    if not (t[0] == "e" and t[1] == eng and eng in ("pe", "sp")):
                        deps.append((t, "bar"))
                pending_bar[eng] = None
            for k in reads:
                t = last_w.get(k)
                if t is not None:
                    deps.append((t, "raw"))
                if isinstance(k, tuple) and k[0] == "ps":
                    for t in readers.get(k, ()):
                        if t[0] == "e" and t[1] != eng:
                            deps.append((t, "rar"))
            for k in writes:
                t = last_w.get(k)
                if t is not None:
                    deps.append((t, "waw"))
                for t in readers.get(k, ()):
                    deps.append((t, "war"))
            if dch is not None and dch_cnt[dch] > 1:
                deps.append((("d", dch, dch_cnt[dch] - 1), "waw"))
            best = {}
            for t, kind in deps:
                if t == tok:
                    continue
                if t[0] == "e" and t[1] == eng and dch is None and kind != "bar":
                    if eng == "pe" or eng == "sp":
                        continue
                s = src_of(t)
                if seen[eng].get(s, 0) >= t[2]:
                    continue
                if s not in best or best[s][2] < t[2]:
                    best[s] = t
            for s, t in best.items():
                seen[eng][s] = t[2]
                if t[0] == "e":
                    needed[t[1]].add(t[2])
            waits.append(list(best.values()))
            tokens.append(tok)
            for k in writes:
                last_w[k] = tok
                readers[k] = []
            for k in reads:
                if k not in writes:
                    readers.setdefault(k, []).append(tok)
        fin = {}
        for k in final_keys:
            t = last_w.get(k)
            if t is not None:
                s = src_of(t)
                if s not in fin or fin[s][2] < t[2]:
                    fin[s] = t
                if t[0] == "e":
                    needed[t[1]].add(t[2])
        rank = {}
        for e in needed:
            for i, n in enumerate(sorted(needed[e])):
                rank[(e, n)] = i + 1
        n_epochs = {e: (len(needed[e]) + EPOCH - 1) // EPOCH for e in needed}
        self.stats = dict(n_ops=len(ops), n_eng=dict(n_eng),
                          n_sig={e: len(needed[e]) for e in needed}, n_dch=len(self.dch_order))
        with contextlib.ExitStack() as st:
            esem = {}
            for e in needed:
                for ep in range(max(1, n_epochs[e])):
                    esem[(e, ep)] = st.enter_context(nc.semaphore(f"s_{e}{ep}"))
            dsem = {}
            for i, d in enumerate(self.dch_order):
                dsem[d] = st.enter_context(nc.semaphore(f"d{i}"))
            block = st.enter_context(nc.Block())

            def tok_wait(engh, t):
                if t[0] == "e":
                    r = rank[(t[1], t[2])]
                    ep = (r - 1) // EPOCH
                    for q in range(ep):
                        engh.wait_ge(esem[(t[1], q)], EPOCH)
                    engh.wait_ge(esem[(t[1], ep)], r - ep * EPOCH)
                else:
                    engh.wait_ge(dsem[t[1]], 16 * t[2])

            per_eng = {e: [] for e in ENGS}
            for i, o in enumerate(ops):
                if o[0] != "BAR":
                    per_eng[o[0]].append(i)

            def run(ename, engh):
                for i in per_eng[ename]:
                    (eng, fn, reads, writes, dch) = ops[i]
                    for t in waits[i]:
                        tok_wait(engh, t)
                    ins = fn(engh)
                    tok = tokens[i]
                    if tok[0] == "d":
                        ins.then_inc(dsem[tok[1]], 16)
                    elif (tok[1], tok[2]) in rank:
                        r = rank[(tok[1], tok[2])]
                        ep = (r - 1) // EPOCH
                        ins.then_inc(esem[(tok[1], ep)], 1)
                if ename == "sp":
                    for t in fin.values():
                        tok_wait(engh, t)

            @block.sync
            def _(e):
                run("sp", e)

            @block.tensor
            def _(e):
                run("pe", e)

            @block.vector
            def _(e):
                run("dve", e)

            @block.scalar
            def _(e):
                run("act", e)

            @block.gpsimd
            def _(e):
                run("pool", e)


D = 1024
NT = 20
T = 2560
SEQS = [(0, 16, 1, True), (16, 2, 0, False), (18, 2, 0, False)]
DN_ALPHA = 8.0 ** 0.25
ARENA = 53000


class Arena:
    def __init__(self, nc):
        self.t = nc.alloc_sbuf_tensor("arena", [128, ARENA], F32)
        self.off = 0

    def mark(self):
        return self.off

    def reset(self, m):
        self.off = m

    def a(self, n, dt=F32, pat=None, **dims):
        sz = 4 if dt in (F32, I32) else 2
        nf = (n * sz + 3) // 4
        nf = (nf + 7) // 8 * 8
        assert self.off + nf <= ARENA, f"arena overflow {self.off}+{nf}"
        v = self.t[:, self.off:self.off + nf]
        self.off += nf
        if dt != F32:
            v = v.bitcast(dt)
        v = v[:, 0:n]
        if pat is not None:
            v = v.rearrange(pat, **dims)
        return v


class PS:
    def __init__(self, nc):
        self.t = nc.alloc_psum_tensor("psum", [128, 8, 512], F32)
        self.i = 0

    def one(self):
        b = self.i % 8
        self.i += 1
        return self.t[:, b, :], ("ps", b)

    def two(self):
        if self.i % 2:
            self.i += 1
        b = self.i % 8
        self.i += 2
        return self.t[:, b:b + 2, :], [("ps", b), ("ps", b + 1)]


def build(nc, n_sub=8, skip_moe=False):
    P = Prog(nc)
    A = Arena(nc)
    ps = PS(nc)

    def din(name, shape):
        return nc.dram_tensor(name, list(shape), F32, kind="ExternalInput").ap()

    def dout(name, shape):
        return nc.dram_tensor(name, list(shape), F32, kind="ExternalOutput").ap()

    x0 = din("x0", [T, D])
    condT_d = din("condT", [128, 16])
    w_mod = din("w_mod", [4, D, 6 * D])
    b_mod = din("b_mod", [4, 6 * D])
    b_modT = din("b_modT", [4, 128, 48])
    ln_g = din("ln_g", [4, 2, D])
    ln_b = din("ln_b", [4, 2, D])
    ret_w_in = din("ret_w_in", [2, D, 6 * D])
    ret_w_out = din("ret_w_out", [2, 2 * D, D])
    ret_decay = din("ret_decay", [16])
    state_ret = din("state_ret", [2, 2, 4, 256, 512])
    rot_cos = din("rot_cos", [128, 2048])
    rot_sin = din("rot_sin", [128, 2048])
    gla_w_in = din("gla_w_in", [D, 3 * D])
    gla_w_a1 = din("gla_w_a1", [2, D, 16])
    gla_w_a2a = din("gla_w_a2a", [2, 17, 512])
    gla_w_out = din("gla_w_out", [D, D])
    state_gla = din("state_gla", [2, 4, 128, 256])
    s5_arow = din("s5_arow", [2, 3, 4096])
    s5_acol = din("s5_acol", [2, 128, 96])
    s5_bblk = din("s5_bblk", [2, 2, 128, 32, 128])
    s5_cblk = din("s5_cblk", [2, 2, 128, 32, 128])
    s5_dT = din("s5_dT", [128, 8])
    s5_w_glu = din("s5_w_glu", [D, 2 * D])
    s5_x0 = din("s5_x0", [2, 2, 128, 32])
    nml = 0 if skip_moe else max(1, n_sub // 2)
    if nml:
        moe_w_router = din("moe_w_router", [nml, D, 32])
        moe_b_router = din("moe_b_router", [nml, 32])
        moe_w_gu = din("moe_w_gu", [nml, 32, D, 2 * D])
        moe_b_guT = din("moe_b_guT", [nml, 128, 512])
        moe_w_down = din("moe_w_down", [nml, 32, D, D])
        moe_b_down = din("moe_b_down", [nml, 32, D])

    xout = dout("xout", [T, D])
    o_ret = dout("o_ret", [2, 2, 2, 4, 256, 512])
    o_gla = dout("o_gla", [2, 2, 4, 128, 256])
    o_s5 = dout("o_s5", [2, 2, 2, 128, 32])
    xs_d = nc.dram_tensor("xs_scr", [T, D], F32, kind="Internal").ap()
    Y_d = nc.dram_tensor("y_scr", [T, D], F32, kind="Internal").ap()
    final_keys = []

    identf = A.a(128)
    identb = A.a(128 * 128 // 128, BF16) if False else A.a(128, BF16)
    hT = A.a(8 * T, BF16, "p (c t) -> p c t", c=8)
    ring = [A.a(8192, BF16) for _ in range(4)]
    grow = A.a(4 * D, BF16, "p (j g d) -> p j g d", j=2, g=2)
    lng = A.a(D)
    lnb = A.a(D)
    modT = A.a(64, F32, "p (j v c) -> p j v c", j=2, v=4)
    scb = A.a(16, BF16, "p (c j) -> p c j", c=8)
    condT = A.a(16, F32, "p (j c) -> p j c", j=2)
    bmT = A.a(48)
    onesf = A.a(128)
    zerob = A.a(8)
    st6 = A.a(12, F32, "p (a b) -> p a b", a=2)
    mv = A.a(8)
    MARK = A.mark()

    dq = [0]

    def dkey(prefix="q"):
        dq[0] += 1
        return (prefix, dq[0])

    ring_i = [0]

    def unit():
        i = ring_i[0] % 4
        ring_i[0] += 1
        return ring[i], ("ring", i)

    def cast_load(dst, src, key, ch):
        P.dma("pool", lambda e: e.dma_start(out=dst, in_=src), ch, writes=[key])

    def kcview(ap2d):
        return ap2d.rearrange("(kc p) n -> p kc n", p=128)

    P.pool(lambda e: e.memset(identf, 0.0), writes=["identf"])
    P.pool(lambda e: e.affine_select(out=identf, in_=identf, pattern=[[-1, 128]], compare_op=ALU.not_equal,
                                     fill=1.0, base=0, channel_multiplier=1), reads=["identf"], writes=["identf"])
    P.dve(lambda e: e.tensor_copy(identb, identf), reads=["identf"], writes=["identb"])
    P.dve(lambda e: e.memset(onesf, 1.0), writes=["onesf"])
    P.dma("sp", lambda e: e.dma_start(out=condT.rearrange("p j c -> p (j c)"), in_=condT_d), "cond", writes=["condT"])
    P.act(lambda e: e.activation(out=scb.rearrange("p c j -> p j c"), in_=condT, func=AF.Silu), reads=["condT"], writes=["scb"])
    P.barrier()

    def ln_tile(xt, yt, z, j, gi, dst_ap, dst_key, tag):
        P.dve(lambda e: e.tensor_tensor(out=yt, in0=yt, in1=grow[:, j, gi, :], op=ALU.mult), reads=[tag + "yt"], writes=[tag + "yt"])
        P.dve(lambda e: e.scalar_tensor_tensor(out=z, in0=xt, scalar=DN_ALPHA, in1=yt, op0=ALU.mult, op1=ALU.add),
              reads=[tag + "xt", tag + "yt"], writes=[tag + "z"])
        for q in range(2):
            P.dve(lambda e, q=q: e.bn_stats(out=st6[:, q, :], in_=z[:, q * 512:(q + 1) * 512]), reads=[tag + "z"], writes=["st6"])
        P.dve(lambda e: e.bn_aggr(out=mv[:, 0:2], in_=st6), reads=["st6"], writes=["mv"])
        P.dve(lambda e: e.tensor_scalar_add(out=mv[:, 2:3], in0=mv[:, 1:2], scalar1=1e-5), reads=["mv"], writes=["mv"])
        P.act(lambda e: e.sqrt(out=mv[:, 2:3], in_=mv[:, 2:3]), reads=["mv"], writes=["mv"])
        P.dve(lambda e: e.reciprocal(out=mv[:, 2:3], in_=mv[:, 2:3]), reads=["mv"], writes=["mv"])
        P.dve(lambda e: e.tensor_scalar(out=z, in0=z, scalar1=mv[:, 0:1], scalar2=mv[:, 2:3], op0=ALU.subtract, op1=ALU.mult),
              reads=[tag + "z", "mv"], writes=[tag + "z"])
        P.dve(lambda e: e.tensor_tensor(out=z, in0=z, in1=lng, op=ALU.mult), reads=[tag + "z", "lng"], writes=[tag + "z"])
        P.dve(lambda e: e.tensor_tensor(out=xt, in0=z, in1=lnb, op=ALU.add), reads=[tag + "z", tag + "xt", "lnb"], writes=[tag + "xt"])
        P.dma("sp", lambda e: e.dma_start(out=dst_ap, in_=xt), (tag + "st",), reads=[tag + "xt"], writes=[dst_key])

    def load_ln(l, i):
        P.dma("sp", lambda e: e.dma_start(out=lng, in_=ln_g[l, i, :].partition_broadcast(128)), "lng", writes=["lng"])
        P.dma("sp", lambda e: e.dma_start(out=lnb, in_=ln_b[l, i, :].partition_broadcast(128)), "lnb", writes=["lnb"])

    def stage_mod(l):
        A.reset(MARK)
        scB = A.a(2 * 8 * 128, BF16, "p (j c n) -> p j c n", j=2, c=8)
        biasrow = A.a(D, BF16)
        e0 = A.a(128, BF16)
        P.dve(lambda e: e.memset(biasrow, 0.0), writes=["biasrow"])
        P.dve(lambda e: e.memset(e0, 0.0), writes=["e0"])
        P.dve(lambda e: e.memset(e0[0:1, :], 1.0), reads=["e0"], writes=["e0"])
        for j in range(2):
            P.dve(lambda e, j=j: e.tensor_copy(scB[:, j, :, :], scb[:, :, j:j + 1].to_broadcast([128, 8, 128])), writes=["scB"])
        P.dma("sp", lambda e: e.dma_start(out=bmT, in_=b_modT[l]), "bmT", writes=["bmT"])
        for v in range(6):
            U, uk = unit()
            Uv = U.rearrange("p (c n) -> p c n", c=8)
            cast_load(Uv, kcview(w_mod[l][:, v * D:(v + 1) * D]), uk, ("ringd", uk[1]))
            if v in (0, 1, 3, 4):
                vi = {0: 0, 1: 1, 3: 2, 4: 3}[v]
                bank, bk = ps.one()
                for fc in range(8):
                    for kc in range(8):
                        P.pe(lambda e, fc=fc, kc=kc, bank=bank, Uv=Uv: e.matmul(bank[:, fc * 2:fc * 2 + 2], lhsT=Uv[:, kc, fc * 128:(fc + 1) * 128],
                                                                               rhs=scb[:, kc, :], start=(kc == 0), stop=(kc == 7)),
                             reads=[uk], writes=[bk])
                bv = bank[:, 0:16].rearrange("p (c j) -> p c j", j=2)
                for j in range(2):
                    P.dve(lambda e, j=j, bv=bv, vi=vi, v=v: e.scalar_tensor_tensor(out=modT[:, j, vi, :], in0=bv[:, :, j],
                                                                                  scalar=(1.0 if vi in (1, 3) else 0.0),
                                                                                  in1=bmT[:, v * 8:(v + 1) * 8], op0=ALU.add, op1=ALU.add),
                          reads=[bk, "bmT"], writes=["modT"])
            else:
                gi = 0 if v == 2 else 1
                P.dma("pool", lambda e, v=v: e.dma_start(out=biasrow[0:1, :], in_=b_mod[l:l + 1, v * D:(v + 1) * D]), "biasrow",
                      reads=["biasrow"], writes=["biasrow"])
                for j in range(2):
                    for half in range(2):
                        bank, bk = ps.one()
                        for kc in range(8):
                            P.pe(lambda e, j=j, kc=kc, half=half, bank=bank, Uv=Uv: e.matmul(bank, lhsT=scB[:, j, kc, :], rhs=Uv[:, kc, half * 512:(half + 1) * 512],
                                                                                            start=(kc == 0), stop=False),
                                 reads=[uk, "scB"], writes=[bk])
                        P.pe(lambda e, half=half, bank=bank: e.matmul(bank, lhsT=e0, rhs=biasrow[:, half * 512:(half + 1) * 512], start=False, stop=True),
                             reads=["e0", "biasrow"], writes=[bk])
                        P.act(lambda e, j=j, gi=gi, half=half, bank=bank: e.copy(out=grow[:, j, gi, half * 512:(half + 1) * 512], in_=bank),
                              reads=[bk], writes=["grow"])
        P.barrier()

    def stage_h(src, vsh, vsc, moe_l=None, G=None):
        m0 = A.mark()
        xts = [A.a(D) for _ in range(3)]
        if moe_l is not None:
            hTf = [A.a(8 * 128, F32, "p (c t) -> p c t", c=8) for _ in range(2)]
            wr = A.a(8 * 32, F32, "p (c n) -> p c n", c=8)
            br = A.a(32)
            lg = A.a(32)
            top8 = A.a(8)
            msk = A.a(32)
            ex = A.a(32)
            sm = A.a(8)
            e0f = A.a(128)
            P.dma("sp", lambda e: e.dma_start(out=wr, in_=kcview(moe_w_router[moe_l])), "wr", writes=["wr"])
            P.dve(lambda e: e.memset(br, 0.0), writes=["br"])
            P.dve(lambda e: e.memset(e0f, 0.0), writes=["e0f"])
            P.dve(lambda e: e.memset(e0f[0:1, :], 1.0), reads=["e0f"], writes=["e0f"])
            P.dma("sp", lambda e: e.dma_start(out=br[0:1, :], in_=moe_b_router[moe_l:moe_l + 1, :]), "br", reads=["br"], writes=["br"])
        for tt in range(NT):
            j = 1 if tt < 16 else 0
            xt = xts[tt % 3]
            xk = ("hxt", tt % 3)
            P.dma("sp", lambda e, xt=xt, tt=tt: e.dma_start(out=xt, in_=src[tt * 128:(tt + 1) * 128, :]), ("hx", tt % 3), writes=[xk])
            for half in range(2):
                bank, bk = ps.one()
                for q in range(4):
                    fc = half * 4 + q
                    P.pe(lambda e, bank=bank, q=q, fc=fc, xt=xt: e.transpose(bank[:, q * 128:(q + 1) * 128], xt[:, fc * 128:(fc + 1) * 128], identf),
                         reads=[xk], writes=[bk])
                for q in range(4):
                    fc = half * 4 + q
                    dst = hT[:, fc, tt * 128:(tt + 1) * 128]
                    srcp = bank[:, q * 128:(q + 1) * 128]
                    if moe_l is not None:
                        hf = hTf[tt % 2]
                        P.dve(lambda e: e.tensor_scalar(out=hf[:, fc, :], in0=srcp, scalar1=modT[:, j, vsc, fc:fc + 1],
                                                        scalar2=modT[:, j, vsh, fc:fc + 1], op0=ALU.mult, op1=ALU.add),
                              reads=[bk], writes=[("hTf", tt % 2, fc)])
                        P.act(lambda e: e.copy(out=dst, in_=hf[:, fc, :]), reads=[("hTf", tt % 2, fc)], writes=[("hT", tt)])
                    elif half == 0:
                        P.act(lambda e: e.activation(out=dst, in_=srcp, func=AF.Identity,
                                                     scale=modT[:, j, vsc, fc:fc + 1], bias=modT[:, j, vsh, fc:fc + 1]),
                              reads=[bk], writes=[("hT", tt)])
                    else:
                        P.dve(lambda e: e.tensor_scalar(out=dst, in0=srcp, scalar1=modT[:, j, vsc, fc:fc + 1],
                                                        scalar2=modT[:, j, vsh, fc:fc + 1], op0=ALU.mult, op1=ALU.add),
                              reads=[bk], writes=[("hT", tt)])
            if moe_l is not None:
                hf = hTf[tt % 2]
                hk = ("hTf", tt % 2)
                bank, bk = ps.one()
                for kc in range(8):
                    P.pe(lambda e, kc=kc, hf=hf, bank=bank: e.matmul(bank[:, 0:32], lhsT=hf[:, kc, :], rhs=wr[:, kc, :], start=(kc == 0), stop=False),
                         reads=[("hTf", tt % 2, kc), "wr"], writes=[bk])
                P.pe(lambda e, bank=bank: e.matmul(bank[:, 0:32], lhsT=e0f, rhs=br, start=False, stop=True),
                     reads=["br", "e0f"], writes=[bk])
                P.act(lambda e, bank=bank: e.copy(out=lg, in_=bank[:, 0:32]), reads=[bk], writes=["lg"])
                P.dve(lambda e: e.max(out=top8, in_=lg), reads=["lg"], writes=["top8"])
                P.dve(lambda e: e.tensor_scalar(out=msk, in0=lg, scalar1=top8[:, 3:4], scalar2=None, op0=ALU.is_ge), reads=["lg", "top8"], writes=["msk"])
                P.dve(lambda e: e.tensor_scalar(out=sm[:, 0:1], in0=top8[:, 0:1], scalar1=-1.0, scalar2=None, op0=ALU.mult), reads=["top8"], writes=["sm"])
                P.act(lambda e: e.activation(out=ex, in_=lg, func=AF.Exp, bias=sm[:, 0:1], scale=1.0), reads=["lg", "sm"], writes=["ex"])
                P.dve(lambda e: e.tensor_tensor(out=ex, in0=ex, in1=msk, op=ALU.mult), reads=["ex", "msk"], writes=["ex"])
                P.dve(lambda e: e.reduce_sum(out=sm[:, 1:2], in_=ex, axis=AX.X), reads=["ex"], writes=["sm2"])
                P.dve(lambda e: e.reciprocal(out=sm[:, 2:3], in_=sm[:, 1:2]), reads=["sm2"], writes=["sm3"])
                P.dve(lambda e, tt=tt: e.tensor_scalar(out=G[:, tt, :], in0=ex, scalar1=sm[:, 2:3], scalar2=None, op0=ALU.mult),
                      reads=["ex", "sm3"], writes=[("G", tt)])
        P.barrier()
        A.reset(m0)

    def stage_epi(l, i, src, dst, dst_name):
        m0 = A.mark()
        xts = [A.a(D) for _ in range(2)]
        yts = [A.a(D) for _ in range(2)]
        zs = [A.a(D) for _ in range(2)]
        load_ln(l, i)
        for tt in range(NT):
            j = 1 if tt < 16 else 0
            b = tt % 2
            tag = f"e{b}"
            P.dma("sp", lambda e, tt=tt, b=b: e.dma_start(out=xts[b], in_=src[tt * 128:(tt + 1) * 128, :]), (tag + "lx",), writes=[tag + "xt"])
            P.dma("sp", lambda e, tt=tt, b=b: e.dma_start(out=yts[b], in_=Y_d[tt * 128:(tt + 1) * 128, :]), (tag + "ly",), reads=[("Y", tt)], writes=[tag + "yt"])
            ln_tile(xts[b], yts[b], zs[b], j, i, dst[tt * 128:(tt + 1) * 128, :], (dst_name, tt), tag)
        P.barrier()
        A.reset(m0)

    def stage_moe(l, src, dst, dst_name):
        A.reset(MARK)
        G = A.a(NT * 32, F32, "p (t n) -> p t n", t=NT)
        stage_h(src, 2, 3, moe_l=l, G=G)
        acc = A.a(10 * D, F32, "p (t d) -> p t d", t=10)
        actT = [A.a(8 * 512, BF16, "p (c t) -> p c t", c=8) for _ in range(2)]
        gc = A.a(512)
        sg = A.a(512)
        l1 = A.a(512)
        xt = A.a(D)
        z = A.a(D)
        bgu = A.a(512, F32, "p (e c) -> p e c", e=32)
        e0 = A.a(128, BF16)
        brows = [A.a(D, BF16) for _ in range(2)]
        P.dve(lambda e: e.memset(e0, 0.0), writes=["e0"])
        P.dve(lambda e: e.memset(e0[0:1, :], 1.0), reads=["e0"], writes=["e0"])
        for q in range(2):
            P.dve(lambda e: e.memset(brows[q], 0.0), writes=[("brow", q)])
        load_ln(l, 1)
        P.dma("sp", lambda e: e.dma_start(out=bgu.rearrange("p e c -> p (e c)"), in_=moe_b_guT[l]), "bgu", writes=["bgu"])
        P.dve(lambda e: e.tensor_scalar_add(out=bgu[:, :, 8:16], in0=bgu[:, :, 8:16], scalar1=1.0), reads=["bgu"], writes=["bgu"])
        for p_ in range(2):
            tok0 = p_ * 1280
            for ti in range(10):
                P.dve(lambda e: e.memset(acc[:, ti, :], 0.0), writes=[("acc", ti)])
            for ex_ in range(int(os.environ.get('MOE_NEXP', '32'))):
                Ug, kg = unit()
                Ul, kl = unit()
                Ud, kd = unit()
                Ug = Ug.rearrange("p (c n) -> p c n", c=8)
                Ul = Ul.rearrange("p (c n) -> p c n", c=8)
                Ud = Ud.rearrange("p (c n) -> p c n", c=8)
                cast_load(Ug, kcview(moe_w_gu[l, ex_][:, 0:D]), kg, ("ringd", kg[1]))
                cast_load(Ul, kcview(moe_w_gu[l, ex_][:, D:2 * D]), kl, ("ringd", kl[1]))
                cast_load(Ud, kcview(moe_w_down[l, ex_]), kd, ("ringd", kd[1]))
                brw = brows[ex_ % 2]
                bwk = ("brow", ex_ % 2)
                P.dma("pool", lambda e: e.dma_start(out=brw[0:1, :], in_=moe_b_down[l, ex_:ex_ + 1, :]), ("browd", ex_ % 2), reads=[bwk], writes=[bwk])
                for nt, (c0, n) in enumerate(((0, 512), (512, 512), (1024, 256))):
                    ab = actT[nt % 2]
                    ak = ("actT", nt % 2)
                    for fc in range(8):
                        bg, kbg = ps.one()
                        bl, kbl = ps.one()
                        for kc in range(8):
                            P.pe(lambda e, kc=kc, fc=fc, bg=bg, Ug=Ug, c0=c0, n=n: e.matmul(bg[:, 0:n], lhsT=Ug[:, kc, fc * 128:(fc + 1) * 128],
                                                                                          rhs=hT[:, kc, tok0 + c0:tok0 + c0 + n], start=(kc == 0), stop=(kc == 7)),
                                 reads=[kg], writes=[kbg])
                        for kc in range(8):
                            P.pe(lambda e, kc=kc, fc=fc, bl=bl, Ul=Ul, c0=c0, n=n: e.matmul(bl[:, 0:n], lhsT=Ul[:, kc, fc * 128:(fc + 1) * 128],
                                                                                          rhs=hT[:, kc, tok0 + c0:tok0 + c0 + n], start=(kc == 0), stop=(kc == 7)),
                                 reads=[kl], writes=[kbl])
                        P.dve(lambda e, bg=bg, fc=fc, n=n, ex_=ex_: e.tensor_scalar(out=gc[:, 0:n], in0=bg[:, 0:n], scalar1=bgu[:, ex_, fc:fc + 1], scalar2=7.0,
                                                                                   op0=ALU.add, op1=ALU.min), reads=[kbg, "bgu"], writes=["gc"])
                        P.act(lambda e, n=n: e.activation(out=sg[:, 0:n], in_=gc[:, 0:n], func=AF.Sigmoid, scale=1.702), reads=["gc"], writes=["sg"])
                        P.dve(lambda e, bl=bl, fc=fc, n=n, ex_=ex_: e.tensor_scalar(out=l1[:, 0:n], in0=bl[:, 0:n], scalar1=bgu[:, ex_, 8 + fc:9 + fc], scalar2=8.0,
                                                                                   op0=ALU.add, op1=ALU.min), reads=[kbl, "bgu"], writes=["l1"])
                        P.dve(lambda e, n=n: e.scalar_tensor_tensor(out=l1[:, 0:n], in0=l1[:, 0:n], scalar=-6.0, in1=gc[:, 0:n], op0=ALU.max, op1=ALU.mult),
                              reads=["l1", "gc"], writes=["l1"])
                        P.dve(lambda e, n=n, fc=fc, ab=ab: e.tensor_tensor(out=ab[:, fc, 0:n], in0=l1[:, 0:n], in1=sg[:, 0:n], op=ALU.mult),
                              reads=["l1", "sg"], writes=[ak])
                    for tl in range(n // 128):
                        ti = c0 // 128 + tl
                        tile = p_ * 10 + ti
                        for half in range(2):
                            by, kby = ps.one()
                            for fc in range(8):
                                P.pe(lambda e, fc=fc, by=by, ab=ab, tl=tl, Ud=Ud, half=half: e.matmul(by, lhsT=ab[:, fc, tl * 128:(tl + 1) * 128],
                                                                                                     rhs=Ud[:, fc, half * 512:(half + 1) * 512], start=(fc == 0), stop=False),
                                     reads=[ak, kd], writes=[kby])
                            P.pe(lambda e: e.matmul(by, lhsT=e0, rhs=brw[:, half * 512:(half + 1) * 512], start=False, stop=True), reads=["e0", bwk], writes=[kby])
                            P.dve(lambda e, by=by, ti=ti, tile=tile, half=half, ex_=ex_: e.scalar_tensor_tensor(
                                out=acc[:, ti, half * 512:(half + 1) * 512], in0=by, scalar=G[:, tile, ex_:ex_ + 1],
                                in1=acc[:, ti, half * 512:(half + 1) * 512], op0=ALU.mult, op1=ALU.add), reads=[kby, ("acc", ti)], writes=[("acc", ti)])
            for ti in range(10):
                tile = p_ * 10 + ti
                j = 1 if tile < 16 else 0
                P.dma("sp", lambda e, tile=tile: e.dma_start(out=xt, in_=src[tile * 128:(tile + 1) * 128, :]), ("mlx",), writes=["mxt"])
                P.dve(lambda e, ti=ti: e.tensor_copy(acc[:, ti, 0:1], acc[:, ti, 0:1]), reads=[("acc", ti)], writes=["myt"])
                ln_tile(xt, acc[:, ti, :], z, j, 1, dst[tile * 128:(tile + 1) * 128, :], (dst_name, tile), "m")
        P.barrier()

    def stage_ret(l, ri):
        A.reset(MARK)
        dec = A.a(16)
        lgam = A.a(16)
        reli = A.a(128, I32)
        REL = A.a(128)
        DPOS = A.a(128)
        DNEG = A.a(128)
        MF = A.a(128)
        MB = A.a(128)
        J1 = A.a(128)
        JB = A.a(128)
        pci = A.a(8, I32)
        PC = A.a(8)
        Dm = A.a(128)
        DBt = A.a(128)
        XiF = A.a(128)
        XiB = A.a(128)
        zc = A.a(8)
        rcos = A.a(2048, BF16)
        rsin = A.a(2048, BF16)
        qT = A.a(2 * 2048, BF16, "p (c t) -> p c t", c=2)
        kT = A.a(2 * 2048, BF16, "p (c t) -> p c t", c=2)
        vv = A.a(16 * 512, BF16, "p (c n) -> p c n", c=16)
        Sf = A.a(1024, F32, "p (c n) -> p c n", c=2)
        Sb = A.a(1024, F32, "p (c n) -> p c n", c=2)
        Sfb = A.a(1024, BF16, "p (c n) -> p c n", c=2)
        x1 = A.a(512)
        x2 = A.a(512)
        ta = A.a(512)
        tb = A.a(512)
        kz = A.a(256, BF16)
        qf = A.a(256, BF16, "p (c n) -> p c n", c=2)
        qb = A.a(256, BF16, "p (c n) -> p c n", c=2)
        PT = A.a(128, BF16)
        sgt = A.a(512)
        on = A.a(512)
        og = A.a(512, BF16)
        ogT = A.a(512, BF16, "p (c n) -> p c n", c=4)
        yp = [A.a(D) for _ in range(2)]
        SbB = A.t[:, 0:1]
        SbB = ring[2]
        SbB0 = ring[2].rearrange("p (c d n) -> p c d n", c=8, d=2)
        SbB1 = ring[3].rearrange("p (c d n) -> p c d n", c=8, d=2)

        def sbb(c):
            return (SbB0 if c < 8 else SbB1)[:, c % 8, :, :]

        P.dma("sp", lambda e: e.dma_start(out=dec, in_=ret_decay.partition_broadcast(128)), "dec", writes=["dec"])
        P.act(lambda e: e.activation(out=lgam, in_=dec, func=AF.Exp, scale=-1.0), reads=["dec"], writes=["lgam"])
        P.act(lambda e: e.activation(out=lgam, in_=lgam, func=AF.Ln, bias=onesf[:, 0:1], scale=1.0), reads=["lgam"], writes=["lgam"])
        P.dve(lambda e: e.tensor_scalar(out=lgam, in0=lgam, scalar1=-1.0, scalar2=None, op0=ALU.mult), reads=["lgam"], writes=["lgam"])
        P.pool(lambda e: e.iota(reli, pattern=[[1, 128]], base=0, channel_multiplier=-1), writes=["reli"])
        P.dve(lambda e: e.tensor_copy(REL, reli), reads=["reli"], writes=["REL"])
        P.dve(lambda e: e.tensor_scalar(out=DPOS, in0=REL, scalar1=0.0, scalar2=None, op0=ALU.max), reads=["REL"], writes=["DPOS"])
        P.dve(lambda e: e.tensor_scalar(out=DNEG, in0=REL, scalar1=-1.0, scalar2=0.0, op0=ALU.mult, op1=ALU.max), reads=["REL"], writes=["DNEG"])
        P.dve(lambda e: e.tensor_scalar(out=MF, in0=REL, scalar1=0.0, scalar2=None, op0=ALU.is_ge), reads=["REL"], writes=["MF"])
        P.dve(lambda e: e.tensor_scalar(out=MB, in0=REL, scalar1=0.0, scalar2=None, op0=ALU.is_le), reads=["REL"], writes=["MB"])
        P.pool(lambda e: e.iota(reli, pattern=[[1, 128]], base=1, channel_multiplier=0), reads=["REL"], writes=["reli"])
        P.dve(lambda e: e.tensor_copy(J1, reli), reads=["reli"], writes=["J1"])
        P.dve(lambda e: e.tensor_scalar(out=JB, in0=J1, scalar1=-1.0, scalar2=129.0, op0=ALU.mult, op1=ALU.add), reads=["J1"], writes=["JB"])
        P.pool(lambda e: e.iota(pci[:, 0:1], pattern=[[0, 1]], base=0, channel_multiplier=1), writes=["pci"])
        P.dve(lambda e: e.tensor_copy(PC[:, 0:1], pci[:, 0:1]), reads=["pci"], writes=["PC"])
        P.dve(lambda e: e.tensor_scalar(out=PC[:, 1:2], in0=PC[:, 0:1], scalar1=-1.0, scalar2=127.0, op0=ALU.mult, op1=ALU.add), reads=["PC"], writes=["PC"])
        P.dve(lambda e: e.memset(PC[:, 2:3], 128.0), reads=["PC"], writes=["PC"])
        P.dma("pool", lambda e: e.dma_start(out=rcos, in_=rot_cos), "rcos", writes=["rcos"])
        P.dma("pool", lambda e: e.dma_start(out=rsin, in_=rot_sin), "rsin", writes=["rsin"])

        for h in range(4):
            UA, ka = ring[0], ("ring", 0)
            UB, kb = ring[1], ("ring", 1)
            UAv = UA.rearrange("p (c n) -> p c n", c=8)
            UBg = UB[:, 0:4096].rearrange("p (c n) -> p c n", c=8)
            UBo = UB[:, 4096:8192].rearrange("p (c n) -> p c n", c=4)
            wi = ret_w_in[ri]
            cast_load(UAv[:, :, 0:256], kcview(wi[:, h * 256:(h + 1) * 256]), ka, ("ringd", 0))
            cast_load(UAv[:, :, 256:512], kcview(wi[:, D + h * 256:D + (h + 1) * 256]), ka, ("ringd", 0))
            cast_load(UAv[:, :, 512:1024], kcview(wi[:, 2 * D + h * 512:2 * D + (h + 1) * 512]), ka, ("ringd", 0))
            cast_load(UBg, kcview(wi[:, 4 * D + h * 512:4 * D + (h + 1) * 512]), kb, ("ringd", 1))
            cast_load(UBo, kcview(ret_w_out[ri][h * 512:(h + 1) * 512, :]), kb, ("ringd", 1))
            lf = lgam[:, ri * 8 + h:ri * 8 + h + 1]
            lb = lgam[:, ri * 8 + 4 + h:ri * 8 + 4 + h + 1]
            P.act(lambda e, lf=lf: e.activation(out=Dm, in_=DPOS, func=AF.Exp, scale=lf), reads=["DPOS", "lgam"], writes=["Dm"])
            P.dve(lambda e: e.tensor_tensor(out=Dm, in0=Dm, in1=MF, op=ALU.mult), reads=["Dm", "MF"], writes=["Dm"])
            P.act(lambda e, lb=lb: e.activation(out=DBt, in_=DNEG, func=AF.Exp, scale=lb), reads=["DNEG", "lgam"], writes=["DBt"])
            P.dve(lambda e: e.tensor_tensor(out=DBt, in0=DBt, in1=MB, op=ALU.mult), reads=["DBt", "MB"], writes=["DBt"])
            P.dve(lambda e: e.tensor_tensor(out=Dm, in0=Dm, in1=DBt, op=ALU.add), reads=["Dm", "DBt"], writes=["Dm"])
            P.act(lambda e, lf=lf: e.activation(out=XiF, in_=J1, func=AF.Exp, scale=lf), reads=["J1", "lgam"], writes=["XiF"])
            P.act(lambda e, lb=lb: e.activation(out=XiB, in_=JB, func=AF.Exp, scale=lb), reads=["JB", "lgam"], writes=["XiB"])
            P.act(lambda e, lf=lf: e.activation(out=zc[:, 0:1], in_=PC[:, 1:2], func=AF.Exp, scale=lf), reads=["PC", "lgam"], writes=["zc"])
            P.act(lambda e, lb=lb: e.activation(out=zc[:, 1:2], in_=PC[:, 0:1], func=AF.Exp, scale=lb), reads=["PC", "lgam"], writes=["zc"])
            P.act(lambda e, lf=lf: e.activation(out=zc[:, 2:3], in_=PC[:, 2:3], func=AF.Exp, scale=lf), reads=["PC", "lgam"], writes=["zc"])
            P.act(lambda e, lb=lb: e.activation(out=zc[:, 3:4], in_=PC[:, 2:3], func=AF.Exp, scale=lb), reads=["PC", "lgam"], writes=["zc"])
            for si, (t0, nch, j, samp) in enumerate(SEQS):
                L = nch * 128
                tok0 = t0 * 128
                for c0 in range(0, L, 512):
                    n = min(512, L - c0)
                    for (dstT, colb, scl, nm) in ((qT, 0, 1.0 / 16.0, "qT"), (kT, 256, 1.0, "kT")):
                        banks = []
                        for dc in range(2):
                            bank, bk = ps.one()
                            banks.append((bank, bk))
                            for kc in range(8):
                                P.pe(lambda e, kc=kc, dc=dc, bank=bank, colb=colb, c0=c0, n=n: e.matmul(
                                    bank[:, 0:n], lhsT=UAv[:, kc, colb + dc * 128:colb + (dc + 1) * 128], rhs=hT[:, kc, tok0 + c0:tok0 + c0 + n],
                                    start=(kc == 0), stop=(kc == 7)), reads=[ka], writes=[bk])
                        if samp:
                            P.act(lambda e, n=n, scl=scl, b=banks[0][0]: e.activation(out=x1[:, 0:n], in_=b[:, 0:n], func=AF.Identity, scale=scl), reads=[banks[0][1]], writes=["x1"])
                            P.act(lambda e, n=n, scl=scl, b=banks[1][0]: e.activation(out=x2[:, 0:n], in_=b[:, 0:n], func=AF.Identity, scale=scl), reads=[banks[1][1]], writes=["x2"])
                            cs = rcos[:, c0:c0 + n]
                            sn = rsin[:, c0:c0 + n]
                            P.dve(lambda e, n=n, cs=cs: e.tensor_tensor(out=ta[:, 0:n], in0=x1[:, 0:n], in1=cs, op=ALU.mult), reads=["x1", "rcos"], writes=["ta"])
                            P.dve(lambda e, n=n, sn=sn: e.tensor_tensor(out=tb[:, 0:n], in0=x2[:, 0:n], in1=sn, op=ALU.mult), reads=["x2", "rsin"], writes=["tb"])
                            P.dve(lambda e, n=n, c0=c0, dstT=dstT: e.tensor_tensor(out=dstT[:, 0, c0:c0 + n], in0=ta[:, 0:n], in1=tb[:, 0:n], op=ALU.subtract),
                                  reads=["ta", "tb"], writes=[nm])
                            P.dve(lambda e, n=n, sn=sn: e.tensor_tensor(out=ta[:, 0:n], in0=x1[:, 0:n], in1=sn, op=ALU.mult), reads=["x1", "rsin"], writes=["ta"])
                            P.dve(lambda e, n=n, cs=cs: e.tensor_tensor(out=tb[:, 0:n], in0=x2[:, 0:n], in1=cs, op=ALU.mult), reads=["x2", "rcos"], writes=["tb"])
                            P.dve(lambda e, n=n, c0=c0, dstT=dstT: e.tensor_tensor(out=dstT[:, 1, c0:c0 + n], in0=ta[:, 0:n], in1=tb[:, 0:n], op=ALU.add),
                                  reads=["ta", "tb"], writes=[nm])
                        else:
                            for dc in range(2):
                                P.act(lambda e, n=n, scl=scl, b=banks[dc][0], dc=dc, c0=c0, dstT=dstT: e.activation(out=dstT[:, dc, c0:c0 + n], in_=b[:, 0:n], func=AF.Identity, scale=scl),
                                      reads=[banks[dc][1]], writes=[nm])
                for c in range(nch):
                    bank, bk = ps.one()
                    for kc in range(8):
                        P.pe(lambda e, kc=kc, c=c, bank=bank: e.matmul(bank, lhsT=hT[:, kc, tok0 + c * 128:tok0 + (c + 1) * 128], rhs=UAv[:, kc, 512:1024],
                                                                      start=(kc == 0), stop=(kc == 7)), reads=[ka], writes=[bk])
                    P.act(lambda e, c=c, bank=bank: e.copy(out=vv[:, c, :], in_=bank), reads=[bk], writes=[("vv", c)])
                for (S, d, nm) in ((Sf, 0, "Sf"), (Sb, 1, "Sb")):
                    if samp:
                        P.dma("sp", lambda e, S=S, d=d: e.dma_start(out=S, in_=state_ret[ri, d, h].rearrange("(c p) n -> p c n", p=128)), ("ld" + nm,), writes=[nm])
                    else:
                        P.dve(lambda e, S=S: e.memset(S, 0.0), writes=[nm])

                def ktok(c, zcol, nm):
                    bankT, bkT = ps.one()
                    bTb = bankT.bitcast(BF16)
                    for dc in range(2):
                        P.pe(lambda e, dc=dc, c=c, bTb=bTb: e.transpose(bTb[:, dc * 128:(dc + 1) * 128], kT[:, dc, c * 128:(c + 1) * 128], identb),
                             reads=["kT"], writes=[bkT])
                    P.dve(lambda e, bTb=bTb, zcol=zcol: e.tensor_scalar(out=kz, in0=bTb[:, 0:256], scalar1=zc[:, zcol:zcol + 1], scalar2=None, op0=ALU.mult),
                          reads=[bkT, "zc"], writes=["kz"])

                def supd(S, nm, c, gcol):
                    for dc in range(2):
                        bankA, bkA = ps.one()
                        P.pe(lambda e, dc=dc, c=c, bankA=bankA: e.matmul(bankA, lhsT=kz[:, dc * 128:(dc + 1) * 128], rhs=vv[:, c, :], start=True, stop=True),
                             reads=["kz", ("vv", c)], writes=[bkA])
                        P.dve(lambda e, dc=dc, bankA=bankA, S=S, gcol=gcol: e.scalar_tensor_tensor(out=S[:, dc, :], in0=S[:, dc, :], scalar=zc[:, gcol:gcol + 1],
                                                                                                 in1=bankA, op0=ALU.mult, op1=ALU.add), reads=[bkA, nm, "zc"], writes=[nm])

                for c in reversed(range(nch)):
                    P.act(lambda e, c=c: e.copy(out=sbb(c), in_=Sb), reads=["Sb"], writes=[("sbb", c)])
                    ktok(c, 1, "kzb")
                    supd(Sb, "Sb", c, 3)
                if not samp:
                    P.dma("sp", lambda e, si=si: e.dma_start(out=o_ret[si - 1, ri, 1, h].rearrange("(c p) n -> p c n", p=128), in_=Sb), "oret",
                          reads=["Sb"], writes=[("o_ret", si, ri, 1, h)])
                    final_keys.append(("o_ret", si, ri, 1, h))
                for c in range(nch):
                    tile = t0 + c
                    P.act(lambda e: e.copy(out=Sfb, in_=Sf), reads=["Sf"], writes=["Sfb"])
                    P.dve(lambda e, c=c: e.tensor_tensor(out=qf, in0=qT[:, :, c * 128:(c + 1) * 128], in1=XiF.unsqueeze(1).to_broadcast([128, 2, 128]), op=ALU.mult),
                          reads=["qT", "XiF"], writes=["qf"])
                    P.dve(lambda e, c=c: e.tensor_tensor(out=qb, in0=qT[:, :, c * 128:(c + 1) * 128], in1=XiB.unsqueeze(1).to_broadcast([128, 2, 128]), op=ALU.mult),
                          reads=["qT", "XiB"], writes=["qb"])
                    bankS, bkS = ps.one()
                    for dc in range(2):
                        P.pe(lambda e, dc=dc, c=c, bankS=bankS: e.matmul(bankS[:, 0:128], lhsT=kT[:, dc, c * 128:(c + 1) * 128], rhs=qT[:, dc, c * 128:(c + 1) * 128],
                                                                        start=(dc == 0), stop=(dc == 1)), reads=["kT", "qT"], writes=[bkS])
                    P.dve(lambda e, bankS=bankS: e.tensor_tensor(out=PT, in0=bankS[:, 0:128], in1=Dm, op=ALU.mult), reads=[bkS, "Dm"], writes=["PT"])
                    bankO, bkO = ps.one()
                    P.pe(lambda e, c=c, bankO=bankO: e.matmul(bankO, lhsT=PT, rhs=vv[:, c, :], start=True, stop=False), reads=["PT", ("vv", c)], writes=[bkO])
                    for dc in range(2):
                        P.pe(lambda e, dc=dc, bankO=bankO: e.matmul(bankO, lhsT=qf[:, dc, :], rhs=Sfb[:, dc, :], start=False, stop=False), reads=["qf", "Sfb"], writes=[bkO])
                    for dc in range(2):
                        P.pe(lambda e, dc=dc, c=c, bankO=bankO: e.matmul(bankO, lhsT=qb[:, dc, :], rhs=sbb(c)[:, dc, :], start=False, stop=(dc == 1)),
                             reads=["qb", ("sbb", c)], writes=[bkO])
                    bankG, bkG = ps.one()
                    for kc in range(8):
                        P.pe(lambda e, kc=kc, c=c, bankG=bankG: e.matmul(bankG, lhsT=hT[:, kc, tok0 + c * 128:tok0 + (c + 1) * 128], rhs=UBg[:, kc, :],
                                                                        start=(kc == 0), stop=(kc == 7)), reads=[kb], writes=[bkG])
                    P.act(lambda e, bankG=bankG: e.activation(out=sgt, in_=bankG, func=AF.Silu), reads=[bkG], writes=["sgt"])
                    P.dve(lambda e, bankO=bankO: e.bn_stats(out=st6[:, 0, :], in_=bankO), reads=[bkO], writes=["st6"])
                    P.dve(lambda e: e.bn_aggr(out=mv[:, 0:2], in_=st6[:, 0:1, :]), reads=["st6"], writes=["mv"])
                    P.dve(lambda e: e.tensor_scalar_add(out=mv[:, 2:3], in0=mv[:, 1:2], scalar1=1e-5), reads=["mv"], writes=["mv"])
                    P.act(lambda e: e.sqrt(out=mv[:, 2:3], in_=mv[:, 2:3]), reads=["mv"], writes=["mv"])
                    P.dve(lambda e: e.reciprocal(out=mv[:, 2:3], in_=mv[:, 2:3]), reads=["mv"], writes=["mv"])
                    P.dve(lambda e, bankO=bankO: e.tensor_scalar(out=on, in0=bankO, scalar1=mv[:, 0:1], scalar2=mv[:, 2:3], op0=ALU.subtract, op1=ALU.mult),
                          reads=[bkO, "mv"], writes=["on"])
                    P.dve(lambda e: e.tensor_tensor(out=og, in0=on, in1=sgt, op=ALU.mult), reads=["on", "sgt"], writes=["og"])
                    bankT, bkT = ps.one()
                    bTb = bankT.bitcast(BF16)
                    for q in range(4):
                        P.pe(lambda e, q=q, bTb=bTb: e.transpose(bTb[:, q * 128:(q + 1) * 128], og[:, q * 128:(q + 1) * 128], identb), reads=["og"], writes=[bkT])
                    P.act(lambda e, bTb=bTb: e.copy(out=ogT.rearrange("p c n -> p (c n)"), in_=bTb[:, 0:512]), reads=[bkT], writes=["ogT"])
                    b2, bk2 = ps.two()
                    for half in range(2):
                        for q in range(4):
                            P.pe(lambda e, q=q, half=half, b2=b2: e.matmul(b2[:, half, :], lhsT=ogT[:, q, :], rhs=UBo[:, q, half * 512:(half + 1) * 512],
                                                                          start=(q == 0), stop=(q == 3)), reads=["ogT", kb], writes=[bk2[half]])
                    ypt = yp[tile % 2]
                    ypk = ("yp", tile % 2)
                    P.act(lambda e, b2=b2, ypt=ypt: e.copy(out=ypt.rearrange("p (h n) -> p h n", h=2), in_=b2), reads=bk2, writes=[ypk])
                    if h == 0:
                        P.dma("sp", lambda e, ypt=ypt, tile=tile: e.dma_start(out=Y_d[tile * 128:(tile + 1) * 128, :], in_=ypt), ("ypds", tile % 2),
                              reads=[ypk], writes=[("Y", tile)])
                    else:
                        P.dma("pool", lambda e, ypt=ypt, tile=tile: e.dma_start(out=Y_d[tile * 128:(tile + 1) * 128, :], in_=ypt, accum_op=ALU.add), ("ypdp", tile % 2),
                              reads=[ypk, ("Y", tile)], writes=[("Y", tile)])
                    ktok(c, 0, "kzf")
                    supd(Sf, "Sf", c, 2)
                if not samp:
                    P.dma("sp", lambda e, si=si: e.dma_start(out=o_ret[si - 1, ri, 0, h].rearrange("(c p) n -> p c n", p=128), in_=Sf), "oret",
                          reads=["Sf"], writes=[("o_ret", si, ri, 0, h)])
                    final_keys.append(("o_ret", si, ri, 0, h))
        P.barrier()

    def stage_gla(l):
        A.reset(MARK)
        reli = A.a(128, I32)
        REL = A.a(128)
        MF = A.a(128)
        MB = A.a(128)
        TriF = A.a(128)
        TriB = A.a(128)
        wa1 = A.a(2 * 8 * 16, BF16, "p (d c n) -> p d c n", d=2, c=8)
        wa2 = A.a(2 * 512, F32, "p (d n) -> p d n", d=2)
        tTa = A.a(2 * 128, F32, "p (d n) -> p d n", d=2)
        lap = A.a(2 * 128, F32, "p (d n) -> p d n", d=2)
        Eq = A.a(2 * 128, F32, "p (d n) -> p d n", d=2)
        Ek = A.a(2 * 128, F32, "p (d n) -> p d n", d=2)
        qT = A.a(2048, BF16)
        kT = A.a(2048, BF16)
        qfT = A.a(2048, BF16)
        kfT = A.a(2048, BF16)
        qbT = A.a(2048, BF16)
        kbT = A.a(2048, BF16)
        ElF = A.a(16)
        ElB = A.a(16)
        vv = A.a(16 * 256, BF16, "p (c n) -> p c n", c=16)
        rs_ = A.a(16 * 256, BF16, "p (c n) -> p c n", c=16)
        SbB = A.a(16 * 256, BF16, "p (c n) -> p c n", c=16)
        Sf = A.a(256)
        Sb = A.a(256)
        Sfb = A.a(256, BF16)
        kt = A.a(128, BF16)
        sa = A.a(128)
        sbm = A.a(128)
        PT = A.a(128, BF16)
        on = A.a(256)
        og = A.a(256, BF16)
        ogT = A.a(256, BF16, "p (c n) -> p c n", c=2)
        yp = [A.a(D) for _ in range(2)]
        P.pool(lambda e: e.iota(reli, pattern=[[1, 128]], base=0, channel_multiplier=-1), writes=["reli"])
        P.dve(lambda e: e.tensor_copy(REL, reli), reads=["reli"], writes=["REL"])
        P.dve(lambda e: e.tensor_scalar(out=MF, in0=REL, scalar1=0.0, scalar2=None, op0=ALU.is_ge), reads=["REL"], writes=["MF"])
        P.dve(lambda e: e.tensor_scalar(out=MB, in0=REL, scalar1=0.0, scalar2=None, op0=ALU.is_le), reads=["REL"], writes=["MB"])
        P.dve(lambda e: e.tensor_scalar(out=TriF, in0=MF, scalar1=-1.0 / 16.0, scalar2=None, op0=ALU.mult), reads=["MF"], writes=["TriF"])
        P.dve(lambda e: e.tensor_scalar(out=TriB, in0=MB, scalar1=-1.0 / 16.0, scalar2=None, op0=ALU.mult), reads=["MB"], writes=["TriB"])
        for d in range(2):
            P.dma("pool", lambda e, d=d: e.dma_start(out=wa1[:, d, :, :], in_=kcview(gla_w_a1[d])), ("wa1", d), writes=["wa1"])
            P.dma("sp", lambda e, d=d: e.dma_start(out=wa2[0:17, d, :], in_=gla_w_a2a[d]), ("wa2", d), writes=["wa2"])
        P.dve(lambda e: e.memset(tTa[0:32, :, :], 1.0), writes=["tTa"])
        for h in range(4):
            U, uk = unit()
            Uin = U[:, 0:6144].rearrange("p (c n) -> p c n", c=8)
            Uo = U[:, 6144:8192].rearrange("p (c n) -> p c n", c=2)
            ch = ("ringd", uk[1])
            cast_load(Uin[:, :, 0:128], kcview(gla_w_in[:, h * 128:(h + 1) * 128]), uk, ch)
            cast_load(Uin[:, :, 128:256], kcview(gla_w_in[:, 512 + h * 128:512 + (h + 1) * 128]), uk, ch)
            cast_load(Uin[:, :, 256:512], kcview(gla_w_in[:, 1024 + h * 256:1024 + (h + 1) * 256]), uk, ch)
            cast_load(Uin[:, :, 512:768], kcview(gla_w_in[:, 2048 + h * 256:2048 + (h + 1) * 256]), uk, ch)
            cast_load(Uo, kcview(gla_w_out[h * 256:(h + 1) * 256, :]), uk, ch)
            for si, (t0, nch, j, samp) in enumerate(SEQS):
                L = nch * 128
                tok0 = t0 * 128
                for c0 in range(0, L, 512):
                    n = min(512, L - c0)
                    for (dstT, colb, scl, nm) in ((qT, 0, 128.0 ** -0.5, "qT"), (kT, 128, 1.0, "kT")):
                        bank, bk = ps.one()
                        for kc in range(8):
                            P.pe(lambda e, kc=kc, bank=bank, colb=colb, c0=c0, n=n: e.matmul(bank[:, 0:n], lhsT=Uin[:, kc, colb:colb + 128],
                                                                                            rhs=hT[:, kc, tok0 + c0:tok0 + c0 + n], start=(kc == 0), stop=(kc == 7)),
                                 reads=[uk], writes=[bk])
                        P.act(lambda e, n=n, scl=scl, bank=bank, c0=c0, dstT=dstT: e.activation(out=dstT[:, c0:c0 + n], in_=bank[:, 0:n], func=AF.Identity, scale=scl),
                              reads=[bk], writes=[nm])
                for c in range(nch):
                    tk = slice(tok0 + c * 128, tok0 + (c + 1) * 128)
                    bank, bk = ps.one()
                    for kc in range(8):
                        P.pe(lambda e, kc=kc, bank=bank, tk=tk: e.matmul(bank, lhsT=hT[:, kc, tk], rhs=Uin[:, kc, 256:768], start=(kc == 0), stop=(kc == 7)),
                             reads=[uk], writes=[bk])
                    P.act(lambda e, c=c, bank=bank: e.copy(out=vv[:, c, :], in_=bank[:, 0:256]), reads=[bk], writes=[("vv", c)])
                    P.act(lambda e, c=c, bank=bank: e.activation(out=rs_[:, c, :], in_=bank[:, 256:512], func=AF.Silu), reads=[bk], writes=[("rs", c)])
                    bt, bkt = ps.one()
                    for d in range(2):
                        for kc in range(8):
                            P.pe(lambda e, kc=kc, d=d, bt=bt, tk=tk: e.matmul(bt[0:16, d * 128:(d + 1) * 128], lhsT=wa1[:, d, kc, :], rhs=hT[:, kc, tk],
                                                                            start=(kc == 0), stop=(kc == 7)), reads=["wa1"], writes=[bkt])
                    P.dve(lambda e, bt=bt: e.tensor_copy(tTa[0:16, :, :].rearrange("p d n -> p (d n)"), bt[0:16, 0:256]), reads=[bkt], writes=["tTa"])
                    bz, bkz = ps.one()
                    for d in range(2):
                        P.pe(lambda e, d=d, bz=bz: e.matmul(bz[:, d * 128:(d + 1) * 128], lhsT=tTa[0:17, d, :], rhs=wa2[0:17, d, h * 128:(h + 1) * 128],
                                                           start=True, stop=True), reads=["tTa", "wa2"], writes=[bkz])
                    P.act(lambda e, bz=bz: e.activation(out=lap.rearrange("p d n -> p (d n)"), in_=bz[:, 0:256], func=AF.Exp, scale=-1.0), reads=[bkz], writes=["lap"])
                    P.act(lambda e: e.activation(out=lap, in_=lap, func=AF.Ln, bias=onesf[:, 0:1], scale=1.0), reads=["lap"], writes=["lap"])
                    bc, bkc = ps.one()
                    P.pe(lambda e, bc=bc: e.matmul(bc[:, 0:128], lhsT=lap[:, 0, :], rhs=TriF, start=True, stop=True), reads=["lap", "TriF"], writes=[bkc])
                    P.pe(lambda e, bc=bc: e.matmul(bc[:, 128:256], lhsT=lap[:, 1, :], rhs=TriB, start=True, stop=True), reads=["lap", "TriB"], writes=[bkc])
                    P.act(lambda e, bc=bc: e.activation(out=Eq.rearrange("p d n -> p (d n)"), in_=bc[:, 0:256], func=AF.Exp), reads=[bkc], writes=["Eq"])
                    P.act(lambda e, bc=bc: e.activation(out=Ek.rearrange("p d n -> p (d n)"), in_=bc[:, 0:256], func=AF.Exp, scale=-1.0), reads=[bkc], writes=["Ek"])
                    cs = slice(c * 128, (c + 1) * 128)
                    P.dve(lambda e, cs=cs: e.tensor_tensor(out=qfT[:, cs], in0=qT[:, cs], in1=Eq[:, 0, :], op=ALU.mult), reads=["qT", "Eq"], writes=["qfT"])
                    P.dve(lambda e, cs=cs: e.tensor_tensor(out=kfT[:, cs], in0=kT[:, cs], in1=Ek[:, 0, :], op=ALU.mult), reads=["kT", "Ek"], writes=["kfT"])
                    P.dve(lambda e, cs=cs: e.tensor_tensor(out=qbT[:, cs], in0=qT[:, cs], in1=Eq[:, 1, :], op=ALU.mult), reads=["qT", "Eq"], writes=["qbT"])
                    P.dve(lambda e, cs=cs: e.tensor_tensor(out=kbT[:, cs], in0=kT[:, cs], in1=Ek[:, 1, :], op=ALU.mult), reads=["kT", "Ek"], writes=["kbT"])
                    P.dve(lambda e, c=c: e.tensor_copy(ElF[:, c:c + 1], Eq[:, 0, 127:128]), reads=["Eq"], writes=["ElF"])
                    P.dve(lambda e, c=c: e.tensor_copy(ElB[:, c:c + 1], Eq[:, 1, 0:1]), reads=["Eq"], writes=["ElB"])
                for (S, d, nm) in ((Sf, 0, "Sf"), (Sb, 1, "Sb")):
                    if samp:
                        P.dma("sp", lambda e, S=S, d=d: e.dma_start(out=S, in_=state_gla[d, h]), ("ld" + nm,), writes=[nm])
                    else:
                        P.dve(lambda e, S=S: e.memset(S, 0.0), writes=[nm])

                def supd(S, nm, kxT, kxn, c, El):
                    bankT, bkT = ps.one()
                    bTb = bankT.bitcast(BF16)
                    P.pe(lambda e, bTb=bTb, c=c: e.transpose(bTb[:, 0:128], kxT[:, c * 128:(c + 1) * 128], identb), reads=[kxn], writes=[bkT])
                    P.act(lambda e, bTb=bTb: e.copy(out=kt, in_=bTb[:, 0:128]), reads=[bkT], writes=["kt"])
                    bankA, bkA = ps.one()
                    P.pe(lambda e, bankA=bankA, c=c: e.matmul(bankA[:, 0:256], lhsT=kt, rhs=vv[:, c, :], start=True, stop=True), reads=["kt", ("vv", c)], writes=[bkA])
                    P.dve(lambda e, bankA=bankA: e.tensor_tensor(out=S, in0=S, in1=bankA[:, 0:256], op=ALU.add), reads=[bkA, nm], writes=[nm])
                    P.dve(lambda e, c=c: e.tensor_scalar(out=S, in0=S, scalar1=El[:, c:c + 1], scalar2=None, op0=ALU.mult), reads=[nm, "ElF", "ElB"], writes=[nm])

                for c in reversed(range(nch)):
                    P.act(lambda e, c=c: e.copy(out=SbB[:, c, :], in_=Sb), reads=["Sb"], writes=[("sbb", c)])
                    supd(Sb, "Sb", kbT, "kbT", c, ElB)
                if not samp:
                    P.dma("sp", lambda e, si=si: e.dma_start(out=o_gla[si - 1, 1, h], in_=Sb), "ogla", reads=["Sb"], writes=[("o_gla", si, 1, h)])
                    final_keys.append(("o_gla", si, 1, h))
                for c in range(nch):
                    tile = t0 + c
                    cs = slice(c * 128, (c + 1) * 128)
                    P.act(lambda e: e.copy(out=Sfb, in_=Sf), reads=["Sf"], writes=["Sfb"])
                    bF, bkF = ps.one()
                    P.pe(lambda e, bF=bF, cs=cs: e.matmul(bF[:, 0:128], lhsT=kfT[:, cs], rhs=qfT[:, cs], start=True, stop=True), reads=["kfT", "qfT"], writes=[bkF])
                    P.pe(lambda e, bF=bF, cs=cs: e.matmul(bF[:, 128:256], lhsT=kbT[:, cs], rhs=qbT[:, cs], start=True, stop=True), reads=["kbT", "qbT"], writes=[bkF])
                    P.dve(lambda e, bF=bF: e.tensor_tensor(out=sa, in0=bF[:, 0:128], in1=MF, op=ALU.mult), reads=[bkF, "MF"], writes=["sa"])
                    P.dve(lambda e, bF=bF: e.tensor_tensor(out=sbm, in0=bF[:, 128:256], in1=MB, op=ALU.mult), reads=[bkF, "MB"], writes=["sbm"])
                    P.dve(lambda e: e.tensor_tensor(out=PT, in0=sa, in1=sbm, op=ALU.add), reads=["sa", "sbm"], writes=["PT"])
                    bO, bkO = ps.one()
                    P.pe(lambda e, bO=bO, c=c: e.matmul(bO[:, 0:256], lhsT=PT, rhs=vv[:, c, :], start=True, stop=False), reads=["PT", ("vv", c)], writes=[bkO])
                    P.pe(lambda e, bO=bO, cs=cs: e.matmul(bO[:, 0:256], lhsT=qfT[:, cs], rhs=Sfb, start=False, stop=False), reads=["qfT", "Sfb"], writes=[bkO])
                    P.pe(lambda e, bO=bO, cs=cs, c=c: e.matmul(bO[:, 0:256], lhsT=qbT[:, cs], rhs=SbB[:, c, :], start=False, stop=True), reads=["qbT", ("sbb", c)], writes=[bkO])
                    P.dve(lambda e, bO=bO: e.bn_stats(out=st6[:, 0, :], in_=bO[:, 0:256]), reads=[bkO], writes=["st6"])
                    P.dve(lambda e: e.bn_aggr(out=mv[:, 0:2], in_=st6[:, 0:1, :]), reads=["st6"], writes=["mv"])
                    P.dve(lambda e: e.tensor_scalar_add(out=mv[:, 2:3], in0=mv[:, 1:2], scalar1=1e-5), reads=["mv"], writes=["mv"])
                    P.act(lambda e: e.sqrt(out=mv[:, 2:3], in_=mv[:, 2:3]), reads=["mv"], writes=["mv"])
                    P.dve(lambda e: e.reciprocal(out=mv[:, 2:3], in_=mv[:, 2:3]), reads=["mv"], writes=["mv"])
                    P.dve(lambda e, bO=bO: e.tensor_scalar(out=on, in0=bO[:, 0:256], scalar1=mv[:, 0:1], scalar2=mv[:, 2:3], op0=ALU.subtract, op1=ALU.mult),
                          reads=[bkO, "mv"], writes=["on"])
                    P.dve(lambda e, c=c: e.tensor_tensor(out=og, in0=on, in1=rs_[:, c, :], op=ALU.mult), reads=["on", ("rs", c)], writes=["og"])
                    bankT, bkT = ps.one()
                    bTb = bankT.bitcast(BF16)
                    for q in range(2):
                        P.pe(lambda e, q=q, bTb=bTb: e.transpose(bTb[:, q * 128:(q + 1) * 128], og[:, q * 128:(q + 1) * 128], identb), reads=["og"], writes=[bkT])
                    P.act(lambda e, bTb=bTb: e.copy(out=ogT.rearrange("p c n -> p (c n)"), in_=bTb[:, 0:256]), reads=[bkT], writes=["ogT"])
                    b2, bk2 = ps.two()
                    for half in range(2):
                        for q in range(2):
                            P.pe(lambda e, q=q, half=half, b2=b2: e.matmul(b2[:, half, :], lhsT=ogT[:, q, :], rhs=Uo[:, q, half * 512:(half + 1) * 512],
                                                                          start=(q == 0), stop=(q == 1)), reads=["ogT", uk], writes=[bk2[half]])
                    ypt = yp[tile % 2]
                    ypk = ("yp", tile % 2)
                    P.act(lambda e, b2=b2, ypt=ypt: e.copy(out=ypt.rearrange("p (h n) -> p h n", h=2), in_=b2), reads=bk2, writes=[ypk])
                    if h == 0:
                        P.dma("sp", lambda e, ypt=ypt, tile=tile: e.dma_start(out=Y_d[tile * 128:(tile + 1) * 128, :], in_=ypt), ("ypds", tile % 2),
                              reads=[ypk], writes=[("Y", tile)])
                    else:
                        P.dma("pool", lambda e, ypt=ypt, tile=tile: e.dma_start(out=Y_d[tile * 128:(tile + 1) * 128, :], in_=ypt, accum_op=ALU.add), ("ypdp", tile % 2),
                              reads=[ypk, ("Y", tile)], writes=[("Y", tile)])
                    supd(Sf, "Sf", kfT, "kfT", c, ElF)
                if not samp:
                    P.dma("sp", lambda e, si=si: e.dma_start(out=o_gla[si - 1, 0, h], in_=Sf), "ogla", reads=["Sf"], writes=[("o_gla", si, 0, h)])
                    final_keys.append(("o_gla", si, 0, h))
        P.barrier()

    def stage_s5(l):
        A.reset(MARK)
        ybuf = A.a(8 * T, BF16, "p (c t) -> p c t", c=8)
        Rg = A.a(5120)
        Bb = A.a(2 * 1024, BF16, "p (k t s) -> p k t s", k=2, t=8)
        dT = A.a(8)
        acol = A.a(96, F32, "p (k t) -> p k t", k=3)
        x0c = A.a(64, F32, "p (k t) -> p k t", k=2)
        thc = A.a(32)
        rc = A.a(32)
        kci = A.a(32, I32)
        kcf = A.a(32)
        j1i = A.a(128, I32)
        J1 = A.a(128)
        rC = A.a(8)
        rS = A.a(8)
        tA = A.a(8)
        tB = A.a(8)
        xe = A.a(16, F32, "p (k t) -> p k t", k=2)
        r0 = ring[0].bitcast(F32)
        r1 = ring[1].bitcast(F32)
        pb = [r0[:, i * 1024:(i + 1) * 1024] for i in range(4)] + [r1[:, i * 1024:(i + 1) * 1024] for i in range(4)]
        arow = Rg[:, 0:3072].rearrange("p (k n) -> p k n", k=3)
        braw = Rg[:, 3072:5120].rearrange("p (k n) -> p k n", k=2)
        Ct = Rg[:, 0:1024].rearrange("p (t j) -> p t j", t=8)
        St = Rg[:, 1024:2048].rearrange("p (t j) -> p t j", t=8)
        Rt = Rg[:, 2048:3072].rearrange("p (t j) -> p t j", t=8)
        xx = Rg[:, 3072:4096].bitcast(BF16)
        xre = xx[:, 0:1024].rearrange("p (t j) -> p t j", t=8)
        xim = xx[:, 1024:2048].rearrange("p (t j) -> p t j", t=8)
        Cb = Rg[:, 4096:5120].bitcast(BF16).rearrange("p (k t s) -> p k t s", k=2, t=8)
        P.dma("sp", lambda e: e.dma_start(out=dT, in_=s5_dT), "dT", writes=["dT"])
        P.pool(lambda e: e.iota(j1i, pattern=[[1, 128]], base=1, channel_multiplier=0), writes=["j1i"])
        P.dve(lambda e: e.tensor_copy(J1, j1i), reads=["j1i"], writes=["J1"])

        def wrap_sin(dst, tmp, tmpi, nm):
            P.dve(lambda e: e.tensor_scalar(out=tmp, in0=dst, scalar1=1.0 / (2 * PI), scalar2=None, op0=ALU.mult), reads=[nm], writes=[nm + "t"])
            P.dve(lambda e: e.tensor_copy(tmpi, tmp), reads=[nm + "t"], writes=[nm + "i"])
            P.dve(lambda e: e.tensor_copy(tmp, tmpi), reads=[nm + "i"], writes=[nm + "t"])
            P.dve(lambda e: e.scalar_tensor_tensor(out=dst, in0=tmp, scalar=-2 * PI, in1=dst, op0=ALU.mult, op1=ALU.add), reads=[nm + "t", nm], writes=[nm])
            P.dve(lambda e: e.tensor_scalar(out=dst, in0=dst, scalar1=PI, scalar2=-PI, op0=ALU.min, op1=ALU.max), reads=[nm], writes=[nm])
            P.act(lambda e: e.activation(out=dst, in_=dst, func=AF.Sin), reads=[nm], writes=[nm])

        for tg in range(4):
            for d in range(2):
                tsl = slice(tg * 8, (tg + 1) * 8)
                P.dma("sp", lambda e: e.dma_start(out=acol.rearrange("p k t -> p (k t)"), in_=s5_acol[d]), "acol", writes=["acol"])
                P.dma("sp", lambda e: e.dma_start(out=x0c[:, 0, :], in_=s5_x0[d, 0]), "x0c0", writes=["x0c"])
                P.dma("sp", lambda e: e.dma_start(out=x0c[:, 1, :], in_=s5_x0[d, 1]), "x0c1", writes=["x0c"])
                P.act(lambda e: e.activation(out=acol[:, 2, :], in_=acol[:, 2, :], func=AF.Exp), reads=["acol"], writes=["acol"])
                P.dve(lambda e: e.tensor_tensor(out=rc, in0=acol[:, 0, :], in1=acol[:, 2, :], op=ALU.mult), reads=["acol"], writes=["rc"])
                P.act(lambda e: e.activation(out=rc, in_=rc, func=AF.Exp), reads=["rc"], writes=["rc"])
                P.dve(lambda e: e.tensor_tensor(out=thc, in0=acol[:, 1, :], in1=acol[:, 2, :], op=ALU.mult), reads=["acol"], writes=["thc"])
                P.dve(lambda e: e.tensor_scalar(out=kcf, in0=thc, scalar1=1.0 / (2 * PI), scalar2=None, op0=ALU.mult), reads=["thc"], writes=["kcf"])
                P.dve(lambda e: e.tensor_copy(kci, kcf), reads=["kcf"], writes=["kci"])
                P.dve(lambda e: e.tensor_copy(kcf, kci), reads=["kci"], writes=["kcf"])
                P.dve(lambda e: e.scalar_tensor_tensor(out=thc, in0=kcf, scalar=-2 * PI, in1=thc, op0=ALU.mult, op1=ALU.add), reads=["kcf", "thc"], writes=["thc"])
                w0, w1, w2, w3, w4, w5 = pb[0], pb[1], pb[2], pb[3], pb[4], pb[5]
                wi_ = pb[6].bitcast(I32)
                P.dma("sp", lambda e: e.dma_start(out=arow, in_=s5_arow[d][:, tg * 1024:(tg + 1) * 1024].partition_broadcast(128)), "arow", writes=["arow"])
                for k in range(2):
                    P.dma("sp", lambda e: e.dma_start(out=braw[:, k, :].rearrange("p (t s) -> p t s", t=8), in_=s5_bblk[d, k][:, tg * 8:(tg + 1) * 8, :]),
                          ("braw", k), writes=["braw"])
                ar = arow[:, 0, :]
                ai_ = arow[:, 1, :]
                stp = arow[:, 2, :]
                P.act(lambda e: e.activation(out=stp, in_=stp, func=AF.Exp), reads=["arow"], writes=["arow"])
                P.dve(lambda e: e.tensor_tensor(out=w0, in0=ar, in1=stp, op=ALU.mult), reads=["arow"], writes=["w0"])
                P.act(lambda e: e.activation(out=w0, in_=w0, func=AF.Exp), reads=["w0"], writes=["w0"])
                P.dve(lambda e: e.tensor_tensor(out=w2, in0=ai_, in1=stp, op=ALU.mult), reads=["arow"], writes=["w2"])
                P.dve(lambda e: e.tensor_scalar(out=w3, in0=w2, scalar1=PI / 2, scalar2=None, op0=ALU.add), reads=["w2"], writes=["w3"])
                wrap_sin(w2, w5, wi_, "w2")
                wrap_sin(w3, w5, wi_, "w3")
                P.dve(lambda e: e.tensor_tensor(out=w3, in0=w3, in1=w0, op=ALU.mult), reads=["w3", "w0"], writes=["w3"])
                P.dve(lambda e: e.tensor_tensor(out=w2, in0=w2, in1=w0, op=ALU.mult), reads=["w2", "w0"], writes=["w2"])
                P.dve(lambda e: e.tensor_scalar(out=w3, in0=w3, scalar1=-1.0, scalar2=None, op0=ALU.add), reads=["w3"], writes=["w3"])
                P.dve(lambda e: e.tensor_tensor(out=w0, in0=ar, in1=ar, op=ALU.mult), reads=["arow", "w0"], writes=["w0"])
                P.dve(lambda e: e.tensor_tensor(out=w1, in0=ai_, in1=ai_, op=ALU.mult), reads=["arow"], writes=["w1"])
                P.dve(lambda e: e.tensor_tensor(out=w0, in0=w0, in1=w1, op=ALU.add), reads=["w0", "w1"], writes=["w0"])
                P.dve(lambda e: e.reciprocal(out=w0, in_=w0), reads=["w0"], writes=["w0"])
                P.dve(lambda e: e.tensor_tensor(out=w1, in0=w3, in1=ar, op=ALU.mult), reads=["w3", "arow", "w1"], writes=["w1"])
                P.dve(lambda e: e.tensor_tensor(out=w4, in0=w2, in1=ai_, op=ALU.mult), reads=["w2", "arow"], writes=["w4"])
                P.dve(lambda e: e.tensor_tensor(out=w1, in0=w1, in1=w4, op=ALU.add), reads=["w1", "w4"], writes=["w1"])
                P.dve(lambda e: e.tensor_tensor(out=w1, in0=w1, in1=w0, op=ALU.mult), reads=["w1", "w0"], writes=["w1"])
                P.dve(lambda e: e.tensor_tensor(out=w4, in0=w2, in1=ar, op=ALU.mult), reads=["w2", "arow", "w4"], writes=["w4"])
                P.dve(lambda e: e.tensor_tensor(out=w5, in0=w3, in1=ai_, op=ALU.mult), reads=["w3", "arow", "w2t", "w3t"], writes=["w5"])
                P.dve(lambda e: e.tensor_tensor(out=w4, in0=w4, in1=w5, op=ALU.subtract), reads=["w4", "w5"], writes=["w4"])
                P.dve(lambda e: e.tensor_tensor(out=w4, in0=w4, in1=w0, op=ALU.mult), reads=["w4", "w0"], writes=["w4"])
                P.dve(lambda e: e.tensor_tensor(out=w0, in0=braw[:, 0, :], in1=w1, op=ALU.mult), reads=["braw", "w1", "w0", "w4"], writes=["w0"])
                P.dve(lambda e: e.tensor_tensor(out=w2, in0=braw[:, 1, :], in1=w4, op=ALU.mult), reads=["braw", "w4", "w2"], writes=["w2"])
                P.dve(lambda e: e.tensor_tensor(out=Bb[:, 0, :, :].rearrange("p t s -> p (t s)"), in0=w0, in1=w2, op=ALU.subtract), reads=["w0", "w2"], writes=["Bb"])
                P.dve(lambda e: e.tensor_tensor(out=w0, in0=braw[:, 1, :], in1=w1, op=ALU.mult), reads=["braw", "w1", "w0"], writes=["w0"])
                P.dve(lambda e: e.tensor_tensor(out=w2, in0=braw[:, 0, :], in1=w4, op=ALU.mult), reads=["braw", "w4", "w2"], writes=["w2"])
                P.dve(lambda e: e.tensor_tensor(out=Bb[:, 1, :, :].rearrange("p t s -> p (t s)"), in0=w0, in1=w2, op=ALU.add), reads=["w0", "w2"], writes=["Bb"])
                P.barrier()
                ang, w5t = pb[0], pb[1]
                angi = pb[2].bitcast(I32)
                for (tab, shift, nm) in ((St, 0.0, "St"), (Ct, PI / 2, "Ct")):
                    P.dve(lambda e: e.tensor_tensor(out=ang.rearrange("p (t j) -> p t j", t=8), in0=J1.unsqueeze(1).to_broadcast([128, 8, 128]),
                                                    in1=thc[:, tsl].unsqueeze(2).to_broadcast([128, 8, 128]), op=ALU.mult), reads=["J1", "thc", "ang"], writes=["ang"])
                    P.dve(lambda e: e.tensor_scalar(out=ang, in0=ang, scalar1=shift, scalar2=None, op0=ALU.add), reads=["ang"], writes=["ang"])
                    wrap_sin(ang, w5t, angi, "ang")
                    P.dve(lambda e: e.tensor_copy(tab.rearrange("p t j -> p (t j)"), ang), reads=["ang"], writes=[nm])
                P.dve(lambda e: e.tensor_copy(Rt, rc[:, tsl].unsqueeze(2).to_broadcast([128, 8, 128])), reads=["rc"], writes=["Rt"])
                P.dve(lambda e: e.memset(Rt[:, :, 0:1], 0.0), reads=["Rt"], writes=["Rt"])
                P.dve(lambda e: e.tensor_tensor(out=rC, in0=rc[:, tsl], in1=Ct[:, :, 127], op=ALU.mult), reads=["rc", "Ct"], writes=["rC"])
                P.dve(lambda e: e.tensor_tensor(out=rS, in0=rc[:, tsl], in1=St[:, :, 127], op=ALU.mult), reads=["rc", "St"], writes=["rS"])
                for k in range(2):
                    P.dma("pool", lambda e: e.dma_start(out=Cb[:, k, :, :], in_=s5_cblk[d, k][:, tg * 8:(tg + 1) * 8, :]), ("cb", k), writes=["Cb"])
                P.dve(lambda e: e.tensor_scalar(out=Cb[:, 1, :, :], in0=Cb[:, 1, :, :], scalar1=-1.0, scalar2=None, op0=ALU.mult), reads=["Cb"], writes=["Cb"])
                P.barrier()
                vre, vim, zre, zim, w0, w1 = pb[0], pb[1], pb[2], pb[3], pb[4], pb[5]
                v3 = {"re": vre.rearrange("p (t j) -> p t j", t=8), "im": vim.rearrange("p (t j) -> p t j", t=8)}
                z3 = {"re": zre.rearrange("p (t j) -> p t j", t=8), "im": zim.rearrange("p (t j) -> p t j", t=8)}
                w0v = w0.rearrange("p (t j) -> p t j", t=8)
                w1v = w1.rearrange("p (t j) -> p t j", t=8)
                Rflat = Rt.rearrange("p t j -> p (t j)")
                for si, (t0, nch, j, samp) in enumerate(SEQS):
                    order = list(range(nch)) if d == 0 else list(reversed(range(nch)))
                    for ci, c in enumerate(order):
                        tile = t0 + c
                        tk = slice(tile * 128, (tile + 1) * 128)
                        bre, kre = ps.two()
                        bim, kim = ps.two()
                        for t in range(8):
                            fc = (tg * 8 + t) // 4
                            P.pe(lambda e: e.matmul(bre[:, t // 4, (t % 4) * 128:(t % 4 + 1) * 128], lhsT=Bb[:, 0, t, :], rhs=hT[:, fc, tk],
                                                    start=True, stop=True), reads=["Bb"], writes=[kre[t // 4]])
                            P.pe(lambda e: e.matmul(bim[:, t // 4, (t % 4) * 128:(t % 4 + 1) * 128], lhsT=Bb[:, 1, t, :], rhs=hT[:, fc, tk],
                                                    start=True, stop=True), reads=["Bb"], writes=[kim[t // 4]])
                        br3 = bre.rearrange("p a (b j) -> p (a b) j", j=128)
                        bi3 = bim.rearrange("p a (b j) -> p (a b) j", j=128)
                        if d == 1:
                            br3 = br3[:, :, ::-1]
                            bi3 = bi3[:, :, ::-1]
                        P.dve(lambda e: e.tensor_tensor(out=v3["re"], in0=br3, in1=Ct, op=ALU.mult), reads=kre + ["Ct"], writes=["vre"])
                        P.dve(lambda e: e.tensor_tensor(out=w0v, in0=bi3, in1=St, op=ALU.mult), reads=kim + ["St", "w0"], writes=["w0"])
                        P.dve(lambda e: e.tensor_tensor(out=vre, in0=vre, in1=w0, op=ALU.add), reads=["vre", "w0"], writes=["vre"])
                        P.dve(lambda e: e.tensor_tensor(out=v3["im"], in0=bi3, in1=Ct, op=ALU.mult), reads=kim + ["Ct"], writes=["vim"])
                        P.dve(lambda e: e.tensor_tensor(out=w0v, in0=br3, in1=St, op=ALU.mult), reads=kre + ["St", "w0"], writes=["w0"])
                        P.dve(lambda e: e.tensor_tensor(out=vim, in0=vim, in1=w0, op=ALU.subtract), reads=["vim", "w0"], writes=["vim"])
                        if ci == 0:
                            if samp:
                                for k, nm in ((0, "re"), (1, "im")):
                                    P.dve(lambda e: e.tensor_tensor(out=tA, in0=rc[:, tsl], in1=x0c[:, k, tsl], op=ALU.mult), reads=["rc", "x0c", "tA"], writes=["tA"])
                                    P.dve(lambda e: e.tensor_tensor(out=v3[nm][:, :, 0], in0=v3[nm][:, :, 0], in1=tA, op=ALU.add), reads=["v" + nm, "tA"], writes=["v" + nm])
                        else:
                            zlr = z3["re"][:, :, 127]
                            zli = z3["im"][:, :, 127]
                            P.dve(lambda e: e.tensor_tensor(out=tA, in0=rC, in1=zlr, op=ALU.mult), reads=["rC", "zre", "tA"], writes=["tA"])
                            P.dve(lambda e: e.tensor_tensor(out=tB, in0=rS, in1=zli, op=ALU.mult), reads=["rS", "zim", "tB"], writes=["tB"])
                            P.dve(lambda e: e.tensor_tensor(out=tA, in0=tA, in1=tB, op=ALU.subtract), reads=["tA", "tB"], writes=["tA"])
                            P.dve(lambda e: e.tensor_tensor(out=v3["re"][:, :, 0], in0=v3["re"][:, :, 0], in1=tA, op=ALU.add), reads=["vre", "tA"], writes=["vre"])
                            P.dve(lambda e: e.tensor_tensor(out=tA, in0=rS, in1=zlr, op=ALU.mult), reads=["rS", "zre", "tA"], writes=["tA"])
                            P.dve(lambda e: e.tensor_tensor(out=tB, in0=rC, in1=zli, op=ALU.mult), reads=["rC", "zim", "tB"], writes=["tB"])
                            P.dve(lambda e: e.tensor_tensor(out=tA, in0=tA, in1=tB, op=ALU.add), reads=["tA", "tB"], writes=["tA"])
                            P.dve(lambda e: e.tensor_tensor(out=v3["im"][:, :, 0], in0=v3["im"][:, :, 0], in1=tA, op=ALU.add), reads=["vim", "tA"], writes=["vim"])
                        P.dve(lambda e: e.tensor_tensor_scan(out=zre, data0=Rflat, data1=vre, initial=0.0, op0=ALU.mult, op1=ALU.add), reads=["Rt", "vre", "zre"], writes=["zre"])
                        P.dve(lambda e: e.tensor_tensor_scan(out=zim, data0=Rflat, data1=vim, initial=0.0, op0=ALU.mult, op1=ALU.add), reads=["Rt", "vim", "zim"], writes=["zim"])
                        xr = xre if d == 0 else xre[:, :, ::-1]
                        xi = xim if d == 0 else xim[:, :, ::-1]
                        P.dve(lambda e: e.tensor_tensor(out=w0v, in0=z3["re"], in1=Ct, op=ALU.mult), reads=["zre", "Ct", "w0"], writes=["w0"])
                        P.dve(lambda e: e.tensor_tensor(out=w1v, in0=z3["im"], in1=St, op=ALU.mult), reads=["zim", "St", "w1"], writes=["w1"])
                        P.dve(lambda e: e.tensor_tensor(out=xr, in0=w0v, in1=w1v, op=ALU.subtract), reads=["w0", "w1", "xre"], writes=["xre"])
                        P.dve(lambda e: e.tensor_tensor(out=w0v, in0=z3["re"], in1=St, op=ALU.mult), reads=["zre", "St", "w0"], writes=["w0"])
                        P.dve(lambda e: e.tensor_tensor(out=w1v, in0=z3["im"], in1=Ct, op=ALU.mult), reads=["zim", "Ct", "w1"], writes=["w1"])
                        P.dve(lambda e: e.tensor_tensor(out=xi, in0=w0v, in1=w1v, op=ALU.add), reads=["w0", "w1", "xim"], writes=["xim"])
                        by, kby = ps.one()
                        for fcl in range(2):
                            for q in range(4):
                                t = fcl * 4 + q
                                P.pe(lambda e: e.matmul(by[:, fcl * 128:(fcl + 1) * 128], lhsT=Cb[:, 0, t, :], rhs=xre[:, t, :], start=(q == 0), stop=False),
                                     reads=["Cb", "xre"], writes=[kby])
                                P.pe(lambda e: e.matmul(by[:, fcl * 128:(fcl + 1) * 128], lhsT=Cb[:, 1, t, :], rhs=xim[:, t, :], start=False, stop=(q == 3)),
                                     reads=["Cb", "xim"], writes=[kby])
                        yb = ybuf[:, tg * 2:tg * 2 + 2, tk]
                        by3 = by[:, 0:256].rearrange("p (c n) -> p c n", c=2)
                        if d == 0:
                            P.act(lambda e: e.copy(out=yb, in_=by3), reads=[kby], writes=[("ybuf", tg, tile)])
                        else:
                            w2v = w0[:, 0:256].rearrange("p (c n) -> p c n", c=2)
                            P.dve(lambda e: e.tensor_tensor(out=w2v, in0=by3, in1=yb, op=ALU.add), reads=[kby, ("ybuf", tg, tile), "w0"], writes=["w0"])
                            for fcl in range(2):
                                fc = tg * 2 + fcl
                                P.dve(lambda e: e.scalar_tensor_tensor(out=ybuf[:, fc, tk], in0=hT[:, fc, tk], scalar=dT[:, fc:fc + 1],
                                                                       in1=w0[:, fcl * 128:(fcl + 1) * 128], op0=ALU.mult, op1=ALU.add),
                                      reads=["w0", "dT"], writes=[("ybuf", tg, tile)])
                    if not samp:
                        zlr = z3["re"][:, :, 127]
                        zli = z3["im"][:, :, 127]
                        Cl = Ct[:, :, 127]
                        Sl = St[:, :, 127]
                        P.dve(lambda e: e.tensor_tensor(out=tA, in0=Cl, in1=zlr, op=ALU.mult), reads=["Ct", "zre", "tA"], writes=["tA"])
                        P.dve(lambda e: e.tensor_tensor(out=tB, in0=Sl, in1=zli, op=ALU.mult), reads=["St", "zim", "tB"], writes=["tB"])
                        P.dve(lambda e: e.tensor_tensor(out=xe[:, 0, :], in0=tA, in1=tB, op=ALU.subtract), reads=["tA", "tB"], writes=["xe0"])
                        P.dve(lambda e: e.tensor_tensor(out=tA, in0=Sl, in1=zlr, op=ALU.mult), reads=["St", "zre", "tA"], writes=["tA"])
                        P.dve(lambda e: e.tensor_tensor(out=tB, in0=Cl, in1=zli, op=ALU.mult), reads=["Ct", "zim", "tB"], writes=["tB"])
                        P.dve(lambda e: e.tensor_tensor(out=xe[:, 1, :], in0=tA, in1=tB, op=ALU.add), reads=["tA", "tB"], writes=["xe1"])
                        for k in range(2):
                            key = ("o_s5", si, d, k, tg)
                            P.dma("sp", lambda e: e.dma_start(out=o_s5[si - 1, d, k][:, tg * 8:(tg + 1) * 8], in_=xe[:, k, :]), ("os5", k),
                                  reads=["xe%d" % k], writes=[key])
                            final_keys.append(key)
                P.barrier()
        for fc in range(8):
            for c0 in range(0, T, 512):
                yb = ybuf[:, fc, c0:c0 + 512]
                w0s, w1s = pb[4][:, 0:512], pb[5][:, 0:512]
                P.act(lambda e: e.activation(out=w0s, in_=yb, func=AF.Square), reads=["g0"], writes=["g0"])
                P.dve(lambda e: e.tensor_scalar(out=w0s, in0=w0s, scalar1=0.044715, scalar2=1.0, op0=ALU.mult, op1=ALU.add), reads=["g0"], writes=["g0"])
                P.dve(lambda e: e.tensor_tensor(out=w0s, in0=w0s, in1=yb, op=ALU.mult), reads=["g0"], writes=["g0"])
                P.act(lambda e: e.activation(out=w1s, in_=w0s, func=AF.Sigmoid, scale=1.5957691216057308), reads=["g0", "g1"], writes=["g1"])
                P.dve(lambda e: e.tensor_tensor(out=yb, in0=yb, in1=w1s, op=ALU.mult), reads=["g1"], writes=[("gy", fc, c0)])
        U0 = ring[2].rearrange("p (c n) -> p c n", c=8)
        U1 = ring[3].rearrange("p (c n) -> p c n", c=8)
        k0, k1 = ("ring", 2), ("ring", 3)
        cast_load(U0, kcview(s5_w_glu[:, 0:D]), k0, ("ringd", 2))
        cast_load(U1, kcview(s5_w_glu[:, D:2 * D]), k1, ("ringd", 3))
        sgb = pb[0]
        yts = [pb[2], pb[3]]
        P.barrier()
        for tt in range(NT):
            tk = slice(tt * 128, (tt + 1) * 128)
            ba, ka_ = ps.two()
            bb, kb_ = ps.two()
            for half in range(2):
                for kc in range(8):
                    P.pe(lambda e: e.matmul(ba[:, half, :], lhsT=ybuf[:, kc, tk], rhs=U0[:, kc, half * 512:(half + 1) * 512],
                                            start=(kc == 0), stop=(kc == 7)), reads=[k0], writes=[ka_[half]])
                for kc in range(8):
                    P.pe(lambda e: e.matmul(bb[:, half, :], lhsT=ybuf[:, kc, tk], rhs=U1[:, kc, half * 512:(half + 1) * 512],
                                            start=(kc == 0), stop=(kc == 7)), reads=[k1], writes=[kb_[half]])
            P.act(lambda e: e.activation(out=sgb.rearrange("p (h n) -> p h n", h=2), in_=bb, func=AF.Sigmoid), reads=kb_, writes=["sgb"])
            yt = yts[tt % 2]
            P.dve(lambda e: e.tensor_tensor(out=yt.rearrange("p (h n) -> p h n", h=2), in0=ba, in1=sgb.rearrange("p (h n) -> p h n", h=2), op=ALU.mult),
                  reads=ka_ + ["sgb"], writes=[("gyt", tt % 2)])
            P.dma("sp", lambda e: e.dma_start(out=Y_d[tt * 128:(tt + 1) * 128, :], in_=yt), ("gyd", tt % 2), reads=[("gyt", tt % 2)], writes=[("Y", tt)])
        P.barrier()

    n_done = 0
    if os.environ.get("DBG_ONLY_MOE"):
        stage_mod(0)
        stage_moe(0, x0, xout, "xout")
        n_done = n_sub
    for l in range(4):
        if n_done >= n_sub:
            break
        stage_mod(l)
        src = x0 if l == 0 else xs_d
        last = (n_done + 1 == n_sub)
        dst, dname = (xout, "xout") if last else (xs_d, "xs")
        A.reset(MARK)
        stage_h(src, 0, 1)
        if l % 3 == 0:
            stage_ret(l, l // 3)
        elif l % 3 == 1:
            stage_gla(l)
        else:
            stage_s5(l)
        A.reset(MARK)
        stage_epi(l, 0, src, dst, dname)
        n_done += 1
        if n_done >= n_sub:
            break
        if skip_moe:
            continue
        last = (n_done + 1 == n_sub)
        dst, dname = (xout, "xout") if last else (xs_d, "xs")
        stage_moe(l, xs_d, dst, dname)
        n_done += 1
    for tt in range(NT):
        final_keys.append(("xout", tt))
    P.emit(final_keys=final_keys)
    return P


def _rot_tables():
    L, GW, dk = 2048, 64, 256
    rows = L // GW
    row = np.repeat(np.arange(rows, dtype=np.float32), GW)
    col = np.tile(np.arange(GW, dtype=np.float32), rows)
    nf = dk // 4
    inv = (10000.0 ** (-np.arange(nf, dtype=np.float32) / nf)).astype(np.float32)
    ang = np.concatenate([row[:, None] * inv, col[:, None] * inv], axis=-1)
    return np.ascontiguousarray(np.cos(ang).T.astype(np.float32)), np.ascontiguousarray(np.sin(ang).T.astype(np.float32))


def _col32(a):
    return np.ascontiguousarray(a.reshape(32, 2, 64).transpose(1, 2, 0).reshape(128, 32))


def _prep_shared(inp):
    f = np.float32
    sh = {}
    sh["w_mod"] = inp["w_mod"]
    sh["b_mod"] = inp["b_mod"]
    sh["b_modT"] = np.ascontiguousarray(inp["b_mod"].reshape(4, 48, 128).transpose(0, 2, 1))
    sh["ln_g"] = inp["ln_g"]
    sh["ln_b"] = inp["ln_b"]
    sh["ret_w_in"] = inp["ret_w_in"]
    sh["ret_w_out"] = inp["ret_w_out"]
    sh["ret_decay"] = np.ascontiguousarray(inp["ret_decay"].reshape(16))
    rc, rs = _rot_tables()
    sh["rot_cos"], sh["rot_sin"] = rc, rs
    sh["gla_w_in"] = np.ascontiguousarray(inp["gla_w_in"][0])
    sh["gla_w_a1"] = np.ascontiguousarray(inp["gla_w_a1"][0])
    sh["gla_w_a2a"] = np.ascontiguousarray(np.concatenate([inp["gla_w_a2"][0], inp["gla_b_a"][0][:, None, :]], axis=1))
    sh["gla_w_out"] = np.ascontiguousarray(inp["gla_w_out"][0])
    ar, ai, ls = inp["s5_a_re"][0], inp["s5_a_im"][0], inp["s5_log_step"][0]
    lse = np.broadcast_to(ls[:, :, None], (2, 64, 64))
    sh["s5_arow"] = np.ascontiguousarray(np.stack([ar.reshape(2, 4096), ai.reshape(2, 4096), lse.reshape(2, 4096)], axis=1))
    sh["s5_acol"] = np.ascontiguousarray(np.stack([np.concatenate([_col32(ar[d]), _col32(ai[d]), _col32(lse[d])], axis=1) for d in range(2)]))
    bb = np.zeros((2, 2, 128, 32, 128), f)
    cb = np.zeros((2, 2, 128, 32, 128), f)
    for d in range(2):
        for k, (bsrc, csrc) in enumerate(((inp["s5_b_re"][0, d], inp["s5_c_re"][0, d]), (inp["s5_b_im"][0, d], inp["s5_c_im"][0, d]))):
            for g in range(64):
                t = g // 2
                so = (g % 2) * 64
                fo = (g % 8) * 16
                bb[d, k, fo:fo + 16, t, so:so + 64] = bsrc[g].T
                cb[d, k, so:so + 64, t, fo:fo + 16] = csrc[g].T
    sh["s5_bblk"], sh["s5_cblk"] = bb, cb
    sh["s5_dT"] = np.ascontiguousarray(inp["s5_d"][0].reshape(8, 128).T)
    sh["s5_w_glu"] = np.ascontiguousarray(inp["s5_w_glu"][0])
    sh["moe_w_router"] = inp["moe_w_router"]
    sh["moe_b_router"] = inp["moe_b_router"]
    sh["moe_w_gu"] = inp["moe_w_gu"]
    sh["moe_b_guT"] = np.ascontiguousarray(inp["moe_b_gu"].reshape(4, 32, 16, 128).transpose(0, 3, 1, 2).reshape(4, 128, 512))
    sh["moe_w_down"] = inp["moe_w_down"]
    sh["moe_b_down"] = inp["moe_b_down"]
    return sh


def _prep_core(inp, sh, c):
    m = dict(sh)
    m["x0"] = np.ascontiguousarray(np.concatenate([inp["x_sample"][c], inp["x_prompt"][2 * c], inp["x_prompt"][2 * c + 1]], axis=0))
    cond = np.stack([inp["c_ctx"], inp["c"][c]])
    m["condT"] = np.ascontiguousarray(cond.reshape(2, 8, 128).transpose(2, 0, 1).reshape(128, 16))
    m["state_ret"] = np.ascontiguousarray(inp["state_ret"][c])
    m["state_gla"] = np.ascontiguousarray(inp["state_gla"][c, 0])
    m["s5_x0"] = np.ascontiguousarray(np.stack([np.stack([_col32(inp["state_s5_re"][c, 0, d]), _col32(inp["state_s5_im"][c, 0, d])]) for d in range(2)]))
    return m


_NC_CACHE = {}


def _get_nc(n_sub=8):
    if n_sub not in _NC_CACHE:
        nc = bass.Bass("TRN2", target_bir_lowering=False)
        build(nc, n_sub)
        _NC_CACHE[n_sub] = nc
    return _NC_CACHE[n_sub]


def _uncol32(a):
    return a.reshape(2, 64, 32).transpose(2, 0, 1).reshape(64, 64)


def kernel(**inputs):
    inp = {k: np.asarray(v) for k, v in inputs.items()}
    sh = _prep_shared(inp)
    nc = _get_nc(8)
    in_maps = [_prep_core(inp, sh, c) for c in range(8)]
    res = run_bass_kernel_spmd(nc, in_maps, core_ids=list(range(8)))
    f = np.float32
    y_prompt = np.zeros((16, 256, 1024), f)
    y_sample = np.zeros((8, 2048, 1024), f)
    new_ret = np.zeros((16, 2, 2, 4, 256, 512), f)
    new_gla = np.zeros((16, 1, 2, 4, 128, 256), f)
    new_re = np.zeros((16, 1, 2, 64, 64), f)
    new_im = np.zeros((16, 1, 2, 64, 64), f)
    for c in range(8):
        r = res.results[c]
        xo = r["xout"]
        y_sample[c] = xo[0:2048]
        y_prompt[2 * c] = xo[2048:2304]
        y_prompt[2 * c + 1] = xo[2304:2560]
        for s in range(2):
            b = 2 * c + s
            new_ret[b] = r["o_ret"][s]
            new_gla[b, 0] = r["o_gla"][s]
            for d in range(2):
                new_re[b, 0, d] = _uncol32(r["o_s5"][s, d, 0])
                new_im[b, 0, d] = _uncol32(r["o_s5"][s, d, 1])
    return (y_prompt, y_sample, new_ret, new_gla, new_re, new_im)
```

```python
import contextlib
import os
import types
import numpy as np
import concourse.bass as bass
import concourse.mybir as mybir
from concourse.bass_utils import run_bass_kernel_spmd

F32 = mybir.dt.float32
BF16 = mybir.dt.bfloat16
I32 = mybir.dt.int32
ALU = mybir.AluOpType
AF = mybir.ActivationFunctionType
AX = mybir.AxisListType

EPOCH = 30000
PI = float(np.pi)


class Prog:
    def __init__(self, nc):
        self.nc = nc
        self.ops = []
        self.dch_order = []

    @staticmethod
    def _freeze(fn):
        if fn.__closure__ is None:
            return fn
        cells = []
        for c in fn.__closure__:
            try:
                cells.append(types.CellType(c.cell_contents))
            except ValueError:
                cells.append(c)
        return types.FunctionType(fn.__code__, fn.__globals__, fn.__name__, fn.__defaults__, tuple(cells))

    def op(self, eng, fn, reads=(), writes=(), dch=None):
        if dch is not None and dch not in self.dch_order:
            self.dch_order.append(dch)
        self.ops.append((eng, self._freeze(fn), tuple(reads), tuple(writes), dch))

    def pe(self, fn, reads=(), writes=()):
        self.op("pe", fn, reads, writes)

    def dve(self, fn, reads=(), writes=()):
        self.op("dve", fn, reads, writes)

    def act(self, fn, reads=(), writes=()):
        self.op("act", fn, reads, writes)

    def pool(self, fn, reads=(), writes=()):
        self.op("pool", fn, reads, writes)

    def dma(self, eng, fn, dch, reads=(), writes=()):
        self.op(eng, fn, reads, writes, dch)

    def barrier(self):
        self.ops.append(("BAR", None, (), (), None))

    def emit(self, final_keys=()):
        nc = self.nc
        ops = self.ops
        ENGS = ("pe", "dve", "act", "pool", "sp")
        last_w = {}
        readers = {}
        n_eng = {e: 0 for e in ENGS}
        dch_cnt = {}
        tokens = []
        waits = []
        needed = {e: set() for e in ENGS}
        seen = {e: {} for e in ENGS}
        pending_bar = {e: None for e in ENGS}

        def src_of(tok):
            return tok[1] if tok[0] == "e" else ("d", tok[1])

        for (eng, fn, reads, writes, dch) in ops:
            if eng == "BAR":
                snap = [("e", e, n_eng[e]) for e in ENGS if n_eng[e] > 0]
                snap += [("d", d, c) for d, c in dch_cnt.items()]
                for e in ENGS:
                    pending_bar[e] = snap
                tokens.append(None)
                waits.append([])
                continue
            if dch is None:
                n_eng[eng] += 1
                tok = ("e", eng, n_eng[eng])
            else:
                dch_cnt[dch] = dch_cnt.get(dch, 0) + 1
                tok = ("d", dch, dch_cnt[dch])
            deps = []
            if pending_bar[eng] is not None:
                for t in pending_bar[eng]:
                    if not (t[0] == "e" and t[1] == eng and eng in ("pe", "sp")):
                        deps.append((t, "bar"))
                pending_bar[eng] = None
            for k in reads:
                t = last_w.get(k)
                if t is not None:
                    deps.append((t, "raw"))
                if isinstance(k, tuple) and k[0] == "ps":
                    for t in readers.get(k, ()):
                        if t[0] == "e" and t[1] != eng:
                            deps.append((t, "rar"))
            for k in writes:
                t = last_w.get(k)
                if t is not None:
                    deps.append((t, "waw"))
                for t in readers.get(k, ()):
                    deps.append((t, "war"))
            if dch is not None and dch_cnt[dch] > 1:
                deps.append((("d", dch, dch_cnt[dch] - 1), "waw"))
            best = {}
            for t, kind in deps:
                if t == tok:
                    continue
                if t[0] == "e" and t[1] == eng and dch is None and kind != "bar":
                    if eng == "pe" or eng == "sp":
                        continue
                s = src_of(t)
                if seen[eng].get(s, 0) >= t[2]:
                    continue
                if s not in best or best[s][2] < t[2]:
                    best[s] = t
            for s, t in best.items():
                seen[eng][s] = t[2]
                if t[0] == "e":
                    needed[t[1]].add(t[2])
            waits.append(list(best.values()))
            tokens.append(tok)
            for k in writes:
                last_w[k] = tok
                readers[k] = []
            for k in reads:
                if k not in writes:
                    readers.setdefault(k, []).append(tok)
        fin = {}
        for k in final_keys:
            t = last_w.get(k)
            if t is not None:
                s = src_of(t)
                if s not in fin or fin[s][2] < t[2]:
                    fin[s] = t
                if t[0] == "e":
                    needed[t[1]].add(t[2])
        rank = {}
        for e in needed:
            for i, n in enumerate(sorted(needed[e])):
                rank[(e, n)] = i + 1
        n_epochs = {e: (len(needed[e]) + EPOCH - 1) // EPOCH for e in needed}
        self.stats = dict(n_ops=len(ops), n_eng=dict(n_eng),
                          n_sig={e: len(needed[e]) for e in needed}, n_dch=len(self.dch_order))
        with contextlib.ExitStack() as st:
            esem = {}
            for e in needed:
                for ep in range(max(1, n_epochs[e])):
                    esem[(e, ep)] = st.enter_context(nc.semaphore(f"s_{e}{ep}"))
            dsem = {}
            for i, d in enumerate(self.dch_order):
                dsem[d] = st.enter_context(nc.semaphore(f"d{i}"))
            block = st.enter_context(nc.Block())

            def tok_wait(engh, t):
                if t[0] == "e":
                    r = rank[(t[1], t[2])]
                    ep = (r - 1) // EPOCH
                    for q in range(ep):
                        engh.wait_ge(esem[(t[1], q)], EPOCH)
                    engh.wait_ge(esem[(t[1], ep)], r - ep * EPOCH)
                else:
                    engh.wait_ge(dsem[t[1]], 16 * t[2])

            per_eng = {e: [] for e in ENGS}
            for i, o in enumerate(ops):
                if o[0] != "BAR":
                    per_eng[o[0]].append(i)

            def run(ename, engh):
                for i in per_eng[ename]:
                    (eng, fn, reads, writes, dch) = ops[i]
                    for t in waits[i]:
                        tok_wait(engh, t)
                    ins = fn(engh)
                    tok = tokens[i]
                    if tok[0] == "d":
                        ins.then_inc(dsem[tok[1]], 16)
                    elif (tok[1], tok[2]) in rank:
                        r = rank[(tok[1], tok[2])]
                        ep = (r - 1) // EPOCH
                        ins.then_inc(esem[(tok[1], ep)], 1)
                if ename == "sp":
                    for t in fin.values():
                        tok_wait(engh, t)

            @block.sync
            def _(e):
                run("sp", e)

            @block.tensor
            def _(e):
                run("pe", e)

            @block.vector
            def _(e):
                run("dve", e)

            @block.scalar
            def _(e):
                run("act", e)

            @block.gpsimd
            def _(e):
                run("pool", e)


D = 1024
NT = 20
T = 2560
SEQS = [(0, 16, 1, True), (16, 2, 0, False), (18, 2, 0, False)]
DN_ALPHA = 8.0 ** 0.25
ARENA = 53000


class Arena:
    def __init__(self, nc):
        self.t = nc.alloc_sbuf_tensor("arena", [128, ARENA], F32)
        self.off = 0

    def mark(self):
        return self.off

    def reset(self, m):
        self.off = m

    def a(self, n, dt=F32, pat=None, **dims):
        sz = 4 if dt in (F32, I32) else 2
        nf = (n * sz + 3) // 4
        nf = (nf + 7) // 8 * 8
        assert self.off + nf <= ARENA, f"arena overflow {self.off}+{nf}"
        v = self.t[:, self.off:self.off + nf]
        self.off += nf
        if dt != F32:
            v = v.bitcast(dt)
        v = v[:, 0:n]
        if pat is not None:
            v = v.rearrange(pat, **dims)
        return v


class PS:
    def __init__(self, nc):
        self.t = nc.alloc_psum_tensor("psum", [128, 8, 512], F32)
        self.i = 0

    def one(self):
        b = self.i % 8
        self.i += 1
        return self.t[:, b, :], ("ps", b)

    def two(self):
        if self.i % 2:
            self.i += 1
        b = self.i % 8
        self.i += 2
        return self.t[:, b:b + 2, :], [("ps", b), ("ps", b + 1)]


def build(nc, n_sub=8, skip_moe=False):
    P = Prog(nc)
    A = Arena(nc)
    ps = PS(nc)

    def din(name, shape):
        return nc.dram_tensor(name, list(shape), F32, kind="ExternalInput").ap()

    def dout(name, shape):
        return nc.dram_tensor(name, list(shape), F32, kind="ExternalOutput").ap()

    x0 = din("x0", [T, D])
    condT_d = din("condT", [128, 16])
    w_mod = din("w_mod", [4, D, 6 * D])
    b_mod = din("b_mod", [4, 6 * D])
    b_modT = din("b_modT", [4, 128, 48])
    ln_g = din("ln_g", [4, 2, D])
    ln_b = din("ln_b", [4, 2, D])
    ret_w_in = din("ret_w_in", [2, D, 6 * D])
    ret_w_out = din("ret_w_out", [2, 2 * D, D])
    ret_decay = din("ret_decay", [16])
    state_ret = din("state_ret", [2, 2, 4, 256, 512])
    rot_cos = din("rot_cos", [128, 2048])
    rot_sin = din("rot_sin", [128, 2048])
    gla_w_in = din("gla_w_in", [D, 3 * D])
    gla_w_a1 = din("gla_w_a1", [2, D, 16])
    gla_w_a2a = din("gla_w_a2a", [2, 17, 512])
    gla_w_out = din("gla_w_out", [D, D])
    state_gla = din("state_gla", [2, 4, 128, 256])
    s5_arow = din("s5_arow", [2, 3, 4096])
    s5_acol = din("s5_acol", [2, 128, 96])
    s5_bblk = din("s5_bblk", [2, 2, 128, 32, 128])
    s5_cblk = din("s5_cblk", [2, 2, 128, 32, 128])
    s5_dT = din("s5_dT", [128, 8])
    s5_w_glu = din("s5_w_glu", [D, 2 * D])
    s5_x0 = din("s5_x0", [2, 2, 128, 32])
    nml = 0 if skip_moe else max(1, n_sub // 2)
    if nml:
        moe_w_router = din("moe_w_router", [nml, D, 32])
        moe_b_router = din("moe_b_router", [nml, 32])
        moe_w_gu = din("moe_w_gu", [nml, 32, D, 2 * D])
        moe_b_guT = din("moe_b_guT", [nml, 128, 512])
        moe_w_down = din("moe_w_down", [nml, 32, D, D])
        moe_b_down = din("moe_b_down", [nml, 32, D])

    xout = dout("xout", [T, D])
    o_ret = dout("o_ret", [2, 2, 2, 4, 256, 512])
    o_gla = dout("o_gla", [2, 2, 4, 128, 256])
    o_s5 = dout("o_s5", [2, 2, 2, 128, 32])
    xs_d = nc.dram_tensor("xs_scr", [T, D], F32, kind="Internal").ap()
    Y_d = nc.dram_tensor("y_scr", [T, D], F32, kind="Internal").ap()
    final_keys = []

    identf = A.a(128)
    identb = A.a(128 * 128 // 128, BF16) if False else A.a(128, BF16)
    hT = A.a(8 * T, BF16, "p (c t) -> p c t", c=8)
    ring = [A.a(8192, BF16) for _ in range(4)]
    grow = A.a(4 * D, BF16, "p (j g d) -> p j g d", j=2, g=2)
    lng = A.a(D)
    lnb = A.a(D)
    modT = A.a(64, F32, "p (j v c) -> p j v c", j=2, v=4)
    scb = A.a(16, BF16, "p (c j) -> p c j", c=8)
    condT = A.a(16, F32, "p (j c) -> p j c", j=2)
    bmT = A.a(48)
    onesf = A.a(128)
    zerob = A.a(8)
    st6 = A.a(12, F32, "p (a b) -> p a b", a=2)
    mv = A.a(8)
    MARK = A.mark()

    dq = [0]

    def dkey(prefix="q"):
        dq[0] += 1
        return (prefix, dq[0])

    ring_i = [0]

    def unit():
        i = ring_i[0] % 4
        ring_i[0] += 1
        return ring[i], ("ring", i)

    def cast_load(dst, src, key, ch):
        P.dma("pool", lambda e: e.dma_start(out=dst, in_=src), ch, writes=[key])

    def kcview(ap2d):
        return ap2d.rearrange("(kc p) n -> p kc n", p=128)

    P.pool(lambda e: e.memset(identf, 0.0), writes=["identf"])
    P.pool(lambda e: e.affine_select(out=identf, in_=identf, pattern=[[-1, 128]], compare_op=ALU.not_equal,
                                     fill=1.0, base=0, channel_multiplier=1), reads=["identf"], writes=["identf"])
    P.dve(lambda e: e.tensor_copy(identb, identf), reads=["identf"], writes=["identb"])
    P.dve(lambda e: e.memset(onesf, 1.0), writes=["onesf"])
    P.dma("sp", lambda e: e.dma_start(out=condT.rearrange("p j c -> p (j c)"), in_=condT_d), "cond", writes=["condT"])
    P.act(lambda e: e.activation(out=scb.rearrange("p c j -> p j c"), in_=condT, func=AF.Silu), reads=["condT"], writes=["scb"])
    P.barrier()

    def ln_tile(xt, yt, z, j, gi, dst_ap, dst_key, tag):
        P.dve(lambda e: e.tensor_tensor(out=yt, in0=yt, in1=grow[:, j, gi, :], op=ALU.mult), reads=[tag + "yt"], writes=[tag + "yt"])
        P.dve(lambda e: e.scalar_tensor_tensor(out=z, in0=xt, scalar=DN_ALPHA, in1=yt, op0=ALU.mult, op1=ALU.add),
              reads=[tag + "xt", tag + "yt"], writes=[tag + "z"])
        for q in range(2):
            P.dve(lambda e, q=q: e.bn_stats(out=st6[:, q, :], in_=z[:, q * 512:(q + 1) * 512]), reads=[tag + "z"], writes=["st6"])
        P.dve(lambda e: e.bn_aggr(out=mv[:, 0:2], in_=st6), reads=["st6"], writes=["mv"])
        P.dve(lambda e: e.tensor_scalar_add(out=mv[:, 2:3], in0=mv[:, 1:2], scalar1=1e-5), reads=["mv"], writes=["mv"])
        P.act(lambda e: e.sqrt(out=mv[:, 2:3], in_=mv[:, 2:3]), reads=["mv"], writes=["mv"])
        P.dve(lambda e: e.reciprocal(out=mv[:, 2:3], in_=mv[:, 2:3]), reads=["mv"], writes=["mv"])
        P.dve(lambda e: e.tensor_scalar(out=z, in0=z, scalar1=mv[:, 0:1], scalar2=mv[:, 2:3], op0=ALU.subtract, op1=ALU.mult),
              reads=[tag + "z", "mv"], writes=[tag + "z"])
        P.dve(lambda e: e.tensor_tensor(out=z, in0=z, in1=lng, op=ALU.mult), reads=[tag + "z", "lng"], writes=[tag + "z"])
        P.dve(lambda e: e.tensor_tensor(out=xt, in0=z, in1=lnb, op=ALU.add), reads=[tag + "z", tag + "xt", "lnb"], writes=[tag + "xt"])
        P.dma("sp", lambda e: e.dma_start(out=dst_ap, in_=xt), (tag + "st",), reads=[tag + "xt"], writes=[dst_key])

    def load_ln(l, i):
        P.dma("sp", lambda e: e.dma_start(out=lng, in_=ln_g[l, i, :].partition_broadcast(128)), "lng", writes=["lng"])
        P.dma("sp", lambda e: e.dma_start(out=lnb, in_=ln_b[l, i, :].partition_broadcast(128)), "lnb", writes=["lnb"])

    def stage_mod(l):
        A.reset(MARK)
        scB = A.a(2 * 8 * 128, BF16, "p (j c n) -> p j c n", j=2, c=8)
        biasrow = A.a(D, BF16)
        e0 = A.a(128, BF16)
        P.dve(lambda e: e.memset(biasrow, 0.0), writes=["biasrow"])
        P.dve(lambda e: e.memset(e0, 0.0), writes=["e0"])
        P.dve(lambda e: e.memset(e0[0:1, :], 1.0), reads=["e0"], writes=["e0"])
        for j in range(2):
            P.dve(lambda e, j=j: e.tensor_copy(scB[:, j, :, :], scb[:, :, j:j + 1].to_broadcast([128, 8, 128])), writes=["scB"])
        P.dma("sp", lambda e: e.dma_start(out=bmT, in_=b_modT[l]), "bmT", writes=["bmT"])
        for v in range(6):
            U, uk = unit()
            Uv = U.rearrange("p (c n) -> p c n", c=8)
            cast_load(Uv, kcview(w_mod[l][:, v * D:(v + 1) * D]), uk, ("ringd", uk[1]))
            if v in (0, 1, 3, 4):
                vi = {0: 0, 1: 1, 3: 2, 4: 3}[v]
                bank, bk = ps.one()
                for fc in range(8):
                    for kc in range(8):
                        P.pe(lambda e, fc=fc, kc=kc, bank=bank, Uv=Uv: e.matmul(bank[:, fc * 2:fc * 2 + 2], lhsT=Uv[:, kc, fc * 128:(fc + 1) * 128],
                                                                               rhs=scb[:, kc, :], start=(kc == 0), stop=(kc == 7)),
                             reads=[uk], writes=[bk])
                bv = bank[:, 0:16].rearrange("p (c j) -> p c j", j=2)
                for j in range(2):
                    P.dve(lambda e, j=j, bv=bv, vi=vi, v=v: e.scalar_tensor_tensor(out=modT[:, j, vi, :], in0=bv[:, :, j],
                                                                                  scalar=(1.0 if vi in (1, 3) else 0.0),
                                                                                  in1=bmT[:, v * 8:(v + 1) * 8], op0=ALU.add, op1=ALU.add),
                          reads=[bk, "bmT"], writes=["modT"])
            else:
                gi = 0 if v == 2 else 1
                P.dma("pool", lambda e, v=v: e.dma_start(out=biasrow[0:1, :], in_=b_mod[l:l + 1, v * D:(v + 1) * D]), "biasrow",
                      reads=["biasrow"], writes=["biasrow"])
                for j in range(2):
                    for half in range(2):
                        bank, bk = ps.one()
                        for kc in range(8):
                            P.pe(lambda e, j=j, kc=kc, half=half, bank=bank, Uv=Uv: e.matmul(bank, lhsT=scB[:, j, kc, :], rhs=Uv[:, kc, half * 512:(half + 1) * 512],
                                                                                            start=(kc == 0), stop=False),
                                 reads=[uk, "scB"], writes=[bk])
                        P.pe(lambda e, half=half, bank=bank: e.matmul(bank, lhsT=e0, rhs=biasrow[:, half * 512:(half + 1) * 512], start=False, stop=True),
                             reads=["e0", "biasrow"], writes=[bk])
                        P.act(lambda e, j=j, gi=gi, half=half, bank=bank: e.copy(out=grow[:, j, gi, half * 512:(half + 1) * 512], in_=bank),
                              reads=[bk], writes=["grow"])
        P.barrier()

    def stage_h(src, vsh, vsc, moe_l=None, G=None, tiles=None, hdst=None):
        m0 = A.mark()
        xts = [A.a(D) for _ in range(3)]
        if moe_l is not None:
            hTf = [A.a(8 * 128, F32, "p (c t) -> p c t", c=8) for _ in range(2)]
            wr = A.a(8 * 32, F32, "p (c n) -> p c n", c=8)
            br = A.a(32)
            lg = A.a(32)
            top8 = A.a(8)
            msk = A.a(32)
            ex = A.a(32)
            sm = A.a(8)
            e0f = A.a(128)
            P.dma("sp", lambda e: e.dma_start(out=wr, in_=kcview(moe_w_router[moe_l])), "wr", writes=["wr"])
            P.dve(lambda e: e.memset(br, 0.0), writes=["br"])
            P.dve(lambda e: e.memset(e0f, 0.0), writes=["e0f"])
            P.dve(lambda e: e.memset(e0f[0:1, :], 1.0), reads=["e0f"], writes=["e0f"])
            P.dma("sp", lambda e: e.dma_start(out=br[0:1, :], in_=moe_b_router[moe_l:moe_l + 1, :]), "br", reads=["br"], writes=["br"])
        if tiles is None:
            tiles = list(range(NT))
        if hdst is None:
            hdst = hT
        for li, tt in enumerate(tiles):
            j = 1 if tt < 16 else 0
            xt = xts[tt % 3]
            xk = ("hxt", tt % 3)
            P.dma("sp", lambda e, xt=xt, tt=tt: e.dma_start(out=xt, in_=src[tt * 128:(tt + 1) * 128, :]), ("hx", tt % 3), writes=[xk])
            for half in range(2):
                bank, bk = ps.one()
                for q in range(4):
                    fc = half * 4 + q
                    P.pe(lambda e, bank=bank, q=q, fc=fc, xt=xt: e.transpose(bank[:, q * 128:(q + 1) * 128], xt[:, fc * 128:(fc + 1) * 128], identf),
                         reads=[xk], writes=[bk])
                for q in range(4):
                    fc = half * 4 + q
                    dst = hdst[:, fc, li * 128:(li + 1) * 128]
                    srcp = bank[:, q * 128:(q + 1) * 128]
                    if moe_l is not None:
                        hf = hTf[tt % 2]
                        P.dve(lambda e: e.tensor_scalar(out=hf[:, fc, :], in0=srcp, scalar1=modT[:, j, vsc, fc:fc + 1],
                                                        scalar2=modT[:, j, vsh, fc:fc + 1], op0=ALU.mult, op1=ALU.add),
                              reads=[bk], writes=[("hTf", tt % 2, fc)])
                        P.act(lambda e: e.copy(out=dst, in_=hf[:, fc, :]), reads=[("hTf", tt % 2, fc)], writes=[("hT", tt)])
                    elif half == 0:
                        P.act(lambda e: e.activation(out=dst, in_=srcp, func=AF.Identity,
                                                     scale=modT[:, j, vsc, fc:fc + 1], bias=modT[:, j, vsh, fc:fc + 1]),
                              reads=[bk], writes=[("hT", tt)])
                    else:
                        P.dve(lambda e: e.tensor_scalar(out=dst, in0=srcp, scalar1=modT[:, j, vsc, fc:fc + 1],
                                                        scalar2=modT[:, j, vsh, fc:fc + 1], op0=ALU.mult, op1=ALU.add),
                              reads=[bk], writes=[("hT", tt)])
            if moe_l is not None:
                hf = hTf[tt % 2]
                hk = ("hTf", tt % 2)
                bank, bk = ps.one()
                for kc in range(8):
                    P.pe(lambda e, kc=kc, hf=hf, bank=bank: e.matmul(bank[:, 0:32], lhsT=hf[:, kc, :], rhs=wr[:, kc, :], start=(kc == 0), stop=False),
                         reads=[("hTf", tt % 2, kc), "wr"], writes=[bk])
                P.pe(lambda e, bank=bank: e.matmul(bank[:, 0:32], lhsT=e0f, rhs=br, start=False, stop=True),
                     reads=["br", "e0f"], writes=[bk])
                P.act(lambda e, bank=bank: e.copy(out=lg, in_=bank[:, 0:32]), reads=[bk], writes=["lg"])
                P.dve(lambda e: e.max(out=top8, in_=lg), reads=["lg"], writes=["top8"])
                P.dve(lambda e: e.tensor_scalar(out=msk, in0=lg, scalar1=top8[:, 3:4], scalar2=None, op0=ALU.is_ge), reads=["lg", "top8"], writes=["msk"])
                P.dve(lambda e: e.tensor_scalar(out=sm[:, 0:1], in0=top8[:, 0:1], scalar1=-1.0, scalar2=None, op0=ALU.mult), reads=["top8"], writes=["sm"])
                P.act(lambda e: e.activation(out=ex, in_=lg, func=AF.Exp, bias=sm[:, 0:1], scale=1.0), reads=["lg", "sm"], writes=["ex"])
                P.dve(lambda e: e.tensor_tensor(out=ex, in0=ex, in1=msk, op=ALU.mult), reads=["ex", "msk"], writes=["ex"])
                P.dve(lambda e: e.reduce_sum(out=sm[:, 1:2], in_=ex, axis=AX.X), reads=["ex"], writes=["sm2"])
                P.dve(lambda e: e.reciprocal(out=sm[:, 2:3], in_=sm[:, 1:2]), reads=["sm2"], writes=["sm3"])
                P.dve(lambda e, tt=tt: e.tensor_scalar(out=G[:, tt, :], in0=ex, scalar1=sm[:, 2:3], scalar2=None, op0=ALU.mult),
                      reads=["ex", "sm3"], writes=[("G", tt)])
        P.barrier()
        A.reset(m0)

    def stage_epi(l, i, src, dst, dst_name):
        m0 = A.mark()
        xts = [A.a(D) for _ in range(2)]
        yts = [A.a(D) for _ in range(2)]
        zs = [A.a(D) for _ in range(2)]
        load_ln(l, i)
        for tt in range(NT):
            j = 1 if tt < 16 else 0
            b = tt % 2
            tag = f"e{b}"
            P.dma("sp", lambda e, tt=tt, b=b: e.dma_start(out=xts[b], in_=src[tt * 128:(tt + 1) * 128, :]), (tag + "lx",), writes=[tag + "xt"])
            P.dma("sp", lambda e, tt=tt, b=b: e.dma_start(out=yts[b], in_=Y_d[tt * 128:(tt + 1) * 128, :]), (tag + "ly",), reads=[("Y", tt)], writes=[tag + "yt"])
            ln_tile(xts[b], yts[b], zs[b], j, i, dst[tt * 128:(tt + 1) * 128, :], (dst_name, tt), tag)
        P.barrier()
        A.reset(m0)

    def stage_moe(l, src, dst, dst_name):
        A.reset(MARK)
        G = A.a(NT * 32, F32, "p (t n) -> p t n", t=NT)
        hflat = hT.rearrange("p c t -> p (c t)")
        hTm = hflat[:, 0:10240].rearrange("p (c t) -> p c t", c=8)
        ring4 = hflat[:, 10240:18432]
        act2 = hflat[:, 18432:20480].rearrange("p (c t) -> p c t", c=8)
        rings = ring + [ring4]
        m1 = A.mark()
        acc = A.a(10 * D, F32, "p (t d) -> p t d", t=10)
        actT = [A.a(8 * 512, BF16, "p (c t) -> p c t", c=8) for _ in range(2)] + [act2]
        gc = A.a(512)
        sg = A.a(512)
        l1 = A.a(512)
        xt = A.a(D)
        z = A.a(D)
        bgu = A.a(512, F32, "p (e c) -> p e c", e=32)
        e0 = A.a(128, BF16)
        brows = [A.a(D, BF16) for _ in range(2)]
        m2 = A.mark()
        nexp = int(os.environ.get('MOE_NEXP', '32'))
        NTL = ((0, 512), (512, 512), (1024, 256))
        for p_ in range(2):
            A.reset(m1)
            stage_h(src, 2, 3, moe_l=l, G=G, tiles=list(range(p_ * 10, p_ * 10 + 10)), hdst=hTm)
            A.reset(m2)
            P.dve(lambda e: e.memset(e0, 0.0), writes=["e0"])
            P.dve(lambda e: e.memset(e0[0:1, :], 1.0), reads=["e0"], writes=["e0"])
            for q in range(2):
                P.dve(lambda e: e.memset(brows[q], 0.0), writes=[("brow", q)])
            load_ln(l, 1)
            P.dma("sp", lambda e: e.dma_start(out=bgu.rearrange("p e c -> p (e c)"), in_=moe_b_guT[l]), "bgu", writes=["bgu"])
            P.dve(lambda e: e.tensor_scalar_add(out=bgu[:, :, 8:16], in0=bgu[:, :, 8:16], scalar1=1.0), reads=["bgu"], writes=["bgu"])
            for ti in range(10):
                P.dve(lambda e: e.memset(acc[:, ti, :], 0.0), writes=[("acc", ti)])
            W = {}

            def load_expert(ex_):
                us = []
                for k in range(3):
                    i = (ex_ * 3 + k) % 5
                    us.append((rings[i].rearrange("p (c n) -> p c n", c=8), ("ring", i), ("ringd", i)))
                cast_load(us[0][0], kcview(moe_w_gu[l, ex_][:, 0:D]), us[0][1], us[0][2])
                cast_load(us[1][0], kcview(moe_w_gu[l, ex_][:, D:2 * D]), us[1][1], us[1][2])
                cast_load(us[2][0], kcview(moe_w_down[l, ex_]), us[2][1], us[2][2])
                brw = brows[ex_ % 2]
                bwk = ("brow", ex_ % 2)
                P.dma("pool", lambda e: e.dma_start(out=brw[0:1, :], in_=moe_b_down[l, ex_:ex_ + 1, :]), ("browd", ex_ % 2), reads=[bwk], writes=[bwk])
                W[ex_] = (us[0], us[1], us[2], brw, bwk)

            def emit_gu(ex_, nt):
                if nt == 0:
                    load_expert(ex_)
                (Ug, kg, _), (Ul, kl, _), _, _, _ = W[ex_]
                c0, n = NTL[nt]
                ab = actT[nt]
                ak = ("actT", nt)
                for fc in range(8):
                    bg, kbg = ps.one()
                    bl, kbl = ps.one()
                    for kc in range(8):
                        P.pe(lambda e: e.matmul(bg[:, 0:n], lhsT=Ug[:, kc, fc * 128:(fc + 1) * 128], rhs=hTm[:, kc, c0:c0 + n], start=(kc == 0), stop=(kc == 7)),
                             reads=[kg], writes=[kbg])
                    for kc in range(8):
                        P.pe(lambda e: e.matmul(bl[:, 0:n], lhsT=Ul[:, kc, fc * 128:(fc + 1) * 128], rhs=hTm[:, kc, c0:c0 + n], start=(kc == 0), stop=(kc == 7)),
                             reads=[kl], writes=[kbl])
                    P.dve(lambda e: e.tensor_scalar(out=gc[:, 0:n], in0=bg[:, 0:n], scalar1=bgu[:, ex_, fc:fc + 1], scalar2=7.0, op0=ALU.add, op1=ALU.min),
                          reads=[kbg, "bgu"], writes=["gc"])
                    P.act(lambda e: e.activation(out=sg[:, 0:n], in_=gc[:, 0:n], func=AF.Sigmoid, scale=1.702), reads=["gc"], writes=["sg"])
                    P.dve(lambda e: e.tensor_scalar(out=l1[:, 0:n], in0=bl[:, 0:n], scalar1=bgu[:, ex_, 8 + fc:9 + fc], scalar2=8.0, op0=ALU.add, op1=ALU.min),
                          reads=[kbl, "bgu"], writes=["l1"])
                    P.dve(lambda e: e.scalar_tensor_tensor(out=l1[:, 0:n], in0=l1[:, 0:n], scalar=-6.0, in1=gc[:, 0:n], op0=ALU.max, op1=ALU.mult),
                          reads=["l1", "gc"], writes=["l1"])
                    P.dve(lambda e: e.tensor_tensor(out=ab[:, fc, 0:n], in0=l1[:, 0:n], in1=sg[:, 0:n], op=ALU.mult), reads=["l1", "sg"], writes=[ak])

            def emit_down(ex_, nt):
                _, _, (Ud, kd, _), brw, bwk = W[ex_]
                c0, n = NTL[nt]
                ab = actT[nt]
                ak = ("actT", nt)
                for tl in range(n // 128):
                    ti = c0 // 128 + tl
                    tile = p_ * 10 + ti
                    for half in range(2):
                        by, kby = ps.one()
                        for fc in range(8):
                            P.pe(lambda e: e.matmul(by, lhsT=ab[:, fc, tl * 128:(tl + 1) * 128], rhs=Ud[:, fc, half * 512:(half + 1) * 512], start=(fc == 0), stop=False),
                                 reads=[ak, kd], writes=[kby])
                        P.pe(lambda e: e.matmul(by, lhsT=e0, rhs=brw[:, half * 512:(half + 1) * 512], start=False, stop=True), reads=["e0", bwk], writes=[kby])
                        P.dve(lambda e: e.scalar_tensor_tensor(out=acc[:, ti, half * 512:(half + 1) * 512], in0=by, scalar=G[:, tile, ex_:ex_ + 1],
                                                               in1=acc[:, ti, half * 512:(half + 1) * 512], op0=ALU.mult, op1=ALU.add),
                              reads=[kby, ("acc", ti)], writes=[("acc", ti)])

            items = [(ex_, nt) for ex_ in range(nexp) for nt in range(3)]
            prev = None
            for it in items:
                emit_gu(*it)
                if prev is not None:
                    emit_down(*prev)
                prev = it
            if prev is not None:
                emit_down(*prev)
            for ti in range(10):
                tile = p_ * 10 + ti
                j = 1 if tile < 16 else 0
                P.dma("sp", lambda e: e.dma_start(out=xt, in_=src[tile * 128:(tile + 1) * 128, :]), ("mlx",), writes=["mxt"])
                P.dve(lambda e: e.tensor_copy(acc[:, ti, 0:1], acc[:, ti, 0:1]), reads=[("acc", ti)], writes=["myt"])
                ln_tile(xt, acc[:, ti, :], z, j, 1, dst[tile * 128:(tile + 1) * 128, :], (dst_name, tile), "m")
            P.barrier()

    def stage_ret(l, ri):
        A.reset(MARK)
        dec = A.a(16)
        lgam = A.a(16)
        reli = A.a(128, I32)
        REL = A.a(128)
        DPOS = A.a(128)
        DNEG = A.a(128)
        MF = A.a(128)
        MB = A.a(128)
        J1 = A.a(128)
        JB = A.a(128)
        pci = A.a(8, I32)
        PC = A.a(8)
        Dm = A.a(128)
        DBt = A.a(128)
        XiF = A.a(128)
        XiB = A.a(128)
        zc = A.a(8)
        rcos = A.a(2048, BF16)
        rsin = A.a(2048, BF16)
        qT = A.a(2 * 2048, BF16, "p (c t) -> p c t", c=2)
        kT = A.a(2 * 2048, BF16, "p (c t) -> p c t", c=2)
        vv = A.a(16 * 512, BF16, "p (c n) -> p c n", c=16)
        Sf = A.a(1024, F32, "p (c n) -> p c n", c=2)
        Sb = A.a(1024, F32, "p (c n) -> p c n", c=2)
        Sfb = A.a(1024, BF16, "p (c n) -> p c n", c=2)
        x1 = A.a(512)
        x2 = A.a(512)
        ta = A.a(512)
        tb = A.a(512)
        kz = A.a(256, BF16)
        qf = A.a(256, BF16, "p (c n) -> p c n", c=2)
        qb = A.a(256, BF16, "p (c n) -> p c n", c=2)
        PT = A.a(128, BF16)
        sgt = A.a(512)
        on = A.a(512)
        og = A.a(512, BF16)
        ogT = A.a(512, BF16, "p (c n) -> p c n", c=4)
        yp = [A.a(D) for _ in range(2)]
        SbB = A.t[:, 0:1]
        SbB = ring[2]
        SbB0 = ring[2].rearrange("p (c d n) -> p c d n", c=8, d=2)
        SbB1 = ring[3].rearrange("p (c d n) -> p c d n", c=8, d=2)

        def sbb(c):
            return (SbB0 if c < 8 else SbB1)[:, c % 8, :, :]

        P.dma("sp", lambda e: e.dma_start(out=dec, in_=ret_decay.partition_broadcast(128)), "dec", writes=["dec"])
        P.act(lambda e: e.activation(out=lgam, in_=dec, func=AF.Exp, scale=-1.0), reads=["dec"], writes=["lgam"])
        P.act(lambda e: e.activation(out=lgam, in_=lgam, func=AF.Ln, bias=onesf[:, 0:1], scale=1.0), reads=["lgam"], writes=["lgam"])
        P.dve(lambda e: e.tensor_scalar(out=lgam, in0=lgam, scalar1=-1.0, scalar2=None, op0=ALU.mult), reads=["lgam"], writes=["lgam"])
        P.pool(lambda e: e.iota(reli, pattern=[[1, 128]], base=0, channel_multiplier=-1), writes=["reli"])
        P.dve(lambda e: e.tensor_copy(REL, reli), reads=["reli"], writes=["REL"])
        P.dve(lambda e: e.tensor_scalar(out=DPOS, in0=REL, scalar1=0.0, scalar2=None, op0=ALU.max), reads=["REL"], writes=["DPOS"])
        P.dve(lambda e: e.tensor_scalar(out=DNEG, in0=REL, scalar1=-1.0, scalar2=0.0, op0=ALU.mult, op1=ALU.max), reads=["REL"], writes=["DNEG"])
        P.dve(lambda e: e.tensor_scalar(out=MF, in0=REL, scalar1=0.0, scalar2=None, op0=ALU.is_ge), reads=["REL"], writes=["MF"])
        P.dve(lambda e: e.tensor_scalar(out=MB, in0=REL, scalar1=0.0, scalar2=None, op0=ALU.is_le), reads=["REL"], writes=["MB"])
        P.pool(lambda e: e.iota(reli, pattern=[[1, 128]], base=1, channel_multiplier=0), reads=["REL"], writes=["reli"])
        P.dve(lambda e: e.tensor_copy(J1, reli), reads=["reli"], writes=["J1"])
        P.dve(lambda e: e.tensor_scalar(out=JB, in0=J1, scalar1=-1.0, scalar2=129.0, op0=ALU.mult, op1=ALU.add), reads=["J1"], writes=["JB"])
        P.pool(lambda e: e.iota(pci[:, 0:1], pattern=[[0, 1]], base=0, channel_multiplier=1), writes=["pci"])
        P.dve(lambda e: e.tensor_copy(PC[:, 0:1], pci[:, 0:1]), reads=["pci"], writes=["PC"])
        P.dve(lambda e: e.tensor_scalar(out=PC[:, 1:2], in0=PC[:, 0:1], scalar1=-1.0, scalar2=127.0, op0=ALU.mult, op1=ALU.add), reads=["PC"], writes=["PC"])
        P.dve(lambda e: e.memset(PC[:, 2:3], 128.0), reads=["PC"], writes=["PC"])
        P.dma("pool", lambda e: e.dma_start(out=rcos, in_=rot_cos), "rcos", writes=["rcos"])
        P.dma("pool", lambda e: e.dma_start(out=rsin, in_=rot_sin), "rsin", writes=["rsin"])

        for h in range(4):
            UA, ka = ring[0], ("ring", 0)
            UB, kb = ring[1], ("ring", 1)
            UAv = UA.rearrange("p (c n) -> p c n", c=8)
            UBg = UB[:, 0:4096].rearrange("p (c n) -> p c n", c=8)
            UBo = UB[:, 4096:8192].rearrange("p (c n) -> p c n", c=4)
            wi = ret_w_in[ri]
            cast_load(UAv[:, :, 0:256], kcview(wi[:, h * 256:(h + 1) * 256]), ka, ("ringd", 0))
            cast_load(UAv[:, :, 256:512], kcview(wi[:, D + h * 256:D + (h + 1) * 256]), ka, ("ringd", 0))
            cast_load(UAv[:, :, 512:1024], kcview(wi[:, 2 * D + h * 512:2 * D + (h + 1) * 512]), ka, ("ringd", 0))
            cast_load(UBg, kcview(wi[:, 4 * D + h * 512:4 * D + (h + 1) * 512]), kb, ("ringd", 1))
            cast_load(UBo, kcview(ret_w_out[ri][h * 512:(h + 1) * 512, :]), kb, ("ringd", 1))
            lf = lgam[:, ri * 8 + h:ri * 8 + h + 1]
            lb = lgam[:, ri * 8 + 4 + h:ri * 8 + 4 + h + 1]
            P.act(lambda e, lf=lf: e.activation(out=Dm, in_=DPOS, func=AF.Exp, scale=lf), reads=["DPOS", "lgam"], writes=["Dm"])
            P.dve(lambda e: e.tensor_tensor(out=Dm, in0=Dm, in1=MF, op=ALU.mult), reads=["Dm", "MF"], writes=["Dm"])
            P.act(lambda e, lb=lb: e.activation(out=DBt, in_=DNEG, func=AF.Exp, scale=lb), reads=["DNEG", "lgam"], writes=["DBt"])
            P.dve(lambda e: e.tensor_tensor(out=DBt, in0=DBt, in1=MB, op=ALU.mult), reads=["DBt", "MB"], writes=["DBt"])
            P.dve(lambda e: e.tensor_tensor(out=Dm, in0=Dm, in1=DBt, op=ALU.add), reads=["Dm", "DBt"], writes=["Dm"])
            P.act(lambda e, lf=lf: e.activation(out=XiF, in_=J1, func=AF.Exp, scale=lf), reads=["J1", "lgam"], writes=["XiF"])
            P.act(lambda e, lb=lb: e.activation(out=XiB, in_=JB, func=AF.Exp, scale=lb), reads=["JB", "lgam"], writes=["XiB"])
            P.act(lambda e, lf=lf: e.activation(out=zc[:, 0:1], in_=PC[:, 1:2], func=AF.Exp, scale=lf), reads=["PC", "lgam"], writes=["zc"])
            P.act(lambda e, lb=lb: e.activation(out=zc[:, 1:2], in_=PC[:, 0:1], func=AF.Exp, scale=lb), reads=["PC", "lgam"], writes=["zc"])
            P.act(lambda e, lf=lf: e.activation(out=zc[:, 2:3], in_=PC[:, 2:3], func=AF.Exp, scale=lf), reads=["PC", "lgam"], writes=["zc"])
            P.act(lambda e, lb=lb: e.activation(out=zc[:, 3:4], in_=PC[:, 2:3], func=AF.Exp, scale=lb), reads=["PC", "lgam"], writes=["zc"])
            for si, (t0, nch, j, samp) in enumerate(SEQS):
                L = nch * 128
                tok0 = t0 * 128
                for c0 in range(0, L, 512):
                    n = min(512, L - c0)
                    for (dstT, colb, scl, nm) in ((qT, 0, 1.0 / 16.0, "qT"), (kT, 256, 1.0, "kT")):
                        banks = []
                        for dc in range(2):
                            bank, bk = ps.one()
                            banks.append((bank, bk))
                            for kc in range(8):
                                P.pe(lambda e, kc=kc, dc=dc, bank=bank, colb=colb, c0=c0, n=n: e.matmul(
                                    bank[:, 0:n], lhsT=UAv[:, kc, colb + dc * 128:colb + (dc + 1) * 128], rhs=hT[:, kc, tok0 + c0:tok0 + c0 + n],
                                    start=(kc == 0), stop=(kc == 7)), reads=[ka], writes=[bk])
                        if samp:
                            P.act(lambda e, n=n, scl=scl, b=banks[0][0]: e.activation(out=x1[:, 0:n], in_=b[:, 0:n], func=AF.Identity, scale=scl), reads=[banks[0][1]], writes=["x1"])
                            P.act(lambda e, n=n, scl=scl, b=banks[1][0]: e.activation(out=x2[:, 0:n], in_=b[:, 0:n], func=AF.Identity, scale=scl), reads=[banks[1][1]], writes=["x2"])
                            cs = rcos[:, c0:c0 + n]
                            sn = rsin[:, c0:c0 + n]
                            P.dve(lambda e, n=n, cs=cs: e.tensor_tensor(out=ta[:, 0:n], in0=x1[:, 0:n], in1=cs, op=ALU.mult), reads=["x1", "rcos"], writes=["ta"])
                            P.dve(lambda e, n=n, sn=sn: e.tensor_tensor(out=tb[:, 0:n], in0=x2[:, 0:n], in1=sn, op=ALU.mult), reads=["x2", "rsin"], writes=["tb"])
                            P.dve(lambda e, n=n, c0=c0, dstT=dstT: e.tensor_tensor(out=dstT[:, 0, c0:c0 + n], in0=ta[:, 0:n], in1=tb[:, 0:n], op=ALU.subtract),
                                  reads=["ta", "tb"], writes=[nm])
                            P.dve(lambda e, n=n, sn=sn: e.tensor_tensor(out=ta[:, 0:n], in0=x1[:, 0:n], in1=sn, op=ALU.mult), reads=["x1", "rsin"], writes=["ta"])
                            P.dve(lambda e, n=n, cs=cs: e.tensor_tensor(out=tb[:, 0:n], in0=x2[:, 0:n], in1=cs, op=ALU.mult), reads=["x2", "rcos"], writes=["tb"])
                            P.dve(lambda e, n=n, c0=c0, dstT=dstT: e.tensor_tensor(out=dstT[:, 1, c0:c0 + n], in0=ta[:, 0:n], in1=tb[:, 0:n], op=ALU.add),
                                  reads=["ta", "tb"], writes=[nm])
                        else:
                            for dc in range(2):
                                P.act(lambda e, n=n, scl=scl, b=banks[dc][0], dc=dc, c0=c0, dstT=dstT: e.activation(out=dstT[:, dc, c0:c0 + n], in_=b[:, 0:n], func=AF.Identity, scale=scl),
                                      reads=[banks[dc][1]], writes=[nm])
                for c in range(nch):
                    bank, bk = ps.one()
                    for kc in range(8):
                        P.pe(lambda e, kc=kc, c=c, bank=bank: e.matmul(bank, lhsT=hT[:, kc, tok0 + c * 128:tok0 + (c + 1) * 128], rhs=UAv[:, kc, 512:1024],
                                                                      start=(kc == 0), stop=(kc == 7)), reads=[ka], writes=[bk])
                    P.act(lambda e, c=c, bank=bank: e.copy(out=vv[:, c, :], in_=bank), reads=[bk], writes=[("vv", c)])
                for (S, d, nm) in ((Sf, 0, "Sf"), (Sb, 1, "Sb")):
                    if samp:
                        P.dma("sp", lambda e, S=S, d=d: e.dma_start(out=S, in_=state_ret[ri, d, h].rearrange("(c p) n -> p c n", p=128)), ("ld" + nm,), writes=[nm])
                    else:
                        P.dve(lambda e, S=S: e.memset(S, 0.0), writes=[nm])

                def ktok(c, zcol, nm):
                    bankT, bkT = ps.one()
                    bTb = bankT.bitcast(BF16)
                    for dc in range(2):
                        P.pe(lambda e, dc=dc, c=c, bTb=bTb: e.transpose(bTb[:, dc * 128:(dc + 1) * 128], kT[:, dc, c * 128:(c + 1) * 128], identb),
                             reads=["kT"], writes=[bkT])
                    P.dve(lambda e, bTb=bTb, zcol=zcol: e.tensor_scalar(out=kz, in0=bTb[:, 0:256], scalar1=zc[:, zcol:zcol + 1], scalar2=None, op0=ALU.mult),
                          reads=[bkT, "zc"], writes=["kz"])

                def supd(S, nm, c, gcol):
                    for dc in range(2):
                        bankA, bkA = ps.one()
                        P.pe(lambda e, dc=dc, c=c, bankA=bankA: e.matmul(bankA, lhsT=kz[:, dc * 128:(dc + 1) * 128], rhs=vv[:, c, :], start=True, stop=True),
                             reads=["kz", ("vv", c)], writes=[bkA])
                        P.dve(lambda e, dc=dc, bankA=bankA, S=S, gcol=gcol: e.scalar_tensor_tensor(out=S[:, dc, :], in0=S[:, dc, :], scalar=zc[:, gcol:gcol + 1],
                                                                                                 in1=bankA, op0=ALU.mult, op1=ALU.add), reads=[bkA, nm, "zc"], writes=[nm])

                for c in reversed(range(nch)):
                    P.act(lambda e, c=c: e.copy(out=sbb(c), in_=Sb), reads=["Sb"], writes=[("sbb", c)])
                    ktok(c, 1, "kzb")
                    supd(Sb, "Sb", c, 3)
                if not samp:
                    P.dma("sp", lambda e, si=si: e.dma_start(out=o_ret[si - 1, ri, 1, h].rearrange("(c p) n -> p c n", p=128), in_=Sb), "oret",
                          reads=["Sb"], writes=[("o_ret", si, ri, 1, h)])
                    final_keys.append(("o_ret", si, ri, 1, h))
                for c in range(nch):
                    tile = t0 + c
                    P.act(lambda e: e.copy(out=Sfb, in_=Sf), reads=["Sf"], writes=["Sfb"])
                    P.dve(lambda e, c=c: e.tensor_tensor(out=qf, in0=qT[:, :, c * 128:(c + 1) * 128], in1=XiF.unsqueeze(1).to_broadcast([128, 2, 128]), op=ALU.mult),
                          reads=["qT", "XiF"], writes=["qf"])
                    P.dve(lambda e, c=c: e.tensor_tensor(out=qb, in0=qT[:, :, c * 128:(c + 1) * 128], in1=XiB.unsqueeze(1).to_broadcast([128, 2, 128]), op=ALU.mult),
                          reads=["qT", "XiB"], writes=["qb"])
                    bankS, bkS = ps.one()
                    for dc in range(2):
                        P.pe(lambda e, dc=dc, c=c, bankS=bankS: e.matmul(bankS[:, 0:128], lhsT=kT[:, dc, c * 128:(c + 1) * 128], rhs=qT[:, dc, c * 128:(c + 1) * 128],
                                                                        start=(dc == 0), stop=(dc == 1)), reads=["kT", "qT"], writes=[bkS])
                    P.dve(lambda e, bankS=bankS: e.tensor_tensor(out=PT, in0=bankS[:, 0:128], in1=Dm, op=ALU.mult), reads=[bkS, "Dm"], writes=["PT"])
                    bankO, bkO = ps.one()
                    P.pe(lambda e, c=c, bankO=bankO: e.matmul(bankO, lhsT=PT, rhs=vv[:, c, :], start=True, stop=False), reads=["PT", ("vv", c)], writes=[bkO])
                    for dc in range(2):
                        P.pe(lambda e, dc=dc, bankO=bankO: e.matmul(bankO, lhsT=qf[:, dc, :], rhs=Sfb[:, dc, :], start=False, stop=False), reads=["qf", "Sfb"], writes=[bkO])
                    for dc in range(2):
                        P.pe(lambda e, dc=dc, c=c, bankO=bankO: e.matmul(bankO, lhsT=qb[:, dc, :], rhs=sbb(c)[:, dc, :], start=False, stop=(dc == 1)),
                             reads=["qb", ("sbb", c)], writes=[bkO])
                    bankG, bkG = ps.one()
                    for kc in range(8):
                        P.pe(lambda e, kc=kc, c=c, bankG=bankG: e.matmul(bankG, lhsT=hT[:, kc, tok0 + c * 128:tok0 + (c + 1) * 128], rhs=UBg[:, kc, :],
                                                                        start=(kc == 0), stop=(kc == 7)), reads=[kb], writes=[bkG])
                    P.act(lambda e, bankG=bankG: e.activation(out=sgt, in_=bankG, func=AF.Silu), reads=[bkG], writes=["sgt"])
                    P.dve(lambda e, bankO=bankO: e.bn_stats(out=st6[:, 0, :], in_=bankO), reads=[bkO], writes=["st6"])
                    P.dve(lambda e: e.bn_aggr(out=mv[:, 0:2], in_=st6[:, 0:1, :]), reads=["st6"], writes=["mv"])
                    P.dve(lambda e: e.tensor_scalar_add(out=mv[:, 2:3], in0=mv[:, 1:2], scalar1=1e-5), reads=["mv"], writes=["mv"])
                    P.act(lambda e: e.sqrt(out=mv[:, 2:3], in_=mv[:, 2:3]), reads=["mv"], writes=["mv"])
                    P.dve(lambda e: e.reciprocal(out=mv[:, 2:3], in_=mv[:, 2:3]), reads=["mv"], writes=["mv"])
                    P.dve(lambda e, bankO=bankO: e.tensor_scalar(out=on, in0=bankO, scalar1=mv[:, 0:1], scalar2=mv[:, 2:3], op0=ALU.subtract, op1=ALU.mult),
                          reads=[bkO, "mv"], writes=["on"])
                    P.dve(lambda e: e.tensor_tensor(out=og, in0=on, in1=sgt, op=ALU.mult), reads=["on", "sgt"], writes=["og"])
                    bankT, bkT = ps.one()
                    bTb = bankT.bitcast(BF16)
                    for q in range(4):
                        P.pe(lambda e, q=q, bTb=bTb: e.transpose(bTb[:, q * 128:(q + 1) * 128], og[:, q * 128:(q + 1) * 128], identb), reads=["og"], writes=[bkT])
                    P.act(lambda e, bTb=bTb: e.copy(out=ogT.rearrange("p c n -> p (c n)"), in_=bTb[:, 0:512]), reads=[bkT], writes=["ogT"])
                    b2, bk2 = ps.two()
                    for half in range(2):
                        for q in range(4):
                            P.pe(lambda e, q=q, half=half, b2=b2: e.matmul(b2[:, half, :], lhsT=ogT[:, q, :], rhs=UBo[:, q, half * 512:(half + 1) * 512],
                                                                          start=(q == 0), stop=(q == 3)), reads=["ogT", kb], writes=[bk2[half]])
                    ypt = yp[tile % 2]
                    ypk = ("yp", tile % 2)
                    P.act(lambda e, b2=b2, ypt=ypt: e.copy(out=ypt.rearrange("p (h n) -> p h n", h=2), in_=b2), reads=bk2, writes=[ypk])
                    if h == 0:
                        P.dma("sp", lambda e, ypt=ypt, tile=tile: e.dma_start(out=Y_d[tile * 128:(tile + 1) * 128, :], in_=ypt), ("ypds", tile % 2),
                              reads=[ypk], writes=[("Y", tile)])
                    else:
                        P.dma("pool", lambda e, ypt=ypt, tile=tile: e.dma_start(out=Y_d[tile * 128:(tile + 1) * 128, :], in_=ypt, accum_op=ALU.add), ("ypdp", tile % 2),
                              reads=[ypk, ("Y", tile)], writes=[("Y", tile)])
                    ktok(c, 0, "kzf")
                    supd(Sf, "Sf", c, 2)
                if not samp:
                    P.dma("sp", lambda e, si=si: e.dma_start(out=o_ret[si - 1, ri, 0, h].rearrange("(c p) n -> p c n", p=128), in_=Sf), "oret",
                          reads=["Sf"], writes=[("o_ret", si, ri, 0, h)])
                    final_keys.append(("o_ret", si, ri, 0, h))
        P.barrier()

    def stage_gla(l):
        A.reset(MARK)
        reli = A.a(128, I32)
        REL = A.a(128)
        MF = A.a(128)
        MB = A.a(128)
        TriF = A.a(128)
        TriB = A.a(128)
        wa1 = A.a(2 * 8 * 16, BF16, "p (d c n) -> p d c n", d=2, c=8)
        wa2 = A.a(2 * 512, F32, "p (d n) -> p d n", d=2)
        tTa = A.a(2 * 128, F32, "p (d n) -> p d n", d=2)
        lap = A.a(2 * 128, F32, "p (d n) -> p d n", d=2)
        Eq = A.a(2 * 128, F32, "p (d n) -> p d n", d=2)
        Ek = A.a(2 * 128, F32, "p (d n) -> p d n", d=2)
        qT = A.a(2048, BF16)
        kT = A.a(2048, BF16)
        qfT = A.a(2048, BF16)
        kfT = A.a(2048, BF16)
        qbT = A.a(2048, BF16)
        kbT = A.a(2048, BF16)
        ElF = A.a(16)
        ElB = A.a(16)
        vv = A.a(16 * 256, BF16, "p (c n) -> p c n", c=16)
        rs_ = A.a(16 * 256, BF16, "p (c n) -> p c n", c=16)
        SbB = A.a(16 * 256, BF16, "p (c n) -> p c n", c=16)
        Sf = A.a(256)
        Sb = A.a(256)
        Sfb = A.a(256, BF16)
        kt = A.a(128, BF16)
        sa = A.a(128)
        sbm = A.a(128)
        PT = A.a(128, BF16)
        on = A.a(256)
        og = A.a(256, BF16)
        ogT = A.a(256, BF16, "p (c n) -> p c n", c=2)
        yp = [A.a(D) for _ in range(2)]
        P.pool(lambda e: e.iota(reli, pattern=[[1, 128]], base=0, channel_multiplier=-1), writes=["reli"])
        P.dve(lambda e: e.tensor_copy(REL, reli), reads=["reli"], writes=["REL"])
        P.dve(lambda e: e.tensor_scalar(out=MF, in0=REL, scalar1=0.0, scalar2=None, op0=ALU.is_ge), reads=["REL"], writes=["MF"])
        P.dve(lambda e: e.tensor_scalar(out=MB, in0=REL, scalar1=0.0, scalar2=None, op0=ALU.is_le), reads=["REL"], writes=["MB"])
        P.dve(lambda e: e.tensor_scalar(out=TriF, in0=MF, scalar1=-1.0 / 16.0, scalar2=None, op0=ALU.mult), reads=["MF"], writes=["TriF"])
        P.dve(lambda e: e.tensor_scalar(out=TriB, in0=MB, scalar1=-1.0 / 16.0, scalar2=None, op0=ALU.mult), reads=["MB"], writes=["TriB"])
        for d in range(2):
            P.dma("pool", lambda e, d=d: e.dma_start(out=wa1[:, d, :, :], in_=kcview(gla_w_a1[d])), ("wa1", d), writes=["wa1"])
            P.dma("sp", lambda e, d=d: e.dma_start(out=wa2[0:17, d, :], in_=gla_w_a2a[d]), ("wa2", d), writes=["wa2"])
        P.dve(lambda e: e.memset(tTa[0:32, :, :], 1.0), writes=["tTa"])
        for h in range(4):
            U, uk = unit()
            Uin = U[:, 0:6144].rearrange("p (c n) -> p c n", c=8)
            Uo = U[:, 6144:8192].rearrange("p (c n) -> p c n", c=2)
            ch = ("ringd", uk[1])
            cast_load(Uin[:, :, 0:128], kcview(gla_w_in[:, h * 128:(h + 1) * 128]), uk, ch)
            cast_load(Uin[:, :, 128:256], kcview(gla_w_in[:, 512 + h * 128:512 + (h + 1) * 128]), uk, ch)
            cast_load(Uin[:, :, 256:512], kcview(gla_w_in[:, 1024 + h * 256:1024 + (h + 1) * 256]), uk, ch)
            cast_load(Uin[:, :, 512:768], kcview(gla_w_in[:, 2048 + h * 256:2048 + (h + 1) * 256]), uk, ch)
            cast_load(Uo, kcview(gla_w_out[h * 256:(h + 1) * 256, :]), uk, ch)
            for si, (t0, nch, j, samp) in enumerate(SEQS):
                L = nch * 128
                tok0 = t0 * 128
                for c0 in range(0, L, 512):
                    n = min(512, L - c0)
                    for (dstT, colb, scl, nm) in ((qT, 0, 128.0 ** -0.5, "qT"), (kT, 128, 1.0, "kT")):
                        bank, bk = ps.one()
                        for kc in range(8):
                            P.pe(lambda e, kc=kc, bank=bank, colb=colb, c0=c0, n=n: e.matmul(bank[:, 0:n], lhsT=Uin[:, kc, colb:colb + 128],
                                                                                            rhs=hT[:, kc, tok0 + c0:tok0 + c0 + n], start=(kc == 0), stop=(kc == 7)),
                                 reads=[uk], writes=[bk])
                        P.act(lambda e, n=n, scl=scl, bank=bank, c0=c0, dstT=dstT: e.activation(out=dstT[:, c0:c0 + n], in_=bank[:, 0:n], func=AF.Identity, scale=scl),
                              reads=[bk], writes=[nm])
                for c in range(nch):
                    tk = slice(tok0 + c * 128, tok0 + (c + 1) * 128)
                    bank, bk = ps.one()
                    for kc in range(8):
                        P.pe(lambda e, kc=kc, bank=bank, tk=tk: e.matmul(bank, lhsT=hT[:, kc, tk], rhs=Uin[:, kc, 256:768], start=(kc == 0), stop=(kc == 7)),
                             reads=[uk], writes=[bk])
                    P.act(lambda e, c=c, bank=bank: e.copy(out=vv[:, c, :], in_=bank[:, 0:256]), reads=[bk], writes=[("vv", c)])
                    P.act(lambda e, c=c, bank=bank: e.activation(out=rs_[:, c, :], in_=bank[:, 256:512], func=AF.Silu), reads=[bk], writes=[("rs", c)])
                    bt, bkt = ps.one()
                    for d in range(2):
                        for kc in range(8):
                            P.pe(lambda e, kc=kc, d=d, bt=bt, tk=tk: e.matmul(bt[0:16, d * 128:(d + 1) * 128], lhsT=wa1[:, d, kc, :], rhs=hT[:, kc, tk],
                                                                            start=(kc == 0), stop=(kc == 7)), reads=["wa1"], writes=[bkt])
                    P.dve(lambda e, bt=bt: e.tensor_copy(tTa[0:16, :, :].rearrange("p d n -> p (d n)"), bt[0:16, 0:256]), reads=[bkt], writes=["tTa"])
                    bz, bkz = ps.one()
                    for d in range(2):
                        P.pe(lambda e, d=d, bz=bz: e.matmul(bz[:, d * 128:(d + 1) * 128], lhsT=tTa[0:17, d, :], rhs=wa2[0:17, d, h * 128:(h + 1) * 128],
                                                           start=True, stop=True), reads=["tTa", "wa2"], writes=[bkz])
                    P.act(lambda e, bz=bz: e.activation(out=lap.rearrange("p d n -> p (d n)"), in_=bz[:, 0:256], func=AF.Exp, scale=-1.0), reads=[bkz], writes=["lap"])
                    P.act(lambda e: e.activation(out=lap, in_=lap, func=AF.Ln, bias=onesf[:, 0:1], scale=1.0), reads=["lap"], writes=["lap"])
                    bc, bkc = ps.one()
                    P.pe(lambda e, bc=bc: e.matmul(bc[:, 0:128], lhsT=lap[:, 0, :], rhs=TriF, start=True, stop=True), reads=["lap", "TriF"], writes=[bkc])
                    P.pe(lambda e, bc=bc: e.matmul(bc[:, 128:256], lhsT=lap[:, 1, :], rhs=TriB, start=True, stop=True), reads=["lap", "TriB"], writes=[bkc])
                    P.act(lambda e, bc=bc: e.activation(out=Eq.rearrange("p d n -> p (d n)"), in_=bc[:, 0:256], func=AF.Exp), reads=[bkc], writes=["Eq"])
                    P.act(lambda e, bc=bc: e.activation(out=Ek.rearrange("p d n -> p (d n)"), in_=bc[:, 0:256], func=AF.Exp, scale=-1.0), reads=[bkc], writes=["Ek"])
                    cs = slice(c * 128, (c + 1) * 128)
                    P.dve(lambda e, cs=cs: e.tensor_tensor(out=qfT[:, cs], in0=qT[:, cs], in1=Eq[:, 0, :], op=ALU.mult), reads=["qT", "Eq"], writes=["qfT"])
                    P.dve(lambda e, cs=cs: e.tensor_tensor(out=kfT[:, cs], in0=kT[:, cs], in1=Ek[:, 0, :], op=ALU.mult), reads=["kT", "Ek"], writes=["kfT"])
                    P.dve(lambda e, cs=cs: e.tensor_tensor(out=qbT[:, cs], in0=qT[:, cs], in1=Eq[:, 1, :], op=ALU.mult), reads=["qT", "Eq"], writes=["qbT"])
                    P.dve(lambda e, cs=cs: e.tensor_tensor(out=kbT[:, cs], in0=kT[:, cs], in1=Ek[:, 1, :], op=ALU.mult), reads=["kT", "Ek"], writes=["kbT"])
                    P.dve(lambda e, c=c: e.tensor_copy(ElF[:, c:c + 1], Eq[:, 0, 127:128]), reads=["Eq"], writes=["ElF"])
                    P.dve(lambda e, c=c: e.tensor_copy(ElB[:, c:c + 1], Eq[:, 1, 0:1]), reads=["Eq"], writes=["ElB"])
                for (S, d, nm) in ((Sf, 0, "Sf"), (Sb, 1, "Sb")):
                    if samp:
                        P.dma("sp", lambda e, S=S, d=d: e.dma_start(out=S, in_=state_gla[d, h]), ("ld" + nm,), writes=[nm])
                    else:
                        P.dve(lambda e, S=S: e.memset(S, 0.0), writes=[nm])

                def supd(S, nm, kxT, kxn, c, El):
                    bankT, bkT = ps.one()
                    bTb = bankT.bitcast(BF16)
                    P.pe(lambda e, bTb=bTb, c=c: e.transpose(bTb[:, 0:128], kxT[:, c * 128:(c + 1) * 128], identb), reads=[kxn], writes=[bkT])
                    P.act(lambda e, bTb=bTb: e.copy(out=kt, in_=bTb[:, 0:128]), reads=[bkT], writes=["kt"])
                    bankA, bkA = ps.one()
                    P.pe(lambda e, bankA=bankA, c=c: e.matmul(bankA[:, 0:256], lhsT=kt, rhs=vv[:, c, :], start=True, stop=True), reads=["kt", ("vv", c)], writes=[bkA])
                    P.dve(lambda e, bankA=bankA: e.tensor_tensor(out=S, in0=S, in1=bankA[:, 0:256], op=ALU.add), reads=[bkA, nm], writes=[nm])
                    P.dve(lambda e, c=c: e.tensor_scalar(out=S, in0=S, scalar1=El[:, c:c + 1], scalar2=None, op0=ALU.mult), reads=[nm, "ElF", "ElB"], writes=[nm])

                for c in reversed(range(nch)):
                    P.act(lambda e, c=c: e.copy(out=SbB[:, c, :], in_=Sb), reads=["Sb"], writes=[("sbb", c)])
                    supd(Sb, "Sb", kbT, "kbT", c, ElB)
                if not samp:
                    P.dma("sp", lambda e, si=si: e.dma_start(out=o_gla[si - 1, 1, h], in_=Sb), "ogla", reads=["Sb"], writes=[("o_gla", si, 1, h)])
                    final_keys.append(("o_gla", si, 1, h))
                for c in range(nch):
                    tile = t0 + c
                    cs = slice(c * 128, (c + 1) * 128)
                    P.act(lambda e: e.copy(out=Sfb, in_=Sf), reads=["Sf"], writes=["Sfb"])
                    bF, bkF = ps.one()
                    P.pe(lambda e, bF=bF, cs=cs: e.matmul(bF[:, 0:128], lhsT=kfT[:, cs], rhs=qfT[:, cs], start=True, stop=True), reads=["kfT", "qfT"], writes=[bkF])
                    P.pe(lambda e, bF=bF, cs=cs: e.matmul(bF[:, 128:256], lhsT=kbT[:, cs], rhs=qbT[:, cs], start=True, stop=True), reads=["kbT", "qbT"], writes=[bkF])
                    P.dve(lambda e, bF=bF: e.tensor_tensor(out=sa, in0=bF[:, 0:128], in1=MF, op=ALU.mult), reads=[bkF, "MF"], writes=["sa"])
                    P.dve(lambda e, bF=bF: e.tensor_tensor(out=sbm, in0=bF[:, 128:256], in1=MB, op=ALU.mult), reads=[bkF, "MB"], writes=["sbm"])
                    P.dve(lambda e: e.tensor_tensor(out=PT, in0=sa, in1=sbm, op=ALU.add), reads=["sa", "sbm"], writes=["PT"])
                    bO, bkO = ps.one()
                    P.pe(lambda e, bO=bO, c=c: e.matmul(bO[:, 0:256], lhsT=PT, rhs=vv[:, c, :], start=True, stop=False), reads=["PT", ("vv", c)], writes=[bkO])
                    P.pe(lambda e, bO=bO, cs=cs: e.matmul(bO[:, 0:256], lhsT=qfT[:, cs], rhs=Sfb, start=False, stop=False), reads=["qfT", "Sfb"], writes=[bkO])
                    P.pe(lambda e, bO=bO, cs=cs, c=c: e.matmul(bO[:, 0:256], lhsT=qbT[:, cs], rhs=SbB[:, c, :], start=False, stop=True), reads=["qbT", ("sbb", c)], writes=[bkO])
                    P.dve(lambda e, bO=bO: e.bn_stats(out=st6[:, 0, :], in_=bO[:, 0:256]), reads=[bkO], writes=["st6"])
                    P.dve(lambda e: e.bn_aggr(out=mv[:, 0:2], in_=st6[:, 0:1, :]), reads=["st6"], writes=["mv"])
                    P.dve(lambda e: e.tensor_scalar_add(out=mv[:, 2:3], in0=mv[:, 1:2], scalar1=1e-5), reads=["mv"], writes=["mv"])
                    P.act(lambda e: e.sqrt(out=mv[:, 2:3], in_=mv[:, 2:3]), reads=["mv"], writes=["mv"])
                    P.dve(lambda e: e.reciprocal(out=mv[:, 2:3], in_=mv[:, 2:3]), reads=["mv"], writes=["mv"])
                    P.dve(lambda e, bO=bO: e.tensor_scalar(out=on, in0=bO[:, 0:256], scalar1=mv[:, 0:1], scalar2=mv[:, 2:3], op0=ALU.subtract, op1=ALU.mult),
                          reads=[bkO, "mv"], writes=["on"])
                    P.dve(lambda e, c=c: e.tensor_tensor(out=og, in0=on, in1=rs_[:, c, :], op=ALU.mult), reads=["on", ("rs", c)], writes=["og"])
                    bankT, bkT = ps.one()
                    bTb = bankT.bitcast(BF16)
                    for q in range(2):
                        P.pe(lambda e, q=q, bTb=bTb: e.transpose(bTb[:, q * 128:(q + 1) * 128], og[:, q * 128:(q + 1) * 128], identb), reads=["og"], writes=[bkT])
                    P.act(lambda e, bTb=bTb: e.copy(out=ogT.rearrange("p c n -> p (c n)"), in_=bTb[:, 0:256]), reads=[bkT], writes=["ogT"])
                    b2, bk2 = ps.two()
                    for half in range(2):
                        for q in range(2):
                            P.pe(lambda e, q=q, half=half, b2=b2: e.matmul(b2[:, half, :], lhsT=ogT[:, q, :], rhs=Uo[:, q, half * 512:(half + 1) * 512],
                                                                          start=(q == 0), stop=(q == 1)), reads=["ogT", uk], writes=[bk2[half]])
                    ypt = yp[tile % 2]
                    ypk = ("yp", tile % 2)
                    P.act(lambda e, b2=b2, ypt=ypt: e.copy(out=ypt.rearrange("p (h n) -> p h n", h=2), in_=b2), reads=bk2, writes=[ypk])
                    if h == 0:
                        P.dma("sp", lambda e, ypt=ypt, tile=tile: e.dma_start(out=Y_d[tile * 128:(tile + 1) * 128, :], in_=ypt), ("ypds", tile % 2),
                              reads=[ypk], writes=[("Y", tile)])
                    else:
                        P.dma("pool", lambda e, ypt=ypt, tile=tile: e.dma_start(out=Y_d[tile * 128:(tile + 1) * 128, :], in_=ypt, accum_op=ALU.add), ("ypdp", tile % 2),
                              reads=[ypk, ("Y", tile)], writes=[("Y", tile)])
                    supd(Sf, "Sf", kfT, "kfT", c, ElF)
                if not samp:
                    P.dma("sp", lambda e, si=si: e.dma_start(out=o_gla[si - 1, 0, h], in_=Sf), "ogla", reads=["Sf"], writes=[("o_gla", si, 0, h)])
                    final_keys.append(("o_gla", si, 0, h))
        P.barrier()

    def stage_s5(l):
        A.reset(MARK)
        ybuf = A.a(8 * T, BF16, "p (c t) -> p c t", c=8)
        Rg = A.a(5120)
        Bb = A.a(2 * 1024, BF16, "p (k t s) -> p k t s", k=2, t=8)
        dT = A.a(8)
        acol = A.a(96, F32, "p (k t) -> p k t", k=3)
        x0c = A.a(64, F32, "p (k t) -> p k t", k=2)
        thc = A.a(32)
        rc = A.a(32)
        kci = A.a(32, I32)
        kcf = A.a(32)
        j1i = A.a(128, I32)
        J1 = A.a(128)
        rC = A.a(8)
        rS = A.a(8)
        tA = A.a(8)
        tB = A.a(8)
        xe = A.a(16, F32, "p (k t) -> p k t", k=2)
        r0 = ring[0].bitcast(F32)
        r1 = ring[1].bitcast(F32)
        pb = [r0[:, i * 1024:(i + 1) * 1024] for i in range(4)] + [r1[:, i * 1024:(i + 1) * 1024] for i in range(4)]
        arow = Rg[:, 0:3072].rearrange("p (k n) -> p k n", k=3)
        braw = Rg[:, 3072:5120].rearrange("p (k n) -> p k n", k=2)
        Ct = Rg[:, 0:1024].rearrange("p (t j) -> p t j", t=8)
        St = Rg[:, 1024:2048].rearrange("p (t j) -> p t j", t=8)
        Rt = Rg[:, 2048:3072].rearrange("p (t j) -> p t j", t=8)
        xx = Rg[:, 3072:4096].bitcast(BF16)
        xre = xx[:, 0:1024].rearrange("p (t j) -> p t j", t=8)
        xim = xx[:, 1024:2048].rearrange("p (t j) -> p t j", t=8)
        Cb = Rg[:, 4096:5120].bitcast(BF16).rearrange("p (k t s) -> p k t s", k=2, t=8)
        P.dma("sp", lambda e: e.dma_start(out=dT, in_=s5_dT), "dT", writes=["dT"])
        P.pool(lambda e: e.iota(j1i, pattern=[[1, 128]], base=1, channel_multiplier=0), writes=["j1i"])
        P.dve(lambda e: e.tensor_copy(J1, j1i), reads=["j1i"], writes=["J1"])

        def wrap_sin(dst, tmp, tmpi, nm):
            P.dve(lambda e: e.tensor_scalar(out=tmp, in0=dst, scalar1=1.0 / (2 * PI), scalar2=None, op0=ALU.mult), reads=[nm], writes=[nm + "t"])
            P.dve(lambda e: e.tensor_copy(tmpi, tmp), reads=[nm + "t"], writes=[nm + "i"])
            P.dve(lambda e: e.tensor_copy(tmp, tmpi), reads=[nm + "i"], writes=[nm + "t"])
            P.dve(lambda e: e.scalar_tensor_tensor(out=dst, in0=tmp, scalar=-2 * PI, in1=dst, op0=ALU.mult, op1=ALU.add), reads=[nm + "t", nm], writes=[nm])
            P.dve(lambda e: e.tensor_scalar(out=dst, in0=dst, scalar1=PI, scalar2=-PI, op0=ALU.min, op1=ALU.max), reads=[nm], writes=[nm])
            P.act(lambda e: e.activation(out=dst, in_=dst, func=AF.Sin), reads=[nm], writes=[nm])

        for tg in range(4):
            for d in range(2):
                tsl = slice(tg * 8, (tg + 1) * 8)
                P.dma("sp", lambda e: e.dma_start(out=acol.rearrange("p k t -> p (k t)"), in_=s5_acol[d]), "acol", writes=["acol"])
                P.dma("sp", lambda e: e.dma_start(out=x0c[:, 0, :], in_=s5_x0[d, 0]), "x0c0", writes=["x0c"])
                P.dma("sp", lambda e: e.dma_start(out=x0c[:, 1, :], in_=s5_x0[d, 1]), "x0c1", writes=["x0c"])
                P.act(lambda e: e.activation(out=acol[:, 2, :], in_=acol[:, 2, :], func=AF.Exp), reads=["acol"], writes=["acol"])
                P.dve(lambda e: e.tensor_tensor(out=rc, in0=acol[:, 0, :], in1=acol[:, 2, :], op=ALU.mult), reads=["acol"], writes=["rc"])
                P.act(lambda e: e.activation(out=rc, in_=rc, func=AF.Exp), reads=["rc"], writes=["rc"])
                P.dve(lambda e: e.tensor_tensor(out=thc, in0=acol[:, 1, :], in1=acol[:, 2, :], op=ALU.mult), reads=["acol"], writes=["thc"])
                P.dve(lambda e: e.tensor_scalar(out=kcf, in0=thc, scalar1=1.0 / (2 * PI), scalar2=None, op0=ALU.mult), reads=["thc"], writes=["kcf"])
                P.dve(lambda e: e.tensor_copy(kci, kcf), reads=["kcf"], writes=["kci"])
                P.dve(lambda e: e.tensor_copy(kcf, kci), reads=["kci"], writes=["kcf"])
                P.dve(lambda e: e.scalar_tensor_tensor(out=thc, in0=kcf, scalar=-2 * PI, in1=thc, op0=ALU.mult, op1=ALU.add), reads=["kcf", "thc"], writes=["thc"])
                w0, w1, w2, w3, w4, w5 = pb[0], pb[1], pb[2], pb[3], pb[4], pb[5]
                wi_ = pb[6].bitcast(I32)
                P.dma("sp", lambda e: e.dma_start(out=arow, in_=s5_arow[d][:, tg * 1024:(tg + 1) * 1024].partition_broadcast(128)), "arow", writes=["arow"])
                for k in range(2):
                    P.dma("sp", lambda e: e.dma_start(out=braw[:, k, :].rearrange("p (t s) -> p t s", t=8), in_=s5_bblk[d, k][:, tg * 8:(tg + 1) * 8, :]),
                          ("braw", k), writes=["braw"])
                ar = arow[:, 0, :]
                ai_ = arow[:, 1, :]
                stp = arow[:, 2, :]
                P.act(lambda e: e.activation(out=stp, in_=stp, func=AF.Exp), reads=["arow"], writes=["arow"])
                P.dve(lambda e: e.tensor_tensor(out=w0, in0=ar, in1=stp, op=ALU.mult), reads=["arow"], writes=["w0"])
                P.act(lambda e: e.activation(out=w0, in_=w0, func=AF.Exp), reads=["w0"], writes=["w0"])
                P.dve(lambda e: e.tensor_tensor(out=w2, in0=ai_, in1=stp, op=ALU.mult), reads=["arow"], writes=["w2"])
                P.dve(lambda e: e.tensor_scalar(out=w3, in0=w2, scalar1=PI / 2, scalar2=None, op0=ALU.add), reads=["w2"], writes=["w3"])
                wrap_sin(w2, w5, wi_, "w2")
                wrap_sin(w3, w5, wi_, "w3")
                P.dve(lambda e: e.tensor_tensor(out=w3, in0=w3, in1=w0, op=ALU.mult), reads=["w3", "w0"], writes=["w3"])
                P.dve(lambda e: e.tensor_tensor(out=w2, in0=w2, in1=w0, op=ALU.mult), reads=["w2", "w0"], writes=["w2"])
                P.dve(lambda e: e.tensor_scalar(out=w3, in0=w3, scalar1=-1.0, scalar2=None, op0=ALU.add), reads=["w3"], writes=["w3"])
                P.dve(lambda e: e.tensor_tensor(out=w0, in0=ar, in1=ar, op=ALU.mult), reads=["arow", "w0"], writes=["w0"])
                P.dve(lambda e: e.tensor_tensor(out=w1, in0=ai_, in1=ai_, op=ALU.mult), reads=["arow"], writes=["w1"])
                P.dve(lambda e: e.tensor_tensor(out=w0, in0=w0, in1=w1, op=ALU.add), reads=["w0", "w1"], writes=["w0"])
                P.dve(lambda e: e.reciprocal(out=w0, in_=w0), reads=["w0"], writes=["w0"])
                P.dve(lambda e: e.tensor_tensor(out=w1, in0=w3, in1=ar, op=ALU.mult), reads=["w3", "arow", "w1"], writes=["w1"])
                P.dve(lambda e: e.tensor_tensor(out=w4, in0=w2, in1=ai_, op=ALU.mult), reads=["w2", "arow"], writes=["w4"])
                P.dve(lambda e: e.tensor_tensor(out=w1, in0=w1, in1=w4, op=ALU.add), reads=["w1", "w4"], writes=["w1"])
                P.dve(lambda e: e.tensor_tensor(out=w1, in0=w1, in1=w0, op=ALU.mult), reads=["w1", "w0"], writes=["w1"])
                P.dve(lambda e: e.tensor_tensor(out=w4, in0=w2, in1=ar, op=ALU.mult), reads=["w2", "arow", "w4"], writes=["w4"])
                P.dve(lambda e: e.tensor_tensor(out=w5, in0=w3, in1=ai_, op=ALU.mult), reads=["w3", "arow", "w2t", "w3t"], writes=["w5"])
                P.dve(lambda e: e.tensor_tensor(out=w4, in0=w4, in1=w5, op=ALU.subtract), reads=["w4", "w5"], writes=["w4"])
                P.dve(lambda e: e.tensor_tensor(out=w4, in0=w4, in1=w0, op=ALU.mult), reads=["w4", "w0"], writes=["w4"])
                P.dve(lambda e: e.tensor_tensor(out=w0, in0=braw[:, 0, :], in1=w1, op=ALU.mult), reads=["braw", "w1", "w0", "w4"], writes=["w0"])
                P.dve(lambda e: e.tensor_tensor(out=w2, in0=braw[:, 1, :], in1=w4, op=ALU.mult), reads=["braw", "w4", "w2"], writes=["w2"])
                P.dve(lambda e: e.tensor_tensor(out=Bb[:, 0, :, :].rearrange("p t s -> p (t s)"), in0=w0, in1=w2, op=ALU.subtract), reads=["w0", "w2"], writes=["Bb"])
                P.dve(lambda e: e.tensor_tensor(out=w0, in0=braw[:, 1, :], in1=w1, op=ALU.mult), reads=["braw", "w1", "w0"], writes=["w0"])
                P.dve(lambda e: e.tensor_tensor(out=w2, in0=braw[:, 0, :], in1=w4, op=ALU.mult), reads=["braw", "w4", "w2"], writes=["w2"])
                P.dve(lambda e: e.tensor_tensor(out=Bb[:, 1, :, :].rearrange("p t s -> p (t s)"), in0=w0, in1=w2, op=ALU.add), reads=["w0", "w2"], writes=["Bb"])
                P.barrier()
                ang, w5t = pb[0], pb[1]
                angi = pb[2].bitcast(I32)
                for (tab, shift, nm) in ((St, 0.0, "St"), (Ct, PI / 2, "Ct")):
                    P.dve(lambda e: e.tensor_tensor(out=ang.rearrange("p (t j) -> p t j", t=8), in0=J1.unsqueeze(1).to_broadcast([128, 8, 128]),
                                                    in1=thc[:, tsl].unsqueeze(2).to_broadcast([128, 8, 128]), op=ALU.mult), reads=["J1", "thc", "ang"], writes=["ang"])
                    P.dve(lambda e: e.tensor_scalar(out=ang, in0=ang, scalar1=shift, scalar2=None, op0=ALU.add), reads=["ang"], writes=["ang"])
                    wrap_sin(ang, w5t, angi, "ang")
                    P.dve(lambda e: e.tensor_copy(tab.rearrange("p t j -> p (t j)"), ang), reads=["ang"], writes=[nm])
                P.dve(lambda e: e.tensor_copy(Rt, rc[:, tsl].unsqueeze(2).to_broadcast([128, 8, 128])), reads=["rc"], writes=["Rt"])
                P.dve(lambda e: e.memset(Rt[:, :, 0:1], 0.0), reads=["Rt"], writes=["Rt"])
                P.dve(lambda e: e.tensor_tensor(out=rC, in0=rc[:, tsl], in1=Ct[:, :, 127], op=ALU.mult), reads=["rc", "Ct"], writes=["rC"])
                P.dve(lambda e: e.tensor_tensor(out=rS, in0=rc[:, tsl], in1=St[:, :, 127], op=ALU.mult), reads=["rc", "St"], writes=["rS"])
                for k in range(2):
                    P.dma("pool", lambda e: e.dma_start(out=Cb[:, k, :, :], in_=s5_cblk[d, k][:, tg * 8:(tg + 1) * 8, :]), ("cb", k), writes=["Cb"])
                P.dve(lambda e: e.tensor_scalar(out=Cb[:, 1, :, :], in0=Cb[:, 1, :, :], scalar1=-1.0, scalar2=None, op0=ALU.mult), reads=["Cb"], writes=["Cb"])
                P.barrier()
                vre, vim, zre, zim, w0, w1 = pb[0], pb[1], pb[2], pb[3], pb[4], pb[5]
                v3 = {"re": vre.rearrange("p (t j) -> p t j", t=8), "im": vim.rearrange("p (t j) -> p t j", t=8)}
                z3 = {"re": zre.rearrange("p (t j) -> p t j", t=8), "im": zim.rearrange("p (t j) -> p t j", t=8)}
                w0v = w0.rearrange("p (t j) -> p t j", t=8)
                w1v = w1.rearrange("p (t j) -> p t j", t=8)
                Rflat = Rt.rearrange("p t j -> p (t j)")
                for si, (t0, nch, j, samp) in enumerate(SEQS):
                    order = list(range(nch)) if d == 0 else list(reversed(range(nch)))
                    for ci, c in enumerate(order):
                        tile = t0 + c
                        tk = slice(tile * 128, (tile + 1) * 128)
                        bre, kre = ps.two()
                        bim, kim = ps.two()
                        for t in range(8):
                            fc = (tg * 8 + t) // 4
                            P.pe(lambda e: e.matmul(bre[:, t // 4, (t % 4) * 128:(t % 4 + 1) * 128], lhsT=Bb[:, 0, t, :], rhs=hT[:, fc, tk],
                                                    start=True, stop=True), reads=["Bb"], writes=[kre[t // 4]])
                            P.pe(lambda e: e.matmul(bim[:, t // 4, (t % 4) * 128:(t % 4 + 1) * 128], lhsT=Bb[:, 1, t, :], rhs=hT[:, fc, tk],
                                                    start=True, stop=True), reads=["Bb"], writes=[kim[t // 4]])
                        br3 = bre.rearrange("p a (b j) -> p (a b) j", j=128)
                        bi3 = bim.rearrange("p a (b j) -> p (a b) j", j=128)
                        if d == 1:
                            br3 = br3[:, :, ::-1]
                            bi3 = bi3[:, :, ::-1]
                        P.dve(lambda e: e.tensor_tensor(out=v3["re"], in0=br3, in1=Ct, op=ALU.mult), reads=kre + ["Ct"], writes=["vre"])
                        P.dve(lambda e: e.tensor_tensor(out=w0v, in0=bi3, in1=St, op=ALU.mult), reads=kim + ["St", "w0"], writes=["w0"])
                        P.dve(lambda e: e.tensor_tensor(out=vre, in0=vre, in1=w0, op=ALU.add), reads=["vre", "w0"], writes=["vre"])
                        P.dve(lambda e: e.tensor_tensor(out=v3["im"], in0=bi3, in1=Ct, op=ALU.mult), reads=kim + ["Ct"], writes=["vim"])
                        P.dve(lambda e: e.tensor_tensor(out=w0v, in0=br3, in1=St, op=ALU.mult), reads=kre + ["St", "w0"], writes=["w0"])
                        P.dve(lambda e: e.tensor_tensor(out=vim, in0=vim, in1=w0, op=ALU.subtract), reads=["vim", "w0"], writes=["vim"])
                        if ci == 0:
                            if samp:
                                for k, nm in ((0, "re"), (1, "im")):
                                    P.dve(lambda e: e.tensor_tensor(out=tA, in0=rc[:, tsl], in1=x0c[:, k, tsl], op=ALU.mult), reads=["rc", "x0c", "tA"], writes=["tA"])
                                    P.dve(lambda e: e.tensor_tensor(out=v3[nm][:, :, 0], in0=v3[nm][:, :, 0], in1=tA, op=ALU.add), reads=["v" + nm, "tA"], writes=["v" + nm])
                        else:
                            zlr = z3["re"][:, :, 127]
                            zli = z3["im"][:, :, 127]
                            P.dve(lambda e: e.tensor_tensor(out=tA, in0=rC, in1=zlr, op=ALU.mult), reads=["rC", "zre", "tA"], writes=["tA"])
                            P.dve(lambda e: e.tensor_tensor(out=tB, in0=rS, in1=zli, op=ALU.mult), reads=["rS", "zim", "tB"], writes=["tB"])
                            P.dve(lambda e: e.tensor_tensor(out=tA, in0=tA, in1=tB, op=ALU.subtract), reads=["tA", "tB"], writes=["tA"])
                            P.dve(lambda e: e.tensor_tensor(out=v3["re"][:, :, 0], in0=v3["re"][:, :, 0], in1=tA, op=ALU.add), reads=["vre", "tA"], writes=["vre"])
                            P.dve(lambda e: e.tensor_tensor(out=tA, in0=rS, in1=zlr, op=ALU.mult), reads=["rS", "zre", "tA"], writes=["tA"])
                            P.dve(lambda e: e.tensor_tensor(out=tB, in0=rC, in1=zli, op=ALU.mult), reads=["rC", "zim", "tB"], writes=["tB"])
                            P.dve(lambda e: e.tensor_tensor(out=tA, in0=tA, in1=tB, op=ALU.add), reads=["tA", "tB"], writes=["tA"])
                            P.dve(lambda e: e.tensor_tensor(out=v3["im"][:, :, 0], in0=v3["im"][:, :, 0], in1=tA, op=ALU.add), reads=["vim", "tA"], writes=["vim"])
                        P.dve(lambda e: e.tensor_tensor_scan(out=zre, data0=Rflat, data1=vre, initial=0.0, op0=ALU.mult, op1=ALU.add), reads=["Rt", "vre", "zre"], writes=["zre"])
                        P.dve(lambda e: e.tensor_tensor_scan(out=zim, data0=Rflat, data1=vim, initial=0.0, op0=ALU.mult, op1=ALU.add), reads=["Rt", "vim", "zim"], writes=["zim"])
                        xr = xre if d == 0 else xre[:, :, ::-1]
                        xi = xim if d == 0 else xim[:, :, ::-1]
                        P.dve(lambda e: e.tensor_tensor(out=w0v, in0=z3["re"], in1=Ct, op=ALU.mult), reads=["zre", "Ct", "w0"], writes=["w0"])
                        P.dve(lambda e: e.tensor_tensor(out=w1v, in0=z3["im"], in1=St, op=ALU.mult), reads=["zim", "St", "w1"], writes=["w1"])
                        P.dve(lambda e: e.tensor_tensor(out=xr, in0=w0v, in1=w1v, op=ALU.subtract), reads=["w0", "w1", "xre"], writes=["xre"])
                        P.dve(lambda e: e.tensor_tensor(out=w0v, in0=z3["re"], in1=St, op=ALU.mult), reads=["zre", "St", "w0"], writes=["w0"])
                        P.dve(lambda e: e.tensor_tensor(out=w1v, in0=z3["im"], in1=Ct, op=ALU.mult), reads=["zim", "Ct", "w1"], writes=["w1"])
                        P.dve(lambda e: e.tensor_tensor(out=xi, in0=w0v, in1=w1v, op=ALU.add), reads=["w0", "w1", "xim"], writes=["xim"])
                        by, kby = ps.one()
                        for fcl in range(2):
                            for q in range(4):
                                t = fcl * 4 + q
                                P.pe(lambda e: e.matmul(by[:, fcl * 128:(fcl + 1) * 128], lhsT=Cb[:, 0, t, :], rhs=xre[:, t, :], start=(q == 0), stop=False),
                                     reads=["Cb", "xre"], writes=[kby])
                                P.pe(lambda e: e.matmul(by[:, fcl * 128:(fcl + 1) * 128], lhsT=Cb[:, 1, t, :], rhs=xim[:, t, :], start=False, stop=(q == 3)),
                                     reads=["Cb", "xim"], writes=[kby])
                        yb = ybuf[:, tg * 2:tg * 2 + 2, tk]
                        by3 = by[:, 0:256].rearrange("p (c n) -> p c n", c=2)
                        if d == 0:
                            P.act(lambda e: e.copy(out=yb, in_=by3), reads=[kby], writes=[("ybuf", tg, tile)])
                        else:
                            w2v = w0[:, 0:256].rearrange("p (c n) -> p c n", c=2)
                            P.dve(lambda e: e.tensor_tensor(out=w2v, in0=by3, in1=yb, op=ALU.add), reads=[kby, ("ybuf", tg, tile), "w0"], writes=["w0"])
                            for fcl in range(2):
                                fc = tg * 2 + fcl
                                P.dve(lambda e: e.scalar_tensor_tensor(out=ybuf[:, fc, tk], in0=hT[:, fc, tk], scalar=dT[:, fc:fc + 1],
                                                                       in1=w0[:, fcl * 128:(fcl + 1) * 128], op0=ALU.mult, op1=ALU.add),
                                      reads=["w0", "dT"], writes=[("ybuf", tg, tile)])
                    if not samp:
                        zlr = z3["re"][:, :, 127]
                        zli = z3["im"][:, :, 127]
                        Cl = Ct[:, :, 127]
                        Sl = St[:, :, 127]
                        P.dve(lambda e: e.tensor_tensor(out=tA, in0=Cl, in1=zlr, op=ALU.mult), reads=["Ct", "zre", "tA"], writes=["tA"])
                        P.dve(lambda e: e.tensor_tensor(out=tB, in0=Sl, in1=zli, op=ALU.mult), reads=["St", "zim", "tB"], writes=["tB"])
                        P.dve(lambda e: e.tensor_tensor(out=xe[:, 0, :], in0=tA, in1=tB, op=ALU.subtract), reads=["tA", "tB"], writes=["xe0"])
                        P.dve(lambda e: e.tensor_tensor(out=tA, in0=Sl, in1=zlr, op=ALU.mult), reads=["St", "zre", "tA"], writes=["tA"])
                        P.dve(lambda e: e.tensor_tensor(out=tB, in0=Cl, in1=zli, op=ALU.mult), reads=["Ct", "zim", "tB"], writes=["tB"])
                        P.dve(lambda e: e.tensor_tensor(out=xe[:, 1, :], in0=tA, in1=tB, op=ALU.add), reads=["tA", "tB"], writes=["xe1"])
                        for k in range(2):
                            key = ("o_s5", si, d, k, tg)
                            P.dma("sp", lambda e: e.dma_start(out=o_s5[si - 1, d, k][:, tg * 8:(tg + 1) * 8], in_=xe[:, k, :]), ("os5", k),
                                  reads=["xe%d" % k], writes=[key])
                            final_keys.append(key)
                P.barrier()
        for fc in range(8):
            for c0 in range(0, T, 512):
                yb = ybuf[:, fc, c0:c0 + 512]
                w0s, w1s = pb[4][:, 0:512], pb[5][:, 0:512]
                P.act(lambda e: e.activation(out=w0s, in_=yb, func=AF.Square), reads=["g0"], writes=["g0"])
                P.dve(lambda e: e.tensor_scalar(out=w0s, in0=w0s, scalar1=0.044715, scalar2=1.0, op0=ALU.mult, op1=ALU.add), reads=["g0"], writes=["g0"])
                P.dve(lambda e: e.tensor_tensor(out=w0s, in0=w0s, in1=yb, op=ALU.mult), reads=["g0"], writes=["g0"])
                P.act(lambda e: e.activation(out=w1s, in_=w0s, func=AF.Sigmoid, scale=1.5957691216057308), reads=["g0", "g1"], writes=["g1"])
                P.dve(lambda e: e.tensor_tensor(out=yb, in0=yb, in1=w1s, op=ALU.mult), reads=["g1"], writes=[("gy", fc, c0)])
        U0 = ring[2].rearrange("p (c n) -> p c n", c=8)
        U1 = ring[3].rearrange("p (c n) -> p c n", c=8)
        k0, k1 = ("ring", 2), ("ring", 3)
        cast_load(U0, kcview(s5_w_glu[:, 0:D]), k0, ("ringd", 2))
        cast_load(U1, kcview(s5_w_glu[:, D:2 * D]), k1, ("ringd", 3))
        sgb = pb[0]
        yts = [pb[2], pb[3]]
        P.barrier()
        for tt in range(NT):
            tk = slice(tt * 128, (tt + 1) * 128)
            ba, ka_ = ps.two()
            bb, kb_ = ps.two()
            for half in range(2):
                for kc in range(8):
                    P.pe(lambda e: e.matmul(ba[:, half, :], lhsT=ybuf[:, kc, tk], rhs=U0[:, kc, half * 512:(half + 1) * 512],
                                            start=(kc == 0), stop=(kc == 7)), reads=[k0], writes=[ka_[half]])
                for kc in range(8):
                    P.pe(lambda e: e.matmul(bb[:, half, :], lhsT=ybuf[:, kc, tk], rhs=U1[:, kc, half * 512:(half + 1) * 512],
                                            start=(kc == 0), stop=(kc == 7)), reads=[k1], writes=[kb_[half]])
            P.act(lambda e: e.activation(out=sgb.rearrange("p (h n) -> p h n", h=2), in_=bb, func=AF.Sigmoid), reads=kb_, writes=["sgb"])
            yt = yts[tt % 2]
            P.dve(lambda e: e.tensor_tensor(out=yt.rearrange("p (h n) -> p h n", h=2), in0=ba, in1=sgb.rearrange("p (h n) -> p h n", h=2), op=ALU.mult),
                  reads=ka_ + ["sgb"], writes=[("gyt", tt % 2)])
            P.dma("sp", lambda e: e.dma_start(out=Y_d[tt * 128:(tt + 1) * 128, :], in_=yt), ("gyd", tt % 2), reads=[("gyt", tt % 2)], writes=[("Y", tt)])
        P.barrier()

    n_done = 0
    if os.environ.get("DBG_ONLY_MOE"):
        stage_mod(0)
        stage_moe(0, x0, xout, "xout")
        n_done = n_sub
    for l in range(4):
        if n_done >= n_sub:
            break
        stage_mod(l)
        src = x0 if l == 0 else xs_d
        last = (n_done + 1 == n_sub)
        dst, dname = (xout, "xout") if last else (xs_d, "xs")
        A.reset(MARK)
        stage_h(src, 0, 1)
        if l % 3 == 0:
            stage_ret(l, l // 3)
        elif l % 3 == 1:
            stage_gla(l)
        else:
            stage_s5(l)
        A.reset(MARK)
        stage_epi(l, 0, src, dst, dname)
        n_done += 1
        if n_done >= n_sub:
            break
        if skip_moe:
            continue
        last = (n_done + 1 == n_sub)
        dst, dname = (xout, "xout") if last else (xs_d, "xs")
        stage_moe(l, xs_d, dst, dname)
        n_done += 1
    for tt in range(NT):
        final_keys.append(("xout", tt))
    P.emit(final_keys=final_keys)
    return P


def _rot_tables():
    L, GW, dk = 2048, 64, 256
    rows = L // GW
    row = np.repeat(np.arange(rows, dtype=np.float32), GW)
    col = np.tile(np.arange(GW, dtype=np.float32), rows)
    nf = dk // 4
    inv = (10000.0 ** (-np.arange(nf, dtype=np.float32) / nf)).astype(np.float32)
    ang = np.concatenate([row[:, None] * inv, col[:, None] * inv], axis=-1)
    return np.ascontiguousarray(np.cos(ang).T.astype(np.float32)), np.ascontiguousarray(np.sin(ang).T.astype(np.float32))


def _col32(a):
    return np.ascontiguousarray(a.reshape(32, 2, 64).transpose(1, 2, 0).reshape(128, 32))


def _prep_shared(inp):
    f = np.float32
    sh = {}
    sh["w_mod"] = inp["w_mod"]
    sh["b_mod"] = inp["b_mod"]
    sh["b_modT"] = np.ascontiguousarray(inp["b_mod"].reshape(4, 48, 128).transpose(0, 2, 1))
    sh["ln_g"] = inp["ln_g"]
    sh["ln_b"] = inp["ln_b"]
    sh["ret_w_in"] = inp["ret_w_in"]
    sh["ret_w_out"] = inp["ret_w_out"]
    sh["ret_decay"] = np.ascontiguousarray(inp["ret_decay"].reshape(16))
    rc, rs = _rot_tables()
    sh["rot_cos"], sh["rot_sin"] = rc, rs
    sh["gla_w_in"] = np.ascontiguousarray(inp["gla_w_in"][0])
    sh["gla_w_a1"] = np.ascontiguousarray(inp["gla_w_a1"][0])
    sh["gla_w_a2a"] = np.ascontiguousarray(np.concatenate([inp["gla_w_a2"][0], inp["gla_b_a"][0][:, None, :]], axis=1))
    sh["gla_w_out"] = np.ascontiguousarray(inp["gla_w_out"][0])
    ar, ai, ls = inp["s5_a_re"][0], inp["s5_a_im"][0], inp["s5_log_step"][0]
    lse = np.broadcast_to(ls[:, :, None], (2, 64, 64))
    sh["s5_arow"] = np.ascontiguousarray(np.stack([ar.reshape(2, 4096), ai.reshape(2, 4096), lse.reshape(2, 4096)], axis=1))
    sh["s5_acol"] = np.ascontiguousarray(np.stack([np.concatenate([_col32(ar[d]), _col32(ai[d]), _col32(lse[d])], axis=1) for d in range(2)]))
    bb = np.zeros((2, 2, 128, 32, 128), f)
    cb = np.zeros((2, 2, 128, 32, 128), f)
    for d in range(2):
        for k, (bsrc, csrc) in enumerate(((inp["s5_b_re"][0, d], inp["s5_c_re"][0, d]), (inp["s5_b_im"][0, d], inp["s5_c_im"][0, d]))):
            for g in range(64):
                t = g // 2
                so = (g % 2) * 64
                fo = (g % 8) * 16
                bb[d, k, fo:fo + 16, t, so:so + 64] = bsrc[g].T
                cb[d, k, so:so + 64, t, fo:fo + 16] = csrc[g].T
    sh["s5_bblk"], sh["s5_cblk"] = bb, cb
    sh["s5_dT"] = np.ascontiguousarray(inp["s5_d"][0].reshape(8, 128).T)
    sh["s5_w_glu"] = np.ascontiguousarray(inp["s5_w_glu"][0])
    sh["moe_w_router"] = inp["moe_w_router"]
    sh["moe_b_router"] = inp["moe_b_router"]
    sh["moe_w_gu"] = inp["moe_w_gu"]
    sh["moe_b_guT"] = np.ascontiguousarray(inp["moe_b_gu"].reshape(4, 32, 16, 128).transpose(0, 3, 1, 2).reshape(4, 128, 512))
    sh["moe_w_down"] = inp["moe_w_down"]
    sh["moe_b_down"] = inp["moe_b_down"]
    return sh


def _prep_core(inp, sh, c):
    m = dict(sh)
    m["x0"] = np.ascontiguousarray(np.concatenate([inp["x_sample"][c], inp["x_prompt"][2 * c], inp["x_prompt"][2 * c + 1]], axis=0))
    cond = np.stack([inp["c_ctx"], inp["c"][c]])
    m["condT"] = np.ascontiguousarray(cond.reshape(2, 8, 128).transpose(2, 0, 1).reshape(128, 16))
    m["state_ret"] = np.ascontiguousarray(inp["state_ret"][c])
    m["state_gla"] = np.ascontiguousarray(inp["state_gla"][c, 0])
    m["s5_x0"] = np.ascontiguousarray(np.stack([np.stack([_col32(inp["state_s5_re"][c, 0, d]), _col32(inp["state_s5_im"][c, 0, d])]) for d in range(2)]))
    return m


_NC_CACHE = {}


def _get_nc(n_sub=8):
    if n_sub not in _NC_CACHE:
        nc = bass.Bass("TRN2", target_bir_lowering=False)
        build(nc, n_sub)
        _NC_CACHE[n_sub] = nc
    return _NC_CACHE[n_sub]


def _uncol32(a):
    return a.reshape(2, 64, 32).transpose(2, 0, 1).reshape(64, 64)


def kernel(**inputs):
    inp = {k: np.asarray(v) for k, v in inputs.items()}
    sh = _prep_shared(inp)
    nc = _get_nc(8)
    in_maps = [_prep_core(inp, sh, c) for c in range(8)]
    res = run_bass_kernel_spmd(nc, in_maps, core_ids=list(range(8)))
    f = np.float32
    y_prompt = np.zeros((16, 256, 1024), f)
    y_sample = np.zeros((8, 2048, 1024), f)
    new_ret = np.zeros((16, 2, 2, 4, 256, 512), f)
    new_gla = np.zeros((16, 1, 2, 4, 128, 256), f)
    new_re = np.zeros((16, 1, 2, 64, 64), f)
    new_im = np.zeros((16, 1, 2, 64, 64), f)
    for c in range(8):
        r = res.results[c]
        xo = r["xout"]
        y_sample[c] = xo[0:2048]
        y_prompt[2 * c] = xo[2048:2304]
        y_prompt[2 * c + 1] = xo[2304:2560]
        for s in range(2):
            b = 2 * c + s
            new_ret[b] = r["o_ret"][s]
            new_gla[b, 0] = r["o_gla"][s]
            for d in range(2):
                new_re[b, 0, d] = _uncol32(r["o_s5"][s, d, 0])
                new_im[b, 0, d] = _uncol32(r["o_s5"][s, d, 1])
    return (y_prompt, y_sample, new_ret, new_gla, new_re, new_im)
```
